# Optimizing a Trainium2 kernel written in Bass

```python
import jax
import jax.numpy as jnp
from jax import lax
import numpy as np

D_MODEL = 2048
BATCH = 8
SEQ = 2048
DEPTH = 1

GRID_W = 64
CTX_LEN = 256

D_CONV = D_MODEL // 2
CONV_WIDTH = 31
D_RWKV = D_MODEL // 2
HEAD_SIZE = 64
N_HEADS_RWKV = D_RWKV // HEAD_SIZE
D_DECAY_LORA = 64
D_AAA_LORA = 64
D_GATE_LORA = 160
N_DIR = 2
GN_EPS = 64e-5
D_SHIFT = 3 * D_RWKV + N_DIR * D_DECAY_LORA + N_DIR * D_AAA_LORA + D_GATE_LORA
P_IN = 2 * D_CONV + D_SHIFT + 2 * D_MODEL
RWKV_SPLITS = (D_RWKV, 2 * D_RWKV, 3 * D_RWKV, 3 * D_RWKV + N_DIR * D_DECAY_LORA,
               3 * D_RWKV + N_DIR * (D_DECAY_LORA + D_AAA_LORA))
N_EXPERTS = 32
TOP_K = 4
D_EXPERT = D_MODEL
SWIGLU_LIMIT = 7.0
SWIGLU_ALPHA = 1.702
LN_EPS = 1e-5
DEEPNORM_ALPHA = (2.0 * DEPTH) ** 0.25
DEEPNORM_BETA = (8.0 * DEPTH) ** -0.25

kernel_name = 'hybrid_conformer_rwkv7_moe_dit_block'


def _layer_norm(h):
    hf = h.astype(jnp.float32)
    mu = jnp.mean(hf, -1, keepdims=True)
    var = jnp.mean(jnp.square(hf - mu), -1, keepdims=True)
    return ((hf - mu) * lax.rsqrt(var + LN_EPS)).astype(h.dtype)


def _modulate(h, shift, scale):
    return _layer_norm(h) * (1.0 + scale[..., None, :]) + shift[..., None, :]


def _post_norm(res, out, gate, g, b):
    return _layer_norm(DEEPNORM_ALPHA * res + gate[..., None, :] * out) * g + b


def _centred_shift(z, mu_prev, mu_next):
    zero = jnp.zeros_like(z[:, :1])
    prev = jnp.concatenate([zero, z[:, :-1]], axis=1)
    nxt = jnp.concatenate([z[:, 1:], zero], axis=1)
    return z + mu_prev * (prev - z) + mu_next * (nxt - z)


def _conv_branch(zc, p, rows):
    B, T, _ = zc.shape
    h = zc[..., :D_CONV] * jax.nn.sigmoid(zc[..., D_CONV:])
    if rows is not None:
        h = h.reshape(B * rows, GRID_W, D_CONV)
    h = lax.conv_general_dilated(h, p['conv_w'][:, None, :], window_strides=(1,),
                                 padding=[(CONV_WIDTH // 2, CONV_WIDTH // 2)],
                                 dimension_numbers=('NWC', 'WIO', 'NWC'),
                                 feature_group_count=D_CONV)
    h = h.reshape(B, T, D_CONV) + p['conv_b']
    h = _layer_norm(h) * p['conv_ln_g'] + p['conv_ln_b']
    return jax.nn.silu(h) @ p['w_conv_o'] + p['b_conv_o']


def _rwkv_prepare(zr, p):
    B, T, _ = zr.shape
    zr = _centred_shift(zr, p['shift_mu'][0], p['shift_mu'][1])
    r, k, v, wd, ad, gd = jnp.split(zr, RWKV_SPLITS, axis=-1)
    heads = lambda t: t.reshape(B, T, N_HEADS_RWKV, HEAD_SIZE)
    g = jax.nn.sigmoid(gd) @ p['g2']
    kk = heads(k * p['k_k']).astype(jnp.float32)
    kk = kk / jnp.maximum(jnp.sqrt(jnp.sum(kk * kk, -1, keepdims=True)), 1e-12)
    dirs = []
    for d in range(N_DIR):
        wd_d = wd[..., d * D_DECAY_LORA:(d + 1) * D_DECAY_LORA]
        ad_d = ad[..., d * D_AAA_LORA:(d + 1) * D_AAA_LORA]
        w_log = -jax.nn.softplus(-(p['w0'][d] + jnp.tanh(wd_d) @ p['w2'][d])) - 0.5
        decay = jnp.exp(-jnp.exp(w_log.astype(jnp.float32)))
        a = jax.nn.sigmoid(p['a0'][d] + ad_d @ p['a2'][d])
        k_d = k * (1.0 + (a - 1.0) * p['k_a'])
        dirs.append((heads(decay), heads(k_d), heads(a).astype(jnp.float32)))
    return heads(r), heads(v), kk, g, dirs


def _wkv_scan(state0, r, decay, k, v, a_vec, b_vec, reverse):
    def step(S, inp):
        r_t, w_t, k_t, v_t, a_t, b_t = inp
        sa = jnp.einsum('bhvk,bhk->bhv', S, a_t)
        S = S * w_t[:, :, None, :] + sa[..., None] * b_t[:, :, None, :] + v_t[..., None] * k_t[:, :, None, :]
        return S, jnp.einsum('bhvk,bhk->bhv', S, r_t)
    xs = tuple(jnp.moveaxis(t.astype(jnp.float32), 1, 0) for t in (r, decay, k, v, a_vec, b_vec))
    S, ys = lax.scan(step, state0, xs, reverse=reverse)
    return S, jnp.moveaxis(ys, 0, 1)


def _rwkv_out(y, r, v, dirs, g, p):
    B, T = y.shape[:2]
    mu = jnp.mean(y, -1, keepdims=True)
    var = jnp.mean(jnp.square(y - mu), -1, keepdims=True)
    yn = ((y - mu) * lax.rsqrt(var + GN_EPS)).reshape(B, T, D_RWKV).astype(r.dtype)
    yn = yn * p['lnx_g'] + p['lnx_b']
    rk = sum(jnp.sum(r * k_d * p['r_k'], -1, keepdims=True) for (_, k_d, _) in dirs)
    bonus = (rk * v).reshape(B, T, D_RWKV)
    return ((yn + bonus) * g) @ p['w_rwkv_o']


def _merge(y_conv, y_rwkv, zg, p):
    mix = jax.nn.sigmoid(zg[..., :D_MODEL]) * y_conv + jax.nn.sigmoid(zg[..., D_MODEL:]) * y_rwkv
    return mix @ p['w_out'] + p['b_out']


def _split_proj(z):
    return z[..., :2 * D_CONV], z[..., 2 * D_CONV:2 * D_CONV + D_SHIFT], z[..., 2 * D_CONV + D_SHIFT:]


def _moe(u, p):
    B, T, D = u.shape
    t = u.reshape(B * T, D)
    logits = (t @ p['w_router'] + p['b_router']).astype(jnp.float32)
    top_vals, top_idx = lax.top_k(logits, TOP_K)
    gates = jax.nn.softmax(top_vals, axis=-1)
    combine = jnp.sum(jax.nn.one_hot(top_idx, N_EXPERTS, dtype=jnp.float32) * gates[..., None], axis=1).astype(t.dtype)
    out = jnp.zeros_like(t)
    for e in range(N_EXPERTS):
        h = t @ p['w_gate_up'][e] + p['b_gate_up'][e]
        gate = jnp.minimum(h[:, :D_EXPERT], SWIGLU_LIMIT)
        up = jnp.clip(h[:, D_EXPERT:], -SWIGLU_LIMIT, SWIGLU_LIMIT)
        hid = (up + 1.0) * (gate * jax.nn.sigmoid(SWIGLU_ALPHA * gate))
        out = out + combine[:, e:e + 1] * (hid @ p['w_down'][e] + p['b_down'][e])
    return out.reshape(B, T, D)


def setup_inputs(seed: int = 0) -> dict:
    key = jax.random.key(seed)
    ks = iter(jax.random.split(key, 48))
    nrm = lambda shape, scale: jax.random.normal(next(ks), shape, jnp.float32) * scale
    L = DEPTH
    D = D_MODEL
    w0_base = jnp.linspace(-6.5, -1.5, D_RWKV, dtype=jnp.float32)
    return {
        'x': nrm((BATCH, SEQ, D), 1.0),
        'c': nrm((BATCH, D), 1.0),
        'ctx': nrm((BATCH, CTX_LEN, D), 1.0),
        'c_ctx': nrm((D,), 1.0),
        'w_ada': nrm((L, D, 6 * D), D ** -0.5),
        'b_ada': nrm((L, 6 * D), 0.01),
        'w_in': nrm((L, D, P_IN), D ** -0.5),
        'b_in': nrm((L, P_IN), 0.01),
        'shift_mu': jax.random.uniform(next(ks), (L, 2, D_SHIFT), jnp.float32, 0.0, 0.5),
        'conv_w': nrm((L, CONV_WIDTH, D_CONV), CONV_WIDTH ** -0.5),
        'conv_b': nrm((L, D_CONV), 0.01),
        'conv_ln_g': 1.0 + nrm((L, D_CONV), 0.02),
        'conv_ln_b': nrm((L, D_CONV), 0.01),
        'w_conv_o': nrm((L, D_CONV, D), D_CONV ** -0.5 * DEEPNORM_BETA),
        'b_conv_o': nrm((L, D), 0.01),
        'w0': w0_base + nrm((L, N_DIR, D_RWKV), 0.1),
        'w2': nrm((L, N_DIR, D_DECAY_LORA, D_RWKV), 0.1 * D_DECAY_LORA ** -0.5),
        'a0': nrm((L, N_DIR, D_RWKV), 0.1),
        'a2': nrm((L, N_DIR, D_AAA_LORA, D_RWKV), D_AAA_LORA ** -0.5),
        'g2': nrm((L, D_GATE_LORA, D_RWKV), D_GATE_LORA ** -0.5),
        'k_k': 0.85 + nrm((L, D_RWKV), 0.02),
        'k_a': 1.0 + nrm((L, D_RWKV), 0.02),
        'r_k': nrm((L, N_HEADS_RWKV, HEAD_SIZE), 0.05),
        'lnx_g': 1.0 + nrm((L, D_RWKV), 0.02),
        'lnx_b': nrm((L, D_RWKV), 0.01),
        'w_rwkv_o': nrm((L, D_RWKV, D), D_RWKV ** -0.5 * DEEPNORM_BETA),
        'w_out': nrm((L, D, D), D ** -0.5 * DEEPNORM_BETA),
        'b_out': nrm((L, D), 0.01),
        'ln1_g': 1.0 + nrm((L, D), 0.02),
        'ln1_b': nrm((L, D), 0.01),
        'w_router': nrm((L, D, N_EXPERTS), D ** -0.5),
        'b_router': nrm((L, N_EXPERTS), 0.01),
        'w_gate_up': nrm((L, N_EXPERTS, D, 2 * D_EXPERT), D ** -0.5),
        'b_gate_up': nrm((L, N_EXPERTS, 2 * D_EXPERT), 0.01),
        'w_down': nrm((L, N_EXPERTS, D_EXPERT, D), D_EXPERT ** -0.5 * DEEPNORM_BETA),
        'b_down': nrm((L, N_EXPERTS, D), 0.01),
        'ln2_g': 1.0 + nrm((L, D), 0.02),
        'ln2_b': nrm((L, D), 0.01),
    }


def reference(x, c, ctx, c_ctx, w_ada, b_ada, w_in, b_in, shift_mu, conv_w, conv_b, conv_ln_g, conv_ln_b,
              w_conv_o, b_conv_o, w0, w2, a0, a2, g2, k_k, k_a, r_k, lnx_g, lnx_b, w_rwkv_o, w_out, b_out,
              ln1_g, ln1_b, w_router, b_router, w_gate_up, b_gate_up, w_down, b_down, ln2_g, ln2_b):
    B, n_lat, _ = x.shape
    rows = n_lat // GRID_W
    h_ctx = ctx
    for l in range(DEPTH):
        need_ctx = l < DEPTH - 1
        p = {'shift_mu': shift_mu[l], 'conv_w': conv_w[l], 'conv_b': conv_b[l], 'conv_ln_g': conv_ln_g[l],
             'conv_ln_b': conv_ln_b[l], 'w_conv_o': w_conv_o[l], 'b_conv_o': b_conv_o[l], 'w0': w0[l],
             'w2': w2[l], 'a0': a0[l], 'a2': a2[l], 'g2': g2[l], 'k_k': k_k[l], 'k_a': k_a[l], 'r_k': r_k[l],
             'lnx_g': lnx_g[l], 'lnx_b': lnx_b[l], 'w_rwkv_o': w_rwkv_o[l], 'w_out': w_out[l], 'b_out': b_out[l],
             'w_router': w_router[l], 'b_router': b_router[l], 'w_gate_up': w_gate_up[l],
             'b_gate_up': b_gate_up[l], 'w_down': w_down[l], 'b_down': b_down[l]}
        mod_l = jnp.split(jax.nn.silu(c) @ w_ada[l] + b_ada[l], 6, axis=-1)
        mod_c = jnp.split(jax.nn.silu(c_ctx) @ w_ada[l] + b_ada[l], 6, axis=-1)

        z_l = _modulate(x, mod_l[0], mod_l[1]) @ w_in[l] + b_in[l]
        z_c = _modulate(h_ctx, mod_c[0], mod_c[1]) @ w_in[l] + b_in[l]
        zc_l, zr_l, zg_l = _split_proj(z_l)
        zc_c, zr_c, zg_c = _split_proj(z_c)

        r_c, v_c, kk_c, g_c, dirs_c = _rwkv_prepare(zr_c, p)
        r_l, v_l, kk_l, g_l, dirs_l = _rwkv_prepare(zr_l, p)
        state0 = jnp.zeros((B, N_HEADS_RWKV, HEAD_SIZE, HEAD_SIZE), jnp.float32)
        y_l = 0.0
        y_c = 0.0
        for d in range(N_DIR):
            rev = d == 1
            dec_c, kd_c, a_c = dirs_c[d]
            s_ctx, yc_d = _wkv_scan(state0, r_c, dec_c, kd_c, v_c, -kk_c, kk_c * a_c, rev)
            dec_l, kd_l, a_l = dirs_l[d]
            _, yl_d = _wkv_scan(s_ctx, r_l, dec_l, kd_l, v_l, -kk_l, kk_l * a_l, rev)
            y_l = y_l + yl_d
            y_c = y_c + yc_d
        out_l = _merge(_conv_branch(zc_l, p, rows), _rwkv_out(y_l, r_l, v_l, dirs_l, g_l, p), zg_l, p)
        x_mid = _post_norm(x, out_l, mod_l[2], ln1_g[l], ln1_b[l])

        x = _post_norm(x_mid, _moe(_modulate(x_mid, mod_l[3], mod_l[4]), p), mod_l[5], ln2_g[l], ln2_b[l])

        if need_ctx:
            out_c = _merge(_conv_branch(zc_c, p, None), _rwkv_out(y_c, r_c, v_c, dirs_c, g_c, p), zg_c, p)
            c_mid = _post_norm(h_ctx, out_c, mod_c[2], ln1_g[l], ln1_b[l])
            h_ctx = _post_norm(c_mid, _moe(_modulate(c_mid, mod_c[3], mod_c[4]), p), mod_c[5], ln2_g[l], ln2_b[l])
    return x
```

```python
from contextlib import ExitStack
import numpy as np
import concourse.bass as bass
import concourse.mybir as mybir
from concourse.bass_utils import run_bass_kernel_spmd

F32 = mybir.dt.float32
BF16 = mybir.dt.bfloat16
AF = mybir.ActivationFunctionType
ALU = mybir.AluOpType
AX = mybir.AxisListType

D = 2048
T = 2048
TC = 256
NT = T // 128
NTC = TC // 128
TT = T + TC
P_IN = 9632
LN_EPS = 1e-5
ALPHA = 2.0 ** 0.25


class Tok:
    __slots__ = ("w", "r", "name", "excl")

    def __init__(self, name="", excl=False):
        self.w = {}
        self.r = {}
        self.name = name
        self.excl = excl


class Sched:
    EPOCH = 30000
    DMA_EPOCH = 1800

    def __init__(self, nc):
        self.nc = nc
        self.eng = {"pe": nc.tensor, "act": nc.scalar, "dve": nc.vector, "pool": nc.gpsimd, "sp": nc.sync}
        self.sem = {}
        self.cnt = {}
        self.seen = {e: {} for e in self.eng}
        self.nsem = 0
        self.dsem = {}
        self.dma_issued = {}
        self.ninst = 0
        for e in self.eng:
            self._new_sem(e)

    def _new_sem(self, e):
        self.sem[e] = self.nc.alloc_semaphore(name="se_%s_%d" % (e, self.nsem))
        self.nsem += 1
        self.cnt[e] = 0

    def _wait(self, e, deps):
        seen = self.seen[e]
        best = {}
        own = self.sem[e].num
        for (semh, val) in deps:
            k = semh.num
            if e == "pe" and k == own:
                continue
            if k in self.dma_issued:
                val = self.dma_issued[k]
            if seen.get(k, 0) >= val:
                continue
            if k not in best or best[k][1] < val:
                best[k] = (semh, val)
        for k, (semh, val) in best.items():
            self.eng[e].wait_ge(semh, val)
            seen[k] = val
            self.ninst += 1

    def _deps(self, reads, writes):
        deps = []
        for t in reads:
            deps.extend(t.w.values())
        for t in writes:
            deps.extend(t.w.values())
            deps.extend(t.r.values())
        return deps

    def pe_rg(self, rg):
        self.next_rg = rg

    def op(self, e, fn, reads=(), writes=()):
        if e == "pe":
            rg = getattr(self, "next_rg", 0)
            self.next_rg = 0
            if rg != getattr(self, "last_rg", 0) and self.cnt["pe"] > 0:
                self.eng["pe"].wait_ge(self.sem["pe"], self.cnt["pe"])
                self.ninst += 1
            self.last_rg = rg
        ex = [t for t in reads if t.excl]
        if ex:
            reads = [t for t in reads if not t.excl]
            writes = list(writes) + ex
        self._wait(e, self._deps(reads, writes))
        if self.cnt[e] >= self.EPOCH:
            self._new_sem(e)
        inst = fn(self.eng[e])
        self.cnt[e] += 1
        self.ninst += 1
        inst.then_inc(self.sem[e], 1)
        rec = (self.sem[e], self.cnt[e])
        k = self.sem[e].num
        for t in reads:
            t.r[k] = rec
        for t in writes:
            t.w[k] = rec
        return inst

    def dma(self, q, sname, out, in_, reads=(), writes=(), **kw):
        self._wait(q, self._deps(reads, writes))
        ent = self.dsem.get(sname)
        if ent is None or self.dma_issued[ent.num] >= 16 * self.DMA_EPOCH:
            ent = self.nc.alloc_semaphore(name="sd_%s_%d" % (sname, self.nsem))
            self.nsem += 1
            self.dsem[sname] = ent
            self.dma_issued[ent.num] = 0
        inst = self.eng[q].dma_start(out=out, in_=in_, **kw)
        self.ninst += 1
        self.dma_issued[ent.num] += 16
        inst.then_inc(ent, 16)
        rec = (ent, self.dma_issued[ent.num])
        for t in reads:
            t.r[ent.num] = rec
        for t in writes:
            t.w[ent.num] = rec
        return inst

    def barrier(self):
        for e in self.eng:
            deps = [(self.sem[o], self.cnt[o]) for o in self.eng if o != e and self.cnt[o] > 0]
            for k, ent in self.dsem.items():
                deps.append((ent, self.dma_issued[ent.num]))
            self._wait(e, deps)

    def finish(self, toks):
        deps = []
        for t in toks:
            deps.extend(t.w.values())
        self._wait("sp", deps)


class Ctx:
    pass


def build_program(debug=None):
    nc = bass.Bass("TRN2", target_bir_lowering=False)
    S = Sched(nc)
    g = Ctx()
    g.nc = nc
    g.S = S
    g.debug = debug
    if debug is not None and debug.startswith("rwkv1"):
        g.nheads = 1
        g.rw_stop = int(debug[5:6] or 9)
        g.no_bonus = debug.endswith("nb")
        debug = "rwkv"

    def dram_in(name, shape, dt=F32):
        return nc.dram_tensor(name, list(shape), dt, kind="ExternalInput").ap()

    def dram_out(name, shape, dt=F32):
        return nc.dram_tensor(name, list(shape), dt, kind="ExternalOutput").ap()

    g.x = dram_in("x", [T, D])
    g.ctx = dram_in("ctx", [TC, D])
    g.cc = dram_in("cc", [128, 16, 2])
    g.w_ada = dram_in("w_ada", [D, 6 * D])
    g.b_ada = dram_in("b_ada", [128, 96])
    g.w_in = dram_in("w_in", [D, P_IN])
    g.pv_in = dram_in("pv", [128, NPV])
    g.g2 = dram_in("g2", [160, 1024])
    g.w2bd = dram_in("w2bd", [16, 128, 128])
    g.a2bd = dram_in("a2bd", [16, 128, 128])
    g.lnx_g = dram_in("lnx_g", [16, 64])
    g.lnx_b = dram_in("lnx_b", [16, 64])
    g.w_conv_o = dram_in("w_conv_o", [1024, D])
    g.w_rwkv_o = dram_in("w_rwkv_o", [1024, D])
    g.w_out = dram_in("w_out", [D, D])
    g.ln1_g = dram_in("ln1_g", [1, D])
    g.ln1_b = dram_in("ln1_b", [1, D])
    g.ln2_g = dram_in("ln2_g", [1, D])
    g.ln2_b = dram_in("ln2_b", [1, D])
    g.w_router = dram_in("w_router", [128, 16, 32])
    g.b_router = dram_in("b_router", [1, 32])
    g.w_gu = dram_in("w_gu", [32, D, 2 * D])
    g.b_gu = dram_in("b_gu", [128, 1024])
    g.w_dn = dram_in("w_dn", [32, D, D])
    g.b_dn = dram_in("b_dn", [32, D])

    g.ident = nc.alloc_sbuf_tensor("ident", [128, 128], F32)
    g.anti = nc.alloc_sbuf_tensor("anti", [128, 128], F32)
    g.eps_ln = nc.alloc_sbuf_tensor("eps_ln", [128, 1], F32)
    g.t_const = Tok()
    S.op("pool", lambda e: e.memset(g.eps_ln[:], LN_EPS), writes=[g.t_const])
    g.t_ident = Tok()
    make_identity(g, g.ident, g.anti, g.t_ident)
    g.pv = nc.alloc_sbuf_tensor("pv_sb", [128, NPV], F32)
    g.t_pv = Tok()
    S.dma("sp", "ld_small", g.pv[:], g.pv_in, writes=[g.t_pv])
    phase_mod(g)
    phase_ln1(g, False)
    if debug == "ln1":
        o = dram_out("dbg_xmT", [128, 16, TT], BF16)

        tk = Tok()
        S.dma("sp", "dbg", o, g.xmT[:], reads=[g.t_xmT], writes=[tk])
        o2 = dram_out("dbg_mod", [128, 96, 2], F32)
        S.dma("sp", "dbg", o2, g.mod[:], reads=[g.t_mod], writes=[tk])
        S.finish([tk])
        return nc
    phase_proj(g, False)
    phase_ln1(g, True)
    phase_proj(g, True)
    g.es1.close()
    if debug == "proj":
        tk = Tok()
        for nm, src, tok in (("RKV", g.RKV, g.t_RKV), ("SGs", g.SGs, g.t_SGs), ("AGs", g.AGs, g.t_AGs),
                             ("Gs", g.Gs, g.t_Gs), ("ZC", g.ZC, g.t_ZC), ("SGZ", g.SGZ, g.t_SGZ)):
            o = dram_out("dbg_" + nm, list(src.shape), F32)
            S.dma("sp", "dbg", o, src, reads=[tok], writes=[tk])
        S.finish([tk])
        return nc
    phase_rwkv(g)
    if debug is None or debug == "full1":
        phase_mix(g)
        phase_out1(g)
        g.out_toks = []
        phase_moe(g, dram_out("out", [T, D], F32))
        S.finish(g.out_toks)
        return nc
    if debug == "rwkv":
        tk = Tok()
        o = dram_out("dbg_ORW", [1024, T], F32)
        S.dma("sp", "dbg", o, g.ORW, reads=[g.t_ORW], writes=[tk])
        for nm, buf in g.rw_dbg.items():
            o = dram_out("dbg_" + nm, list(buf.shape), F32)
            S.dma("sp", "dbg", o, buf, reads=[], writes=[tk])
        S.finish([tk])
        return nc
    return nc


def phase_mod(g):
    nc, S = g.nc, g.S
    g.mod = nc.alloc_sbuf_tensor("mod", [128, 96, 2], F32)
    g.t_mod = Tok("mod")
    g.modp = nc.alloc_sbuf_tensor("modp", [128, 96, 2], F32)
    sc = nc.alloc_sbuf_tensor("sc", [128, 16, 2], F32)
    t_sc = Tok()
    bada = nc.alloc_sbuf_tensor("bada", [128, 96], F32)
    t_b = Tok()
    S.dma("sp", "ld_small", sc[:], g.cc, writes=[t_sc])
    S.dma("sp", "ld_small", bada[:], g.b_ada, writes=[t_b])
    S.op("act", lambda e: e.activation(out=sc[:], in_=sc[:], func=AF.Silu), reads=[t_sc], writes=[t_sc])
    NG = 24
    wsrc = g.w_ada.rearrange("(kc p) n -> p kc n", p=128)
    with nc.sbuf_tensor("wada0", [128, 16, 512], F32) as wb0, nc.sbuf_tensor("wada1", [128, 16, 512], F32) as wb1, \
            nc.psum_tensor("ps_mod", [128, 96, 2], F32) as ps:
        wb = [wb0, wb1]
        t_wb = [Tok(), Tok()]
        t_ps = Tok(excl=True)
        for gi in range(NG):
            b = gi % 2
            S.dma("sp" if gi % 2 == 0 else "pool", "ld_wada%d" % b, wb[b][:], wsrc[:, :, gi * 512:(gi + 1) * 512],
                  writes=[t_wb[b]])
            for j in range(4):
                fo = gi * 4 + j
                for kc in range(16):
                    S.op("pe", lambda e, kc=kc, j=j, fo=fo, b=b: e.matmul(
                        ps[:, fo, :], lhsT=wb[b][:, kc, j * 128:(j + 1) * 128], rhs=sc[:, kc, :],
                        start=(kc == 0), stop=(kc == 15)),
                         reads=[t_wb[b], t_sc], writes=[t_ps])
        for i in range(2):
            S.op("dve", lambda e, i=i: e.tensor_tensor(out=g.mod[:, :, i], in0=ps[:, :, i], in1=bada[:], op=ALU.add),
                 reads=[t_ps, t_b], writes=[g.t_mod])
    S.op("dve", lambda e: e.tensor_scalar(out=g.modp[:], in0=g.mod[:], scalar1=1.0, scalar2=None, op0=ALU.add),
         reads=[g.t_mod], writes=[g.t_mod])
    S.barrier()


def phase_ln1(g, rev_pass):
    nc, S = g.nc, g.S
    if not rev_pass:
        g.es1 = ExitStack()
        g.xmT = g.es1.enter_context(nc.sbuf_tensor("xmT", [128, 16, TT], BF16))
        g.t_xmT = Tok("xmT")
    sfx = "r" if rev_pass else "f"
    with ExitStack() as es:
        sb = lambda name, shape, dt=F32: es.enter_context(nc.sbuf_tensor(name + sfx, shape, dt))
        psb = lambda name, shape, dt=F32: es.enter_context(nc.psum_tensor(name + sfx, shape, dt))
        xt = [sb("xt0", [128, D]), sb("xt1", [128, D])]
        st, mv = sb("ln_st", [128, 4, 6]), sb("ln_mv", [128, 4])
        pst = [psb("ps_tr%d" % i, [128, 4, 128]) for i in range(4)]
        t_xt = [Tok(), Tok()]
        t_st = Tok()
        t_pst = [Tok(excl=True) for _ in range(4)]
        for tt in range(NT + NTC):
            b = tt % 2
            if tt < NT:
                src = g.x[tt * 128:(tt + 1) * 128, :]
                col = 0
                pos = TC + (NT - 1 - tt) * 128 if rev_pass else TC + tt * 128
            else:
                src = g.ctx[(tt - NT) * 128:(tt - NT + 1) * 128, :]
                col = 1
                pos = (NTC - 1 - (tt - NT)) * 128 if rev_pass else (tt - NT) * 128
            S.dma("sp", "ld_x%d" % b, xt[b][:], src, writes=[t_xt[b]])
            ln_rows(g, xt[b], t_xt[b], st, mv, t_st)
            for fc in range(16):
                pb = fc // 4
                if rev_pass:
                    S.op("pe", lambda e, fc=fc, pb=pb, b=b: e.matmul(
                        pst[pb][:, fc % 4, :], lhsT=xt[b][:, fc * 128:(fc + 1) * 128], rhs=g.anti[:],
                        start=True, stop=True), reads=[t_xt[b], g.t_ident], writes=[t_pst[pb]])
                else:
                    S.op("pe", lambda e, fc=fc, pb=pb, b=b: e.transpose(
                        pst[pb][:, fc % 4, :], xt[b][:, fc * 128:(fc + 1) * 128], g.ident[:]),
                         reads=[t_xt[b], g.t_ident], writes=[t_pst[pb]])
                if fc % 4 == 3:
                    for f2 in range(fc - 3, fc + 1):
                        S.op("act", lambda e, f2=f2, pb=pb, pos=pos, col=col: e.activation(
                            out=g.xmT[:, f2, pos:pos + 128], in_=pst[pb][:, f2 % 4, :], func=AF.Identity,
                            bias=g.mod[:, f2, col:col + 1], scale=g.modp[:, 16 + f2, col:col + 1]),
                             reads=[t_pst[pb], g.t_mod], writes=[g.t_xmT])
    S.barrier()


def ln_rows(g, xt, t_x, st, mv, t_st, n=4):
    S = g.S
    for q in range(n):
        S.op("dve", lambda e, q=q: e.bn_stats(out=st[:, q, :], in_=xt[:, q * 512:(q + 1) * 512]),
             reads=[t_x], writes=[t_st])
    S.op("dve", lambda e: e.bn_aggr(out=mv[:, 0:2], in_=st[:, 0:n, :].rearrange("p a b -> p (a b)")),
         reads=[t_st], writes=[t_st])
    S.op("act", lambda e: e.activation(out=mv[:, 3:4], in_=mv[:, 1:2], func=AF.Sqrt, bias=g.eps_ln[:, 0:1],
                                       scale=1.0), reads=[t_st, g.t_const], writes=[t_st])
    S.op("dve", lambda e: e.reciprocal(out=mv[:, 2:3], in_=mv[:, 3:4]), reads=[t_st], writes=[t_st])
    S.op("dve", lambda e: e.tensor_scalar(out=xt[:, 0:n * 512], in0=xt[:, 0:n * 512], scalar1=mv[:, 0:1],
                                          scalar2=mv[:, 2:3], op0=ALU.subtract, op1=ALU.mult),
         reads=[t_st, t_x], writes=[t_x])


def make_identity(g, ident, anti, tok):
    nc, S = g.nc, g.S
    S.op("pool", lambda e: e.memset(ident[:], 0.0), writes=[tok])
    S.op("pool", lambda e: e.memset(anti[:], 0.0), writes=[tok])
    S.op("pool", lambda e: e.affine_select(out=ident[:], in_=ident[:], pattern=[[-1, 128]],
                                           compare_op=ALU.not_equal, fill=1.0, base=0, channel_multiplier=1),
         reads=[tok], writes=[tok])
    S.op("pool", lambda e: e.affine_select(out=anti[:], in_=anti[:], pattern=[[1, 128]],
                                           compare_op=ALU.not_equal, fill=1.0, base=-127, channel_multiplier=1),
         reads=[tok], writes=[tok])


def pv_layout():
    names = []
    for q in range(3):
        for h in range(16):
            names += ["b_%d_%d" % (q, h), "cp_%d_%d" % (q, h), "cn_%d_%d" % (q, h)]
    for nm in ("wd", "ad", "gd1", "gd2"):
        names += ["b_" + nm, "cp_" + nm, "cn_" + nm]
    for h in range(16):
        names += ["w0_%d" % h, "a0_%d" % h, "kk_%d" % h, "ka_%d" % h, "rk_%d" % h]
    for c in range(8):
        names += ["cba_%d" % c, "cbg_%d" % c, "convb_%d" % c, "clng_%d" % c, "clnb_%d" % c]
        names += ["cw_%d_%d" % (c, j) for j in range(31)]
    for c in range(32):
        names += ["bzg_%d" % c]
    for c in range(16):
        names += ["bco_%d" % c, "bout_%d" % c]
    return {n: i for i, n in enumerate(names)}


PVL = pv_layout()
NPV = len(PVL)


def make_pv(inp):
    pv = np.zeros((128, NPV), np.float32)
    b_in = inp["b_in"][0]
    mu = inp["shift_mu"][0]

    def dup(v):
        return np.concatenate([v, v])
    for q in range(3):
        for h in range(16):
            c0 = 2048 + q * 1024 + h * 64
            zc = c0 - 2048
            pv[:, PVL["b_%d_%d" % (q, h)]] = dup(b_in[c0:c0 + 64])
            pv[:, PVL["cp_%d_%d" % (q, h)]] = np.concatenate([mu[0, zc:zc + 64], mu[1, zc:zc + 64]])
            pv[:, PVL["cn_%d_%d" % (q, h)]] = np.concatenate([mu[1, zc:zc + 64], mu[0, zc:zc + 64]])
    for nm, c0 in (("wd", 5120), ("ad", 5248)):
        zc = c0 - 2048
        pv[:, PVL["b_" + nm]] = b_in[c0:c0 + 128]
        pv[:, PVL["cp_" + nm]] = np.concatenate([mu[0, zc:zc + 64], mu[1, zc + 64:zc + 128]])
        pv[:, PVL["cn_" + nm]] = np.concatenate([mu[1, zc:zc + 64], mu[0, zc + 64:zc + 128]])
    pv[:, PVL["b_gd1"]] = b_in[5376:5504]
    pv[:, PVL["cp_gd1"]] = mu[0, 5376 - 2048:5504 - 2048]
    pv[:, PVL["cn_gd1"]] = mu[1, 5376 - 2048:5504 - 2048]
    pv[:32, PVL["b_gd2"]] = b_in[5504:5536]
    pv[:32, PVL["cp_gd2"]] = mu[0, 5504 - 2048:5536 - 2048]
    pv[:32, PVL["cn_gd2"]] = mu[1, 5504 - 2048:5536 - 2048]
    for h in range(16):
        sl = slice(h * 64, h * 64 + 64)
        pv[:, PVL["w0_%d" % h]] = np.concatenate([inp["w0"][0, 0, sl], inp["w0"][0, 1, sl]])
        pv[:, PVL["a0_%d" % h]] = np.concatenate([inp["a0"][0, 0, sl], inp["a0"][0, 1, sl]])
        pv[:, PVL["kk_%d" % h]] = dup(inp["k_k"][0, sl])
        pv[:, PVL["ka_%d" % h]] = dup(inp["k_a"][0, sl])
        pv[:, PVL["rk_%d" % h]] = dup(inp["r_k"][0, h])
    for c in range(8):
        sl = slice(c * 128, c * 128 + 128)
        pv[:, PVL["cba_%d" % c]] = b_in[sl]
        pv[:, PVL["cbg_%d" % c]] = b_in[1024 + c * 128:1024 + c * 128 + 128]
        pv[:, PVL["convb_%d" % c]] = inp["conv_b"][0, sl]
        pv[:, PVL["clng_%d" % c]] = inp["conv_ln_g"][0, sl]
        pv[:, PVL["clnb_%d" % c]] = inp["conv_ln_b"][0, sl]
        for j in range(31):
            pv[:, PVL["cw_%d_%d" % (c, j)]] = inp["conv_w"][0, j, sl]
    for c in range(32):
        pv[:, PVL["bzg_%d" % c]] = b_in[5536 + c * 128:5536 + c * 128 + 128]
    for c in range(16):
        pv[:, PVL["bco_%d" % c]] = inp["b_conv_o"][0, c * 128:c * 128 + 128]
        pv[:, PVL["bout_%d" % c]] = inp["b_out"][0, c * 128:c * 128 + 128]
    return pv


def pvc(g, name):
    i = PVL[name]
    return g.pv[:, i:i + 1]


TG_ALL = [(0, 512), (512, 512), (1024, 512), (1536, 512), (2048, 256)]
TG_LAT = [(TC + i * 512, 512) for i in range(4)]


def phase_proj(g, rev_pass):
    nc, S = g.nc, g.S
    if not rev_pass:
        dr = lambda name, shape, dt=F32: nc.dram_tensor(name, list(shape), dt, kind="Internal").ap()
        g.RKV = dr("s_rkv", [3, 16, 128, TT])
        g.SGs = dr("s_sg", [16, 128, TT])
        g.AGs = dr("s_ag", [16, 128, TT])
        g.Gs = dr("s_g", [T, 1024])
        g.ZC = dr("s_zc", [1024, T])
        g.SGZ = dr("s_sgz", [4096, T])
        g.t_RKV, g.t_SGs, g.t_AGs, g.t_Gs, g.t_ZC, g.t_SGZ = (Tok() for _ in range(6))
        g.wdt = g.es1.enter_context(nc.sbuf_tensor("wdt", [128, TT], F32))
        g.ads = g.es1.enter_context(nc.sbuf_tensor("ads", [128, TT], F32))
        g.t_wdt, g.t_ads = Tok(), Tok()
    sfx = "r" if rev_pass else "f"
    wsrc = g.w_in.rearrange("(kc p) n -> p kc n", p=128)
    with ExitStack() as es:
        sb = lambda name, shape, dt=F32: es.enter_context(nc.sbuf_tensor(name + sfx, shape, dt))
        psb = lambda name, shape, dt=F32: es.enter_context(nc.psum_tensor(name + sfx, shape, dt))
        wf = [sb("wf0", [128, 16, 128]), sb("wf1", [128, 16, 128])]
        wb = [sb("wb0", [128, 16, 128], BF16), sb("wb1", [128, 16, 128], BF16)]
        zraw = sb("zraw", [128, TT])
        zs = [sb("zs0", [128, TT]), sb("zs1", [128, TT])]
        c0t = sb("c0t", [128, 1])
        lw2 = sb("lw2", [128, 2, 128])
        pp = [psb("pp%d" % i, [128, 512]) for i in range(4)]
        t_wf = [Tok(), Tok()]
        t_wb = [Tok(), Tok()]
        t_pp = [Tok(excl=True) for _ in range(4)]
        t_zraw, t_c0 = Tok(), Tok()
        t_zs = [Tok(), Tok()]
        st = {"wi": 0, "pi": 0, "zi": 0}

        def load_w(col0, M, dupl=False):
            i = st["wi"] % 2
            st["wi"] += 1
            S.dma("sp", "ld_w%d" % i, wf[i][:, :, 0:M], wsrc[:, :, col0:col0 + M], writes=[t_wf[i]])
            S.op("pool", lambda e: e.tensor_copy(out=wb[i][:, :, 0:M], in_=wf[i][:, :, 0:M]),
                 reads=[t_wf[i]], writes=[t_wb[i]])
            if dupl:
                S.op("pool", lambda e: e.tensor_copy(out=wb[i][:, :, M:2 * M], in_=wf[i][:, :, 0:M]),
                     reads=[t_wf[i]], writes=[t_wb[i]])
            return i

        def mm_group(i, M, p0, n):
            pi = st["pi"] % 4
            st["pi"] += 1
            for kc in range(16):
                S.op("pe", lambda e, kc=kc: e.matmul(pp[pi][0:M, 0:n], lhsT=wb[i][:, kc, 0:M],
                                                      rhs=g.xmT[:, kc, p0:p0 + n], start=(kc == 0), stop=(kc == 15)),
                     reads=[t_wb[i], g.t_xmT], writes=[t_pp[pi]])
            return pi

        def shift(zin, t_in, zout, t_out, plo, phi, cp, cn, segs):
            ps_ = slice(plo, phi)
            S.op("dve", lambda e: e.tensor_scalar(out=c0t[ps_, :], in0=cp[ps_, :], scalar1=cn[ps_, :], scalar2=-1.0,
                                                  op0=ALU.add, op1=ALU.mult), reads=[g.t_pv], writes=[t_c0])
            S.op("dve", lambda e: e.tensor_scalar(out=c0t[ps_, :], in0=c0t[ps_, :], scalar1=1.0, scalar2=None,
                                                  op0=ALU.add), reads=[t_c0], writes=[t_c0])
            lo, hi = segs[0][0], segs[-1][1]
            S.op("dve", lambda e: e.tensor_scalar(out=zout[ps_, lo:hi], in0=zin[ps_, lo:hi], scalar1=c0t[ps_, :],
                                                  scalar2=None, op0=ALU.mult), reads=[t_in, t_c0], writes=[t_out])
            for (a, b) in segs:
                S.op("dve", lambda e, a=a, b=b: e.scalar_tensor_tensor(
                    out=zout[ps_, a + 1:b], in0=zin[ps_, a:b - 1], scalar=cp[ps_, :], in1=zout[ps_, a + 1:b],
                    op0=ALU.mult, op1=ALU.add), reads=[t_in, g.t_pv], writes=[t_out])
                S.op("dve", lambda e, a=a, b=b: e.scalar_tensor_tensor(
                    out=zout[ps_, a:b - 1], in0=zin[ps_, a + 1:b], scalar=cn[ps_, :], in1=zout[ps_, a:b - 1],
                    op0=ALU.mult, op1=ALU.add), reads=[t_in, g.t_pv], writes=[t_out])

        SEG2 = [(0, TC), (TC, TT)]

        def one_pass(d, col0, M, dupl, bias, cp, cn, zout, t_out):
            i = load_w(col0, M, dupl)
            lo, hi = d * 64, d * 64 + 64
            for (p0, n) in TG_ALL:
                pi = mm_group(i, 128, p0, n)
                S.op("act", lambda e, pi=pi, p0=p0, n=n: e.activation(
                    out=zraw[lo:hi, p0:p0 + n], in_=pp[pi][lo:hi, 0:n], func=AF.Identity,
                    bias=bias[lo:hi, :], scale=1.0), reads=[t_pp[pi], g.t_pv], writes=[t_zraw])
            shift(zraw, t_zraw, zout, t_out, lo, hi, cp, cn, SEG2)

        def rkv_pass(d):
            lo, hi = d * 64, d * 64 + 64
            one_pass(d, 5120, 128, False, pvc(g, "b_wd"), pvc(g, "cp_wd"), pvc(g, "cn_wd"), g.wdt, g.t_wdt)
            S.op("act", lambda e: e.activation(out=g.wdt[lo:hi, :], in_=g.wdt[lo:hi, :], func=AF.Tanh),
                 reads=[g.t_wdt], writes=[g.t_wdt])
            one_pass(d, 5248, 128, False, pvc(g, "b_ad"), pvc(g, "cp_ad"), pvc(g, "cn_ad"), g.ads, g.t_ads)
            for q in range(3):
                for h in range(16):
                    zi = st["zi"] % 2
                    st["zi"] += 1
                    one_pass(d, 2048 + q * 1024 + h * 64, 64, True, pvc(g, "b_%d_%d" % (q, h)),
                             pvc(g, "cp_%d_%d" % (q, h)), pvc(g, "cn_%d_%d" % (q, h)), zs[zi], t_zs[zi])
                    S.dma("sp", "st_a%d" % zi, g.RKV[q, h, lo:hi, :], zs[zi][lo:hi, :], reads=[t_zs[zi]],
                          writes=[g.t_RKV])

        def lora_stage():
            t_lw = Tok()
            for h in range(16):
                S.dma("sp", "ld_small", lw2[:, 0, :], g.w2bd[h], writes=[t_lw])
                S.dma("sp", "ld_small", lw2[:, 1, :], g.a2bd[h], writes=[t_lw])
                for k, (src, t_src, bname, dstd, t_dst) in enumerate(
                        ((g.wdt, g.t_wdt, "w0_%d" % h, g.SGs, g.t_SGs), (g.ads, g.t_ads, "a0_%d" % h, g.AGs, g.t_AGs))):
                    zi = k
                    for (p0, n) in TG_ALL:
                        pi = st["pi"] % 4
                        st["pi"] += 1
                        S.op("pe", lambda e, k=k, pi=pi, p0=p0, n=n, src=src: e.matmul(
                            pp[pi][:, 0:n], lhsT=lw2[:, k, :], rhs=src[:, p0:p0 + n], start=True, stop=True),
                             reads=[t_lw, t_src], writes=[t_pp[pi]])
                        S.op("act", lambda e, pi=pi, p0=p0, n=n, zi=zi, bname=bname: e.activation(
                            out=zs[zi][:, p0:p0 + n], in_=pp[pi][:, 0:n], func=AF.Sigmoid, bias=pvc(g, bname),
                            scale=1.0), reads=[t_pp[pi], g.t_pv], writes=[t_zs[zi]])
                    S.dma("sp", "st_a%d" % zi, dstd[h], zs[zi][:], reads=[t_zs[zi]], writes=[t_dst])

        if rev_pass:
            rkv_pass(1)
            lora_stage()
        else:
            rkv_pass(0)
            sgd1, sgd2 = sb("sgd1", [128, T]), sb("sgd2", [32, T])
            g2a, g2b = sb("g2a", [128, 1024]), sb("g2b", [32, 1024])
            t_sgd, t_g2 = Tok(), Tok()
            for nm, col0, M, dst in (("gd1", 5376, 128, sgd1), ("gd2", 5504, 32, sgd2)):
                i = load_w(col0, M)
                for (p0, n) in TG_LAT:
                    pi = mm_group(i, M, p0, n)
                    S.op("act", lambda e, pi=pi, p0=p0, n=n, M=M, nm=nm: e.activation(
                        out=zraw[0:M, p0:p0 + n], in_=pp[pi][0:M, 0:n], func=AF.Identity,
                        bias=pvc(g, "b_" + nm)[0:M, :], scale=1.0), reads=[t_pp[pi], g.t_pv], writes=[t_zraw])
                shift(zraw, t_zraw, zs[0], t_zs[0], 0, M, pvc(g, "cp_" + nm), pvc(g, "cn_" + nm), [(TC, TT)])
                S.op("act", lambda e, M=M, dst=dst: e.activation(out=dst[0:M, :], in_=zs[0][0:M, TC:TT],
                                                                 func=AF.Sigmoid), reads=[t_zs[0]], writes=[t_sgd])
            S.dma("sp", "ld_small", g2a[:], g.g2[0:128, :], writes=[t_g2])
            S.dma("sp", "ld_small", g2b[:], g.g2[128:160, :], writes=[t_g2])
            for c in range(32):
                zi = c % 2
                for hf in range(2):
                    pi = st["pi"] % 4
                    st["pi"] += 1
                    S.op("pe", lambda e, c=c, hf=hf, pi=pi: e.matmul(
                        pp[pi][0:64, :], lhsT=sgd1[:, c * 64:(c + 1) * 64], rhs=g2a[:, hf * 512:(hf + 1) * 512],
                        start=True, stop=False), reads=[t_sgd, t_g2], writes=[t_pp[pi]])
                    S.op("pe", lambda e, c=c, hf=hf, pi=pi: e.matmul(
                        pp[pi][0:64, :], lhsT=sgd2[:, c * 64:(c + 1) * 64], rhs=g2b[:, hf * 512:(hf + 1) * 512],
                        start=False, stop=True), reads=[t_sgd, t_g2], writes=[t_pp[pi]])
                    S.op("dve", lambda e, hf=hf, pi=pi, zi=zi: e.tensor_copy(
                        out=zs[zi][0:64, hf * 512:(hf + 1) * 512], in_=pp[pi][0:64, :]),
                         reads=[t_pp[pi]], writes=[t_zs[zi]])
                S.dma("sp", "st_a%d" % zi, g.Gs[c * 64:(c + 1) * 64, :], zs[zi][0:64, 0:1024],
                      reads=[t_zs[zi]], writes=[g.t_Gs])
            for c in range(8):
                ia = load_w(c * 128, 128)
                ig = load_w(1024 + c * 128, 128)
                for ti, (p0, n) in enumerate(TG_LAT):
                    pa = mm_group(ia, 128, p0, n)
                    pg = mm_group(ig, 128, p0, n)
                    S.op("act", lambda e, pg=pg, ti=ti: e.activation(
                        out=zs[0][:, ti * 512:(ti + 1) * 512], in_=pp[pg][:, :], func=AF.Sigmoid,
                        bias=pvc(g, "cbg_%d" % c), scale=1.0), reads=[t_pp[pg], g.t_pv], writes=[t_zs[0]])
                    S.op("dve", lambda e, pa=pa, ti=ti: e.scalar_tensor_tensor(
                        out=zraw[:, ti * 512:(ti + 1) * 512], in0=pp[pa][:, :], scalar=pvc(g, "cba_%d" % c),
                        in1=zs[0][:, ti * 512:(ti + 1) * 512], op0=ALU.add, op1=ALU.mult),
                         reads=[t_pp[pa], t_zs[0], g.t_pv], writes=[t_zraw])
                hv = zraw[:, 0:T].rearrange("p (r w) -> p r w", w=64)
                ov = zs[1][:, 0:T].rearrange("p (r w) -> p r w", w=64)
                S.op("dve", lambda e: e.tensor_scalar(out=zs[1][:, 0:T], in0=zraw[:, 0:T],
                                                      scalar1=pvc(g, "cw_%d_15" % c), scalar2=pvc(g, "convb_%d" % c),
                                                      op0=ALU.mult, op1=ALU.add),
                     reads=[t_zraw, g.t_pv], writes=[t_zs[1]])
                for j in range(31):
                    o = j - 15
                    if o == 0:
                        continue
                    lo, hi = max(0, -o), min(64, 64 - o)
                    S.op("dve", lambda e, j=j, o=o, lo=lo, hi=hi: e.scalar_tensor_tensor(
                        out=ov[:, :, lo:hi], in0=hv[:, :, lo + o:hi + o], scalar=pvc(g, "cw_%d_%d" % (c, j)),
                        in1=ov[:, :, lo:hi], op0=ALU.mult, op1=ALU.add), reads=[t_zraw, g.t_pv], writes=[t_zs[1]])
                S.dma("sp", "st_a1", g.ZC[c * 128:(c + 1) * 128, :], zs[1][:, 0:T], reads=[t_zs[1]],
                      writes=[g.t_ZC])
            for c in range(32):
                i = load_w(5536 + c * 128, 128)
                zi = c % 2
                for ti, (p0, n) in enumerate(TG_LAT):
                    pi = mm_group(i, 128, p0, n)
                    S.op("act", lambda e, pi=pi, ti=ti, zi=zi: e.activation(
                        out=zs[zi][:, ti * 512:(ti + 1) * 512], in_=pp[pi][:, :], func=AF.Sigmoid,
                        bias=pvc(g, "bzg_%d" % c), scale=1.0), reads=[t_pp[pi], g.t_pv], writes=[t_zs[zi]])
                S.dma("sp", "st_a%d" % zi, g.SGZ[c * 128:(c + 1) * 128, :], zs[zi][:, 0:T],
                      reads=[t_zs[zi]], writes=[g.t_SGZ])
    S.barrier()


KAPPA = float(np.exp(-0.5))
GN_EPS = 64e-5
NCH = TT // 64


def phase_rwkv(g):
    nc, S = g.nc, g.S
    g.ORW = nc.dram_tensor("s_orw", [1024, T], F32, kind="Internal").ap()
    g.t_ORW = Tok()
    with ExitStack() as es:
        sb = lambda name, shape, dt=F32: es.enter_context(nc.sbuf_tensor("rw_" + name, shape, dt))
        psb = lambda name, shape, dt=F32: es.enter_context(nc.psum_tensor("rwp_" + name, shape, dt))
        R, Kt, Vt, SG, AG, KK, LI, TM = (sb(n, [128, TT]) for n in ("R", "Kt", "Vt", "SG", "AG", "KK", "LI", "TM"))
        QR, BK = sb("QR", [128, 2, TT]), sb("BK", [128, 2, TT])
        BKh = sb("BKh", [64, NCH, 2, 128])
        Vtm = sb("Vtm", [64, NCH, 128])
        Ys = sb("Ys", [64, 2, 32, 66])
        DC = sb("DC", [128, NCH])
        A_sb = sb("A", [64, 2, 320])
        W = [sb("W0", [64, 2, 3, 64]), sb("W1", [64, 2, 3, 64])]
        P1s, UTs = sb("P1s", [64, 2, 64]), sb("UTs", [64, 2, 64])
        Sst = sb("Sst", [128, 64])
        maskc = sb("maskc", [128, TT])
        maskA = sb("maskA", [64, 320])
        onesbd = sb("onesbd", [128, 128])
        onesel = sb("onesel", [128, 2])
        omka = sb("omka", [128, 1])
        LG, LB = sb("LG", [64, 64]), sb("LB", [64, 64])
        st1, st2 = sb("st1", [64, 32]), sb("st2", [64, 32])
        epsg = sb("epsg", [64, 1])
        banks = [psb("bk%d" % i, [128, 512]) for i in range(8)]
        PA = [banks[0][0:64, 0:320], banks[1][0:64, 0:320]]
        PS = banks[2][0:64, 0:384].rearrange("p (d w) -> p d w", w=192)
        PQ = banks[3][0:64, 0:256].rearrange("p (a w) -> p a w", w=64)
        PSn = banks[4][:, 0:128].rearrange("p (d w) -> p d w", w=64)
        PT = banks[5]
        PT2 = banks[6]
        PU = banks[7][0:64, 0:128].rearrange("p (a w) -> p a w", w=64)
        t = {n: Tok(n) for n in ("R", "Kt", "Vt", "SG", "AG", "KK", "LI", "TM", "QR", "BK", "BKh", "Vtm", "Ys", "DC",
                                 "A", "W0", "W1", "P1s", "UTs", "Sst", "c", "LG", "st", "G")}
        for i in range(8):
            t["B%d" % i] = Tok("B%d" % i, excl=True)
        t["PA0"], t["PA1"], t["PS"], t["PQ1"], t["PQ3"], t["PSn"], t["PT"], t["PT2"], t["PQ2"] = (
            t["B0"], t["B1"], t["B2"], t["B3"], t["B3"], t["B4"], t["B5"], t["B6"], t["B7"])
        S.op("pool", lambda e: e.memset(maskc[:], 1.0), writes=[t["c"]])
        S.op("pool", lambda e: e.memset(maskc[:].rearrange("p (c w) -> p c w", w=64)[:, :, 0:1], 0.0),
             reads=[t["c"]], writes=[t["c"]])
        S.op("pool", lambda e: e.memset(onesbd[:], 0.0), writes=[t["c"]])
        S.op("pool", lambda e: e.memset(onesbd[0:64, 0:64], 1.0), reads=[t["c"]], writes=[t["c"]])
        S.op("pool", lambda e: e.memset(onesbd[64:128, 64:128], 1.0), reads=[t["c"]], writes=[t["c"]])
        S.op("pool", lambda e: e.memset(onesel[:], 0.0), writes=[t["c"]])
        S.op("pool", lambda e: e.memset(onesel[0:64, 0:1], 1.0), reads=[t["c"]], writes=[t["c"]])
        S.op("pool", lambda e: e.memset(onesel[64:128, 1:2], 1.0), reads=[t["c"]], writes=[t["c"]])
        S.op("pool", lambda e: e.memset(epsg[:], GN_EPS), writes=[t["c"]])
        S.op("pool", lambda e: e.memset(maskA[:], 1.0), writes=[t["c"]])
        for blk, (cm, base, pat) in enumerate(((-1, -1, 1), (-1, 0, 1), (-1, -1, 1), (-1, 0, 1), (1, -1, -1))):
            S.op("pool", lambda e, blk=blk, cm=cm, base=base, pat=pat: e.affine_select(
                out=maskA[:, blk * 64:(blk + 1) * 64], in_=maskA[:, blk * 64:(blk + 1) * 64], pattern=[[pat, 64]],
                compare_op=ALU.is_ge, fill=0.0, base=base, channel_multiplier=cm), reads=[t["c"]], writes=[t["c"]])
        I64 = g.ident[0:64, 0:64]
        J64 = g.anti[0:64, 64:128]
        g.rw_dbg = {}
        for h in range(getattr(g, "nheads", 16)):
            for buf, nm, src, tk in ((R, "R", g.RKV[0, h], g.t_RKV), (Kt, "Kt", g.RKV[1, h], g.t_RKV),
                                     (Vt, "Vt", g.RKV[2, h], g.t_RKV), (SG, "SG", g.SGs[h], g.t_SGs),
                                     (AG, "AG", g.AGs[h], g.t_AGs)):
                S.dma("sp", "ld_rw_" + nm, buf[:], src, reads=[tk], writes=[t[nm]])
            S.dma("pool", "ld_rw_lg", LG[:], g.lnx_g[h:h + 1, :].partition_broadcast(64), writes=[t["LG"]])
            S.dma("pool", "ld_rw_lg", LB[:], g.lnx_b[h:h + 1, :].partition_broadcast(64), writes=[t["LG"]])
            kkc, kac, rkc = pvc(g, "kk_%d" % h), pvc(g, "ka_%d" % h), pvc(g, "rk_%d" % h)
            S.op("dve", lambda e: e.tensor_scalar(out=omka[:], in0=kac, scalar1=-1.0, scalar2=1.0, op0=ALU.mult,
                                                  op1=ALU.add), reads=[g.t_pv], writes=[t["c"]])
            S.op("dve", lambda e: e.tensor_scalar(out=TM[:], in0=Kt[:], scalar1=kkc, scalar2=None, op0=ALU.mult),
                 reads=[t["Kt"], g.t_pv], writes=[t["TM"]])
            S.op("act", lambda e: e.activation(out=KK[:], in_=TM[:], func=AF.Square), reads=[t["TM"]], writes=[t["KK"]])
            for i, (p0, n) in enumerate(TG_ALL):
                pt, tn = (PT, "PT") if i % 2 == 0 else (PT2, "PT2")
                S.op("pe", lambda e, pt=pt, p0=p0, n=n: e.matmul(pt[:, 0:n], lhsT=onesbd[:], rhs=KK[:, p0:p0 + n],
                                                                start=True, stop=True),
                     reads=[t["KK"], t["c"]], writes=[t[tn]])
                S.op("act", lambda e, pt=pt, p0=p0, n=n: e.activation(out=LI[:, p0:p0 + n], in_=pt[:, 0:n],
                                                                      func=AF.Sqrt), reads=[t[tn]], writes=[t["LI"]])
            S.op("dve", lambda e: e.tensor_scalar(out=LI[:], in0=LI[:], scalar1=1e-12, scalar2=None, op0=ALU.max),
                 reads=[t["LI"]], writes=[t["LI"]])
            S.op("dve", lambda e: e.reciprocal(out=LI[:], in_=LI[:]), reads=[t["LI"]], writes=[t["LI"]])
            S.op("dve", lambda e: e.tensor_tensor(out=KK[:], in0=TM[:], in1=LI[:], op=ALU.mult),
                 reads=[t["TM"], t["LI"]], writes=[t["KK"]])
            if getattr(g, "rw_stop", 9) <= 1:
                continue
            S.op("dve", lambda e: e.tensor_tensor_scan(out=LI[:], data0=maskc[:], data1=SG[:], initial=0.0,
                                                       op0=ALU.mult, op1=ALU.add),
                 reads=[t["SG"], t["c"], t["KK"]], writes=[t["LI"]])
            S.op("dve", lambda e: e.tensor_tensor(out=SG[:], in0=LI[:], in1=SG[:], op=ALU.subtract),
                 reads=[t["LI"]], writes=[t["SG"]])
            LIv = LI[:].rearrange("p (c w) -> p c w", w=64)
            S.op("act", lambda e: e.activation(out=DC[:], in_=LIv[:, :, 63], func=AF.Exp, scale=-KAPPA),
                 reads=[t["LI"]], writes=[t["DC"]])
            S.op("act", lambda e: e.activation(out=TM[:], in_=LI[:], func=AF.Exp, scale=-KAPPA),
                 reads=[t["LI"], t["KK"]], writes=[t["TM"]])
            S.op("dve", lambda e: e.tensor_tensor(out=QR[:, 1, :], in0=R[:], in1=TM[:], op=ALU.mult),
                 reads=[t["R"], t["TM"]], writes=[t["QR"]])
            S.op("act", lambda e: e.activation(out=TM[:], in_=SG[:], func=AF.Exp, scale=-KAPPA),
                 reads=[t["SG"], t["QR"]], writes=[t["TM"]])
            S.op("dve", lambda e: e.scalar_tensor_tensor(out=QR[:, 0, :], in0=KK[:], scalar=-1.0, in1=TM[:],
                                                         op0=ALU.mult, op1=ALU.mult),
                 reads=[t["KK"], t["TM"]], writes=[t["QR"]])
            S.op("act", lambda e: e.activation(out=TM[:], in_=LI[:], func=AF.Exp, scale=KAPPA),
                 reads=[t["LI"], t["QR"]], writes=[t["TM"]])
            S.op("dve", lambda e: e.tensor_scalar(out=SG[:], in0=AG[:], scalar1=kac, scalar2=omka[:, 0:1],
                                                  op0=ALU.mult, op1=ALU.add),
                 reads=[t["AG"], t["c"], g.t_pv, t["TM"]], writes=[t["SG"]])
            S.op("dve", lambda e: e.tensor_tensor(out=Kt[:], in0=Kt[:], in1=SG[:], op=ALU.mult),
                 reads=[t["SG"], t["TM"]], writes=[t["Kt"]])
            S.op("dve", lambda e: e.tensor_tensor(out=AG[:], in0=KK[:], in1=AG[:], op=ALU.mult),
                 reads=[t["KK"]], writes=[t["AG"]])
            S.op("dve", lambda e: e.tensor_tensor(out=BK[:, 0, :], in0=AG[:], in1=TM[:], op=ALU.mult),
                 reads=[t["AG"], t["TM"]], writes=[t["BK"]])
            S.op("dve", lambda e: e.tensor_tensor(out=BK[:, 1, :], in0=Kt[:], in1=TM[:], op=ALU.mult),
                 reads=[t["Kt"], t["TM"]], writes=[t["BK"]])
            S.op("dve", lambda e: e.scalar_tensor_tensor(out=R[:], in0=R[:], scalar=rkc, in1=Kt[:], op0=ALU.mult,
                                                         op1=ALU.mult), reads=[t["Kt"], t["QR"], g.t_pv], writes=[t["R"]])
            TMv = TM[:].rearrange("p (c w) -> p c w", w=64)
            S.op("dve", lambda e: e.tensor_tensor(out=TMv, in0=TMv, in1=DC[:].unsqueeze(2).to_broadcast([128, NCH, 64]),
                                                  op=ALU.mult), reads=[t["DC"], t["BK"]], writes=[t["TM"]])
            S.op("dve", lambda e: e.tensor_tensor(out=SG[:], in0=AG[:], in1=TM[:], op=ALU.mult),
                 reads=[t["AG"], t["TM"], t["Kt"]], writes=[t["SG"]])
            S.op("dve", lambda e: e.tensor_tensor(out=LI[:], in0=Kt[:], in1=TM[:], op=ALU.mult),
                 reads=[t["Kt"], t["TM"]], writes=[t["LI"]])
            if getattr(g, "rw_stop", 9) <= 2:
                continue
            import os
            _f3 = os.environ.get("RW3", "abcd")
            for c in range(int(os.environ.get("RW3N", NCH))):
                csl = slice(c * 64, (c + 1) * 64)
                pt, tn = (PT, "PT") if c % 2 == 0 else (PT2, "PT2")
                ptv = pt[0:64, 0:384].rearrange("p (a b) -> p a b", b=128)
                for a, (src, tk) in enumerate(((SG, "SG"), (LI, "LI"), (Vt, "Vt"))):
                    if "a" not in _f3:
                        continue
                    S.op("pe", lambda e, a=a, src=src, ptv=ptv, csl=csl: e.matmul(ptv[:, a, :], lhsT=src[:, csl], rhs=g.ident[:], start=True, stop=True),
                         reads=[t[tk], g.t_ident], writes=[t[tn]])
                if "b" in _f3:
                    S.op("act", lambda e, c=c, ptv=ptv: e.activation(out=BKh[:, c, :, :], in_=ptv[:, 0:2, :], func=AF.Identity),
                         reads=[t[tn]], writes=[t["BKh"]])
                if "c" in _f3:
                    S.op("dve", lambda e, c=c, ptv=ptv: e.tensor_copy(out=Vtm[:, c, :], in_=ptv[:, 2, :]),
                         reads=[t[tn]], writes=[t["Vtm"]])
                if c >= 4 and "d" in _f3:
                    S.op("pe", lambda e, c=c, pt=pt, csl=csl: e.matmul(pt[0:64, 384:386], lhsT=R[:, csl], rhs=onesel[:],
                                                                       start=True, stop=True),
                         reads=[t["R"], t["c"]], writes=[t[tn]])
                    S.op("dve", lambda e, c=c, pt=pt: e.tensor_copy(out=Ys[:, :, c - 4, 64], in_=pt[0:64, 384:386]),
                         reads=[t[tn]], writes=[t["Ys"]])
            if getattr(g, "rw_stop", 9) <= 3:
                continue
            S.op("pool", lambda e: e.memset(Sst[:], 0.0), reads=[t["Sst"]], writes=[t["Sst"]])
            _f4 = os.environ.get("RW4", "AIS")
            for c in range(int(os.environ.get("RW4N", NCH))):
                csl = slice(c * 64, (c + 1) * 64)
                for d in range(2):
                    ds = slice(d * 64, d * 64 + 64)
                    pn = "PA%d" % d
                    S.pe_rg(d * 64)
                    S.op("pe", lambda e, d=d, ds=ds: e.matmul(PA[d][:, 0:128], lhsT=BK[ds, 0, csl], rhs=QR[ds, :, csl],
                                                              start=True, stop=True),
                         reads=[t["BK"], t["QR"]], writes=[t[pn]])
                    S.pe_rg(d * 64)
                    S.op("pe", lambda e, d=d, ds=ds: e.matmul(PA[d][:, 128:256], lhsT=BK[ds, 1, csl], rhs=QR[ds, :, csl],
                                                              start=True, stop=True),
                         reads=[t["BK"], t["QR"]], writes=[t[pn]])
                    S.pe_rg(d * 64)
                    S.op("pe", lambda e, d=d, ds=ds: e.matmul(PA[d][:, 256:320], lhsT=QR[ds, 0, csl], rhs=BK[ds, 0, csl],
                                                              start=True, stop=True),
                         reads=[t["BK"], t["QR"]], writes=[t[pn]])
                    S.op("dve", lambda e, d=d: e.tensor_tensor(out=A_sb[:, d, :], in0=PA[d][:, :], in1=maskA[:],
                                                               op=ALU.mult), reads=[t[pn], t["c"]], writes=[t["A"]])
                if "I" not in _f4:
                    continue
                Av = A_sb[:].rearrange("p d (b w) -> p d b w", w=64)
                S.op("act", lambda e: e.activation(out=W[0][:, :, 0, :], in_=Av[:, :, 0, :], func=AF.Identity),
                     reads=[t["A"]], writes=[t["W0"]])
                S.op("act", lambda e: e.activation(out=W[0][:, :, 2, :], in_=Av[:, :, 4, :], func=AF.Identity),
                     reads=[t["A"]], writes=[t["W0"]])
                S.op("dve", lambda e: e.tensor_copy(out=W[0][:, :, 1, :], in_=I64.unsqueeze(1).to_broadcast([64, 2, 64])),
                     reads=[g.t_ident], writes=[t["W0"]])
                PSv = PS.rearrange("p d (b w) -> p d b w", w=64)
                for s_ in range(6):
                    wc, wn = W[s_ % 2], W[(s_ + 1) % 2]
                    tc_, tn_ = t["W%d" % (s_ % 2)], t["W%d" % ((s_ + 1) % 2)]
                    for d in range(2):
                        S.op("pe", lambda e, d=d, wc=wc: e.matmul(PS[:, d, 0:128], lhsT=wc[:, d, 2, :], rhs=wc[:, d, 0:2, :],
                                                                  start=True, stop=True), reads=[tc_], writes=[t["PS"]])
                        if s_ < 5:
                            S.op("pe", lambda e, d=d, wc=wc: e.matmul(PS[:, d, 128:192], lhsT=wc[:, d, 0, :],
                                                                      rhs=wc[:, d, 2, :], start=True, stop=True),
                                 reads=[tc_], writes=[t["PS"]])
                    if s_ < 5:
                        for blk in (0, 2):
                            S.op("act", lambda e, wn=wn, blk=blk: e.activation(out=wn[:, :, blk, :], in_=PSv[:, :, blk, :],
                                                                               func=AF.Identity), reads=[t["PS"]], writes=[tn_])
                    S.op("dve", lambda e, wc=wc, wn=wn: e.tensor_tensor(out=wn[:, :, 1, :], in0=wc[:, :, 1, :],
                                                                        in1=PSv[:, :, 1, :], op=ALU.add),
                         reads=[t["PS"], tc_], writes=[tn_])
                Wf, tWf = W[0], t["W0"]
                if "S" not in _f4:
                    continue
                _s4 = int(os.environ.get("RW4S", 9))
                for d in range(2):
                    ds = slice(d * 64, d * 64 + 64)
                    S.pe_rg(d * 64)
                    S.op("pe", lambda e, d=d, ds=ds: e.matmul(PQ[:, d, :], lhsT=QR[ds, 0, csl], rhs=Sst[ds, :],
                                                              start=True, stop=True),
                         reads=[t["QR"], t["Sst"]], writes=[t["PQ1"]])
                    S.op("pe", lambda e, d=d, ds=ds: e.matmul(PQ[:, 2 + d, :], lhsT=A_sb[:, d, 128:192], rhs=Vtm[:, c, ds],
                                                              start=True, stop=True),
                         reads=[t["A"], t["Vtm"]], writes=[t["PQ1"]])
                S.op("act", lambda e: e.activation(out=P1s[:], in_=PQ[:, 0:2, :], func=AF.Identity),
                     reads=[t["PQ1"]], writes=[t["P1s"]])
                S.op("dve", lambda e: e.tensor_tensor(out=P1s[:], in0=P1s[:], in1=PQ[:, 2:4, :], op=ALU.add),
                     reads=[t["PQ1"]], writes=[t["P1s"]])
                if _s4 < 2:
                    continue
                for d in range(2):
                    S.op("pe", lambda e, d=d: e.matmul(PU[:, d, :], lhsT=Wf[:, d, 1, :], rhs=P1s[:, d, :],
                                                       start=True, stop=True), reads=[tWf, t["P1s"]], writes=[t["PQ2"]])
                S.op("dve", lambda e: e.tensor_copy(out=UTs[:], in_=PU[:, 0:2, :]), reads=[t["PQ2"]], writes=[t["UTs"]])
                if _s4 < 3:
                    continue
                for d in range(2):
                    ds = slice(d * 64, d * 64 + 64)
                    S.op("pe", lambda e, d=d: e.matmul(PSn[:, d, :], lhsT=BKh[:, c, 0, :], rhs=UTs[:, d, :],
                                                       start=True, stop=False), reads=[t["BKh"], t["UTs"]], writes=[t["PSn"]])
                    S.op("pe", lambda e, d=d, ds=ds: e.matmul(PSn[:, d, :], lhsT=BKh[:, c, 1, :], rhs=Vtm[:, c, ds],
                                                              start=False, stop=True),
                         reads=[t["BKh"], t["Vtm"]], writes=[t["PSn"]])
                if c >= 4 and _s4 >= 4:
                    for d in range(2):
                        ds = slice(d * 64, d * 64 + 64)
                        S.pe_rg(d * 64)
                        S.op("pe", lambda e, d=d, ds=ds: e.matmul(PQ[:, d, :], lhsT=QR[ds, 1, csl], rhs=Sst[ds, :],
                                                                  start=True, stop=True),
                             reads=[t["QR"], t["Sst"], t["P1s"]], writes=[t["PQ3"]])
                        S.op("pe", lambda e, d=d: e.matmul(PQ[:, 2 + d, :], lhsT=A_sb[:, d, 64:128], rhs=UTs[:, d, :],
                                                           start=True, stop=False), reads=[t["A"], t["UTs"]],
                             writes=[t["PQ3"]])
                        S.op("pe", lambda e, d=d, ds=ds: e.matmul(PQ[:, 2 + d, :], lhsT=A_sb[:, d, 192:256],
                                                                  rhs=Vtm[:, c, ds], start=False, stop=True),
                             reads=[t["A"], t["Vtm"]], writes=[t["PQ3"]])
                    S.op("act", lambda e, c=c: e.activation(out=Ys[:, :, c - 4, 0:64], in_=PQ[:, 0:2, :], func=AF.Identity),
                         reads=[t["PQ3"]], writes=[t["Ys"]])
                    S.op("dve", lambda e, c=c: e.tensor_tensor(out=Ys[:, :, c - 4, 0:64], in0=Ys[:, :, c - 4, 0:64],
                                                               in1=PQ[:, 2:4, :], op=ALU.add),
                         reads=[t["PQ3"]], writes=[t["Ys"]])
                for d in range(2):
                    ds = slice(d * 64, d * 64 + 64)
                    S.op("dve", lambda e, d=d, ds=ds, c=c: e.scalar_tensor_tensor(
                        out=Sst[ds, :], in0=Sst[ds, :], scalar=DC[ds, c:c + 1], in1=PSn[ds, d, :], op0=ALU.mult,
                        op1=ALU.add), reads=[t["PSn"], t["DC"], t["Sst"]], writes=[t["Sst"]])
            if getattr(g, "nheads", 16) == 1:
                g.rw_dbg = {"Ys": Ys[:], "QR": QR[:], "BK": BK[:], "BKh": BKh[:], "Vtm": Vtm[:], "DC": DC[:], "A": A_sb[:],
                            "Winv": W[0][:], "Sst": Sst[:]}
            if getattr(g, "rw_stop", 9) <= 4:
                continue
            Yt = TM[0:64, 0:32 * 66].rearrange("p (c w) -> p c w", w=66)
            Gt = KK[0:64, 0:2048].rearrange("p (c w) -> p c w", w=64)
            Yc = QR[0:64, 0, 0:2048].rearrange("p (c w) -> p c w", w=64)
            Y2 = QR[0:64, 1, 0:2048].rearrange("p (c w) -> p c w", w=64)
            S.dma("sp", "ld_rw_G", Gt, g.Gs[:, h * 64:(h + 1) * 64].rearrange("(c p) v -> p c v", p=64),
                  reads=[g.t_Gs, t["KK"]], writes=[t["KK"]])
            for c4 in range(8):
                pt, tn = (PT, "PT") if c4 % 2 == 0 else (PT2, "PT2")
                ptv = pt[0:64, 0:264].rearrange("p (a b) -> p a b", b=66)
                for a in range(4):
                    c = c4 * 4 + a
                    S.op("pe", lambda e, a=a, c=c, ptv=ptv: e.matmul(ptv[:, a, :], lhsT=I64, rhs=Ys[:, 0, c, :],
                                                                     start=True, stop=False),
                         reads=[t["Ys"], g.t_ident], writes=[t[tn]])
                    S.op("pe", lambda e, a=a, c=c, ptv=ptv: e.matmul(ptv[:, a, :], lhsT=J64, rhs=Ys[:, 1, 31 - c, :],
                                                                     start=False, stop=True),
                         reads=[t["Ys"], g.t_ident], writes=[t[tn]])
                S.op("act", lambda e, c4=c4, ptv=ptv: e.activation(out=Yt[:, c4 * 4:(c4 + 1) * 4, :], in_=ptv,
                                                                   func=AF.Identity), reads=[t[tn], t["LI"], t["SG"]],
                     writes=[t["TM"]])
            S.op("dve", lambda e: e.tensor_reduce(out=st1[:], in_=Yt[:, :, 0:64], axis=AX.X, op=ALU.add),
                 reads=[t["TM"]], writes=[t["st"]])
            S.op("dve", lambda e: e.tensor_scalar(out=st1[:], in0=st1[:], scalar1=1.0 / 64, scalar2=None, op0=ALU.mult),
                 reads=[t["st"]], writes=[t["st"]])
            S.op("dve", lambda e: e.tensor_tensor(out=Yc, in0=Yt[:, :, 0:64],
                                                  in1=st1[:].unsqueeze(2).to_broadcast([64, 32, 64]), op=ALU.subtract),
                 reads=[t["TM"], t["st"], t["BK"], t["Sst"]], writes=[t["QR"]])
            S.op("act", lambda e: e.activation(out=Y2, in_=Yc, func=AF.Square), reads=[t["QR"]], writes=[t["QR"]])
            S.op("dve", lambda e: e.tensor_reduce(out=st2[:], in_=Y2, axis=AX.X, op=ALU.add),
                 reads=[t["QR"]], writes=[t["st"]])
            S.op("act", lambda e: e.activation(out=st2[:], in_=st2[:], func=AF.Sqrt, bias=epsg[:, 0:1], scale=1.0 / 64),
                 reads=[t["st"], t["c"]], writes=[t["st"]])
            S.op("dve", lambda e: e.reciprocal(out=st2[:], in_=st2[:]), reads=[t["st"]], writes=[t["st"]])
            S.op("dve", lambda e: e.tensor_tensor(out=Yc, in0=Yc, in1=st2[:].unsqueeze(2).to_broadcast([64, 32, 64]),
                                                  op=ALU.mult), reads=[t["st"], t["QR"]], writes=[t["QR"]])
            S.op("dve", lambda e: e.tensor_tensor(out=Yc, in0=Yc, in1=LG[:].unsqueeze(1).to_broadcast([64, 32, 64]),
                                                  op=ALU.mult), reads=[t["LG"], t["QR"]], writes=[t["QR"]])
            S.op("dve", lambda e: e.tensor_tensor(out=Yc, in0=Yc, in1=LB[:].unsqueeze(1).to_broadcast([64, 32, 64]),
                                                  op=ALU.add), reads=[t["LG"], t["QR"]], writes=[t["QR"]])
            S.op("dve", lambda e: e.tensor_tensor(out=Y2, in0=Vtm[:, 4:36, 0:64],
                                                  in1=Yt[:, :, 64:65].to_broadcast([64, 32, 64]), op=ALU.mult),
                 reads=[t["Vtm"], t["TM"], t["QR"]], writes=[t["QR"]])
            S.op("dve", lambda e: e.tensor_tensor(out=Yc, in0=Yc, in1=Y2, op=ALU.add), reads=[t["QR"]], writes=[t["QR"]])
            S.op("dve", lambda e: e.tensor_tensor(out=Yc, in0=Yc, in1=Gt, op=ALU.mult), reads=[t["QR"], t["KK"]],
                 writes=[t["QR"]])
            Of = BK[0:64, 0, 0:2048]
            for c8 in range(4):
                pt, tn = (PT, "PT") if c8 % 2 == 0 else (PT2, "PT2")
                for a in range(8):
                    c = c8 * 8 + a
                    S.op("pe", lambda e, a=a, c=c, pt=pt: e.matmul(pt[0:64, a * 64:(a + 1) * 64], lhsT=Yc[:, c, :], rhs=I64, start=True, stop=True),
                         reads=[t["QR"], g.t_ident], writes=[t[tn]])
                S.op("act", lambda e, c8=c8, pt=pt: e.activation(out=Of[:, c8 * 512:(c8 + 1) * 512], in_=pt[0:64, :],
                                                                 func=AF.Identity), reads=[t[tn], t["A"]], writes=[t["BK"]])
            S.dma("sp", "st_rw", g.ORW[h * 64:(h + 1) * 64, :], Of, reads=[t["BK"]], writes=[g.t_ORW, t["BK"]])
    S.barrier()


def bank_toks(n):
    return [Tok("bank%d" % i, excl=True) for i in range(n)]


def phase_mix(g):
    nc, S = g.nc, g.S
    g.es2 = ExitStack()
    g.mixT = g.es2.enter_context(nc.sbuf_tensor("mixT", [128, 16, T], BF16))
    g.t_mixT = Tok()
    with ExitStack() as es:
        sb = lambda name, shape, dt=F32: es.enter_context(nc.sbuf_tensor("mx_" + name, shape, dt))
        HN, ORWb = sb("HN", [128, 8, T], BF16), sb("ORWb", [128, 8, T], BF16)
        ZCt, SQ = sb("ZCt", [128, 8, 512]), sb("SQ", [128, 8, 512])
        mu, rs, tmp = sb("mu", [128, 512]), sb("rs", [128, 512]), sb("tmp", [128, 512])
        onesM = sb("onesM", [128, 128])
        wf = [sb("wf0", [128, 8, 128]), sb("wf1", [128, 8, 128])]
        wb = [sb("wb0", [128, 8, 128], BF16), sb("wb1", [128, 8, 128], BF16)]
        sz = [sb("sz0", [128, 512]), sb("sz1", [128, 512])]
        t1, t2 = sb("t1", [128, 512]), sb("t2", [128, 512])
        banks = [es.enter_context(nc.psum_tensor("mx_bk%d" % i, [128, 512], F32)) for i in range(4)]
        tb = bank_toks(4)
        tk = {n: Tok(n) for n in ("HN", "ORWb", "ZCt", "SQ", "mu", "rs", "tmp", "c", "wf0", "wf1", "wb0", "wb1",
                                  "sz0", "sz1", "t1", "t2")}
        S.op("pool", lambda e: e.memset(onesM[:], 1.0 / 1024), writes=[tk["c"]])
        zcv = g.ZC.rearrange("(c p) t -> p c t", p=128)
        for tg in range(4):
            tsl = slice(tg * 512, (tg + 1) * 512)
            S.dma("sp", "mx_ld", ZCt[:], zcv[:, :, tsl], reads=[g.t_ZC], writes=[tk["ZCt"]])
            S.op("act", lambda e: e.activation(out=SQ[:], in_=ZCt[:], func=AF.Square), reads=[tk["ZCt"]], writes=[tk["SQ"]])
            for cc in range(8):
                S.op("pe", lambda e, cc=cc: e.matmul(banks[0][:, :], lhsT=onesM[:], rhs=ZCt[:, cc, :], start=(cc == 0),
                                                    stop=(cc == 7)), reads=[tk["ZCt"], tk["c"]], writes=[tb[0]])
            for cc in range(8):
                S.op("pe", lambda e, cc=cc: e.matmul(banks[1][:, :], lhsT=onesM[:], rhs=SQ[:, cc, :], start=(cc == 0),
                                                    stop=(cc == 7)), reads=[tk["SQ"], tk["c"]], writes=[tb[1]])
            S.op("act", lambda e: e.activation(out=mu[:], in_=banks[0][:, :], func=AF.Identity), reads=[tb[0]],
                 writes=[tk["mu"]])
            S.op("dve", lambda e: e.tensor_tensor(out=tmp[:], in0=mu[:], in1=mu[:], op=ALU.mult), reads=[tk["mu"]],
                 writes=[tk["tmp"]])
            S.op("dve", lambda e: e.tensor_tensor(out=rs[:], in0=banks[1][:, :], in1=tmp[:], op=ALU.subtract),
                 reads=[tb[1], tk["tmp"]], writes=[tk["rs"]])
            S.op("act", lambda e: e.activation(out=rs[:], in_=rs[:], func=AF.Sqrt, bias=g.eps_ln[:, 0:1], scale=1.0),
                 reads=[tk["rs"], g.t_const], writes=[tk["rs"]])
            S.op("dve", lambda e: e.reciprocal(out=rs[:], in_=rs[:]), reads=[tk["rs"]], writes=[tk["rs"]])
            for cc in range(8):
                S.op("dve", lambda e, cc=cc: e.tensor_tensor(out=tmp[:], in0=ZCt[:, cc, :], in1=mu[:], op=ALU.subtract),
                     reads=[tk["ZCt"], tk["mu"]], writes=[tk["tmp"]])
                S.op("dve", lambda e: e.tensor_tensor(out=tmp[:], in0=tmp[:], in1=rs[:], op=ALU.mult),
                     reads=[tk["rs"]], writes=[tk["tmp"]])
                S.op("act", lambda e, cc=cc, tsl=tsl: e.activation(out=HN[:, cc, tsl], in_=tmp[:], func=AF.Silu,
                                                                   bias=pvc(g, "clnb_%d" % cc), scale=pvc(g, "clng_%d" % cc)),
                     reads=[tk["tmp"], g.t_pv], writes=[tk["HN"]])
        for kc in range(8):
            for tg in range(4):
                i = (kc * 4 + tg) % 2
                tsl = slice(tg * 512, (tg + 1) * 512)
                S.dma("sp", "mx_ld%d" % i, sz[i][:], g.ORW[kc * 128:(kc + 1) * 128, tsl], reads=[g.t_ORW],
                      writes=[tk["sz%d" % i]])
                S.op("pool", lambda e, i=i, kc=kc, tsl=tsl: e.tensor_copy(out=ORWb[:, kc, tsl], in_=sz[i][:]),
                     reads=[tk["sz%d" % i]], writes=[tk["ORWb"]])
        wcv = g.w_conv_o.rearrange("(kc p) n -> p kc n", p=128)
        wrv = g.w_rwkv_o.rearrange("(kc p) n -> p kc n", p=128)
        for f in range(16):
            fsl = slice(f * 128, (f + 1) * 128)
            for i, wv in enumerate((wcv, wrv)):
                S.dma("sp", "mx_w%d" % i, wf[i][:], wv[:, :, fsl], writes=[tk["wf%d" % i]])
                S.op("pool", lambda e, i=i: e.tensor_copy(out=wb[i][:], in_=wf[i][:]), reads=[tk["wf%d" % i]],
                     writes=[tk["wb%d" % i]])
            for tg in range(4):
                tsl = slice(tg * 512, (tg + 1) * 512)
                ba, bb = (0, 1) if tg % 2 == 0 else (2, 3)
                for kc in range(8):
                    S.op("pe", lambda e, kc=kc, ba=ba, tsl=tsl: e.matmul(banks[ba][:, :], lhsT=wb[0][:, kc, :],
                                                                        rhs=HN[:, kc, tsl], start=(kc == 0), stop=(kc == 7)),
                         reads=[tk["wb0"], tk["HN"]], writes=[tb[ba]])
                for kc in range(8):
                    S.op("pe", lambda e, kc=kc, bb=bb, tsl=tsl: e.matmul(banks[bb][:, :], lhsT=wb[1][:, kc, :],
                                                                        rhs=ORWb[:, kc, tsl], start=(kc == 0), stop=(kc == 7)),
                         reads=[tk["wb1"], tk["ORWb"]], writes=[tb[bb]])
                S.dma("sp", "mx_sz0", sz[0][:], g.SGZ[f * 128:(f + 1) * 128, tsl], reads=[g.t_SGZ], writes=[tk["sz0"]])
                S.dma("sp", "mx_sz1", sz[1][:], g.SGZ[2048 + f * 128:2048 + (f + 1) * 128, tsl], reads=[g.t_SGZ],
                      writes=[tk["sz1"]])
                S.op("dve", lambda e, ba=ba: e.scalar_tensor_tensor(out=t1[:], in0=banks[ba][:, :], scalar=pvc(g, "bco_%d" % f),
                                                                    in1=sz[0][:], op0=ALU.add, op1=ALU.mult),
                     reads=[tb[ba], tk["sz0"], g.t_pv], writes=[tk["t1"]])
                S.op("dve", lambda e, bb=bb: e.tensor_tensor(out=t2[:], in0=banks[bb][:, :], in1=sz[1][:], op=ALU.mult),
                     reads=[tb[bb], tk["sz1"]], writes=[tk["t2"]])
                S.op("pool", lambda e, f=f, tsl=tsl: e.tensor_tensor(out=g.mixT[:, f, tsl], in0=t1[:], in1=t2[:], op=ALU.add),
                     reads=[tk["t1"], tk["t2"]], writes=[g.t_mixT])
    S.barrier()


def phase_out1(g):
    nc, S = g.nc, g.S
    dr = lambda name, shape, dt=F32: nc.dram_tensor(name, list(shape), dt, kind="Internal").ap()
    g.XMID = dr("s_xmid", [T, D])
    g.UT = dr("s_ut", [128, 16, T], BF16)
    g.CT = dr("s_ct", [32, T])
    g.t_XMID, g.t_UT, g.t_CT = Tok(), Tok(), Tok()
    with ExitStack() as es:
        sb = lambda name, shape, dt=F32: es.enter_context(nc.sbuf_tensor("o1_" + name, shape, dt))
        OF = sb("OF", [128, 16, 512])
        wf = [sb("wf0", [128, 16, 128]), sb("wf1", [128, 16, 128])]
        wb = [sb("wb0", [128, 16, 128], BF16), sb("wb1", [128, 16, 128], BF16)]
        xt, un = sb("xt", [128, D]), sb("un", [128, D])
        G1, B1 = sb("G1", [128, D]), sb("B1", [128, D])
        st, mv = sb("st", [128, 4, 6]), sb("mv", [128, 4])
        uTb, uTf = sb("uTb", [128, 16, 128], BF16), sb("uTf", [128, 16, 128])
        WR, BR = sb("WR", [128, 16, 32]), sb("BR", [128, 32])
        lg, ex, m8, sm = sb("lg", [128, 32]), sb("ex", [128, 32]), sb("m8", [128, 8]), sb("sm", [128, 4])
        cTt = sb("cTt", [32, 128])
        banks = [es.enter_context(nc.psum_tensor("o1_bk%d" % i, [128, 512], F32)) for i in range(7)]
        tb = bank_toks(7)
        tk = {n: Tok(n) for n in ("OF", "wf0", "wf1", "wb0", "wb1", "xt", "un", "GB", "st", "uTb", "uTf", "WR", "lg",
                                  "cTt")}
        S.dma("pool", "o1_c", G1[:], g.ln1_g[0:1, :].partition_broadcast(128), writes=[tk["GB"]])
        S.dma("pool", "o1_c", B1[:], g.ln1_b[0:1, :].partition_broadcast(128), writes=[tk["GB"]])
        S.dma("pool", "o1_c", BR[:], g.b_router[0:1, :].partition_broadcast(128), writes=[tk["WR"]])
        S.dma("sp", "o1_c2", WR[:], g.w_router, writes=[tk["WR"]])
        wov = g.w_out.rearrange("(kc p) n -> p kc n", p=128)
        for tg in range(4):
            tsl = slice(tg * 512, (tg + 1) * 512)
            for f in range(16):
                i = f % 2
                S.dma("sp", "o1_w%d" % i, wf[i][:], wov[:, :, f * 128:(f + 1) * 128], writes=[tk["wf%d" % i]])
                S.op("pool", lambda e, i=i: e.tensor_copy(out=wb[i][:], in_=wf[i][:]), reads=[tk["wf%d" % i]],
                     writes=[tk["wb%d" % i]])
                bk = 4 + i
                for kc in range(16):
                    S.op("pe", lambda e, kc=kc, i=i, bk=bk: e.matmul(banks[bk][:, :], lhsT=wb[i][:, kc, :],
                                                                    rhs=g.mixT[:, kc, tsl], start=(kc == 0), stop=(kc == 15)),
                         reads=[tk["wb%d" % i], g.t_mixT], writes=[tb[bk]])
                S.op("dve", lambda e, f=f, bk=bk: e.tensor_scalar(out=OF[:, f, :], in0=banks[bk][:, :],
                                                                  scalar1=pvc(g, "bout_%d" % f), scalar2=g.mod[:, 32 + f, 0:1],
                                                                  op0=ALU.add, op1=ALU.mult),
                     reads=[tb[bk], g.t_pv, g.t_mod], writes=[tk["OF"]])
            for j in range(4):
                tok0 = tg * 512 + j * 128
                S.dma("sp", "o1_x", xt[:], g.x[tok0:tok0 + 128, :], writes=[tk["xt"]])
                for f in range(16):
                    S.op("pe", lambda e, f=f, j=j: e.transpose(
                        banks[f // 4][:, (f % 4) * 128:(f % 4 + 1) * 128], OF[:, f, j * 128:(j + 1) * 128], g.ident[:]),
                         reads=[tk["OF"], g.t_ident], writes=[tb[f // 4]])
                for q in range(4):
                    S.op("dve", lambda e, q=q: e.scalar_tensor_tensor(
                        out=xt[:, q * 512:(q + 1) * 512], in0=xt[:, q * 512:(q + 1) * 512], scalar=ALPHA,
                        in1=banks[q][:, :], op0=ALU.mult, op1=ALU.add), reads=[tb[q]], writes=[tk["xt"]])
                ln_rows(g, xt, tk["xt"], st, mv, tk["st"])
                S.op("dve", lambda e: e.tensor_tensor(out=xt[:], in0=xt[:], in1=G1[:], op=ALU.mult),
                     reads=[tk["GB"]], writes=[tk["xt"]])
                S.op("pool", lambda e: e.tensor_tensor(out=xt[:], in0=xt[:], in1=B1[:], op=ALU.add),
                     reads=[tk["GB"]], writes=[tk["xt"]])
                S.dma("sp", "o1_xm", g.XMID[tok0:tok0 + 128, :], xt[:], reads=[tk["xt"]], writes=[g.t_XMID])
                S.op("pool", lambda e: e.tensor_copy(out=un[:], in_=xt[:]), reads=[tk["xt"]], writes=[tk["un"]])
                ln_rows(g, un, tk["un"], st, mv, tk["st"])
                for f in range(16):
                    S.op("pe", lambda e, f=f: e.transpose(banks[f // 4][:, (f % 4) * 128:(f % 4 + 1) * 128],
                                                          un[:, f * 128:(f + 1) * 128], g.ident[:]),
                         reads=[tk["un"], g.t_ident], writes=[tb[f // 4]])
                for f in range(16):
                    src = banks[f // 4][:, (f % 4) * 128:(f % 4 + 1) * 128]
                    S.op("act", lambda e, f=f, src=src: e.activation(out=uTb[:, f, :], in_=src, func=AF.Identity,
                                                                     bias=g.mod[:, 48 + f, 0:1], scale=g.modp[:, 64 + f, 0:1]),
                         reads=[tb[f // 4], g.t_mod], writes=[tk["uTb"]])
                    S.op("act", lambda e, f=f, src=src: e.activation(out=uTf[:, f, :], in_=src, func=AF.Identity,
                                                                     bias=g.mod[:, 48 + f, 0:1], scale=g.modp[:, 64 + f, 0:1]),
                         reads=[tb[f // 4], g.t_mod], writes=[tk["uTf"]])
                S.dma("sp", "o1_ut", g.UT[:, :, tok0:tok0 + 128], uTb[:], reads=[tk["uTb"]], writes=[g.t_UT])
                for kc in range(16):
                    S.op("pe", lambda e, kc=kc: e.matmul(banks[6][:, 0:32], lhsT=uTf[:, kc, :], rhs=WR[:, kc, :],
                                                        start=(kc == 0), stop=(kc == 15)),
                         reads=[tk["uTf"], tk["WR"]], writes=[tb[6]])
                S.op("dve", lambda e: e.tensor_tensor(out=lg[:], in0=banks[6][:, 0:32], in1=BR[:], op=ALU.add),
                     reads=[tb[6], tk["WR"]], writes=[tk["lg"]])
                S.op("dve", lambda e: e.max(out=m8[:], in_=lg[:]), reads=[tk["lg"]], writes=[tk["lg"]])
                S.op("dve", lambda e: e.tensor_scalar(out=sm[:, 0:1], in0=m8[:, 0:1], scalar1=-1.0, scalar2=None,
                                                      op0=ALU.mult), reads=[tk["lg"]], writes=[tk["lg"]])
                S.op("act", lambda e: e.activation(out=ex[:], in_=lg[:], func=AF.Exp, bias=sm[:, 0:1], scale=1.0),
                     reads=[tk["lg"]], writes=[tk["lg"]])
                S.op("dve", lambda e: e.tensor_scalar(out=lg[:], in0=lg[:], scalar1=m8[:, 3:4], scalar2=None,
                                                      op0=ALU.is_ge), reads=[tk["lg"]], writes=[tk["lg"]])
                S.op("dve", lambda e: e.tensor_tensor(out=ex[:], in0=ex[:], in1=lg[:], op=ALU.mult),
                     reads=[tk["lg"]], writes=[tk["lg"]])
                S.op("dve", lambda e: e.tensor_reduce(out=sm[:, 1:2], in_=ex[:], axis=AX.X, op=ALU.add),
                     reads=[tk["lg"]], writes=[tk["lg"]])
                S.op("dve", lambda e: e.reciprocal(out=sm[:, 2:3], in_=sm[:, 1:2]), reads=[tk["lg"]], writes=[tk["lg"]])
                S.op("dve", lambda e: e.tensor_scalar(out=ex[:], in0=ex[:], scalar1=sm[:, 2:3], scalar2=None,
                                                      op0=ALU.mult), reads=[tk["lg"]], writes=[tk["lg"]])
                S.op("pe", lambda e: e.matmul(banks[6][0:32, 128:256], lhsT=ex[:], rhs=g.ident[:], start=True, stop=True),
                     reads=[tk["lg"], g.t_ident], writes=[tb[6]])
                S.op("act", lambda e: e.activation(out=cTt[:], in_=banks[6][0:32, 128:256], func=AF.Identity),
                     reads=[tb[6]], writes=[tk["cTt"]])
                S.dma("sp", "o1_ct", g.CT[:, tok0:tok0 + 128], cTt[:], reads=[tk["cTt"]], writes=[g.t_CT])
    g.es2.close()
    S.barrier()


def phase_moe(g, out):
    nc, S = g.nc, g.S
    HB = 1024
    for hb in range(2):
        with ExitStack() as es0:
            acc = es0.enter_context(nc.sbuf_tensor("me_acc%d" % hb, [128, 16, HB], F32))
            t_acc = Tok()
            with ExitStack() as es:
                sb = lambda name, shape, dt=F32: es.enter_context(nc.sbuf_tensor("me%d_" % hb + name, shape, dt))
                uTh, hidT = sb("uTh", [128, 16, HB], BF16), sb("hidT", [128, 16, HB], BF16)
                cT, rhe, cb = sb("cT", [32, HB]), sb("rhe", [32, HB]), sb("cb", [128, HB])
                ones32 = sb("ones32", [32, 128])
                wf = [sb("wf0", [128, 16, 128]), sb("wf1", [128, 16, 128])]
                wb = [sb("wb%d" % i, [128, 16, 128], BF16) for i in range(4)]
                t1, t2, t3 = sb("t1", [128, 512]), sb("t2", [128, 512]), sb("t3", [128, 512])
                BGU = sb("BGU", [128, 1024])
                bd = sb("bd", [32, 128])
                banks = [es.enter_context(nc.psum_tensor("me%d_bk%d" % (hb, i), [128, 512], F32)) for i in range(8)]
                tb = bank_toks(8)
                tk = {n: Tok(n) for n in ("uTh", "hidT", "cT", "rhe", "cb", "c", "wf0", "wf1", "wb0", "wb1", "wb2", "wb3",
                                          "t1", "t2", "t3", "BGU", "bd")}
                hsl = slice(hb * HB, (hb + 1) * HB)
                S.dma("sp", "me_ld", uTh[:], g.UT[:, :, hsl], reads=[g.t_UT], writes=[tk["uTh"]])
                S.dma("sp", "me_ld", cT[:], g.CT[:, hsl], reads=[g.t_CT], writes=[tk["cT"]])
                S.dma("sp", "me_ld", BGU[:], g.b_gu, writes=[tk["BGU"]])
                S.op("pool", lambda e: e.memset(ones32[:], 1.0), writes=[tk["c"]])
                st = {"w": 0, "pb": 0}
                for ex_ in range(32):
                    S.op("dve", lambda e, ex_=ex_: e.tensor_scalar(out=rhe[:], in0=cT[:], scalar1=g.ident[0:32, ex_:ex_ + 1],
                                                                   scalar2=None, op0=ALU.mult),
                         reads=[tk["cT"], g.t_ident], writes=[tk["rhe"]])
                    for tg in range(2):
                        S.op("pe", lambda e, tg=tg: e.matmul(banks[6 + tg][:, :], lhsT=ones32[:],
                                                            rhs=rhe[:, tg * 512:(tg + 1) * 512], start=True, stop=True),
                             reads=[tk["rhe"], tk["c"]], writes=[tb[6 + tg]])
                        S.op("act", lambda e, tg=tg: e.activation(out=cb[:, tg * 512:(tg + 1) * 512], in_=banks[6 + tg][:, :],
                                                                  func=AF.Identity), reads=[tb[6 + tg]], writes=[tk["cb"]])
                    wgv = g.w_gu[ex_].rearrange("(kc p) n -> p kc n", p=128)
                    wdv = g.w_dn[ex_].rearrange("(kc p) n -> p kc n", p=128)
                    for j in range(16):
                        pair = (st["w"] % 2) * 2
                        st["w"] += 1
                        for i, c0 in enumerate((j * 128, 2048 + j * 128)):
                            S.dma("sp", "me_w%d" % i, wf[i][:], wgv[:, :, c0:c0 + 128], writes=[tk["wf%d" % i]])
                            S.op("pool", lambda e, i=i, pair=pair: e.tensor_copy(out=wb[pair + i][:], in_=wf[i][:]),
                                 reads=[tk["wf%d" % i]], writes=[tk["wb%d" % (pair + i)]])
                        for tg in range(2):
                            tsl = slice(tg * 512, (tg + 1) * 512)
                            pb = (st["pb"] % 2) * 2
                            st["pb"] += 1
                            for i in range(2):
                                for kc in range(16):
                                    S.op("pe", lambda e, kc=kc, i=i, pb=pb, pair=pair, tsl=tsl: e.matmul(
                                        banks[pb + i][:, :], lhsT=wb[pair + i][:, kc, :], rhs=uTh[:, kc, tsl],
                                        start=(kc == 0), stop=(kc == 15)),
                                         reads=[tk["wb%d" % (pair + i)], tk["uTh"]], writes=[tb[pb + i]])
                            bgc = BGU[:, ex_ * 32 + j:ex_ * 32 + j + 1]
                            buc = BGU[:, ex_ * 32 + 16 + j:ex_ * 32 + 16 + j + 1]
                            S.op("dve", lambda e, pb=pb, bgc=bgc: e.tensor_scalar(out=t1[:], in0=banks[pb][:, :], scalar1=bgc,
                                                                                  scalar2=7.0, op0=ALU.add, op1=ALU.min),
                                 reads=[tb[pb], tk["BGU"]], writes=[tk["t1"]])
                            S.op("act", lambda e: e.activation(out=t2[:], in_=t1[:], func=AF.Sigmoid, scale=1.702),
                                 reads=[tk["t1"]], writes=[tk["t2"]])
                            S.op("dve", lambda e, pb=pb, buc=buc: e.tensor_scalar(out=t3[:], in0=banks[pb + 1][:, :],
                                                                                  scalar1=buc, scalar2=-7.0, op0=ALU.add,
                                                                                  op1=ALU.max),
                                 reads=[tb[pb + 1], tk["BGU"]], writes=[tk["t3"]])
                            S.op("dve", lambda e: e.tensor_scalar(out=t3[:], in0=t3[:], scalar1=7.0, scalar2=1.0, op0=ALU.min,
                                                                  op1=ALU.add), reads=[tk["t3"]], writes=[tk["t3"]])
                            S.op("pool", lambda e: e.tensor_tensor(out=t1[:], in0=t1[:], in1=t2[:], op=ALU.mult),
                                 reads=[tk["t2"]], writes=[tk["t1"]])
                            S.op("pool", lambda e: e.tensor_tensor(out=t1[:], in0=t1[:], in1=t3[:], op=ALU.mult),
                                 reads=[tk["t3"]], writes=[tk["t1"]])
                            S.op("pool", lambda e, j=j, tsl=tsl: e.tensor_tensor(out=hidT[:, j, tsl], in0=t1[:], in1=cb[:, tsl],
                                                                                 op=ALU.mult),
                                 reads=[tk["t1"], tk["cb"]], writes=[tk["hidT"]])
                    for f in range(16):
                        wi = st["w"] % 4
                        st["w"] += 1
                        i = f % 2
                        S.dma("sp", "me_w%d" % i, wf[i][:], wdv[:, :, f * 128:(f + 1) * 128], writes=[tk["wf%d" % i]])
                        S.op("pool", lambda e, i=i, wi=wi: e.tensor_copy(out=wb[wi][:], in_=wf[i][:]),
                             reads=[tk["wf%d" % i]], writes=[tk["wb%d" % wi]])
                        if ex_ == 0:
                            S.dma("sp", "me_bd", bd[:], g.b_dn[:, f * 128:(f + 1) * 128], writes=[tk["bd"]])
                        for tg in range(2):
                            tsl = slice(tg * 512, (tg + 1) * 512)
                            bk = 4 + (f * 2 + tg) % 2
                            for kc in range(16):
                                S.op("pe", lambda e, kc=kc, bk=bk, wi=wi, tsl=tsl: e.matmul(
                                    banks[bk][:, :], lhsT=wb[wi][:, kc, :], rhs=hidT[:, kc, tsl], start=(kc == 0),
                                    stop=(kc == 15)), reads=[tk["wb%d" % wi], tk["hidT"]], writes=[tb[bk]])
                            if ex_ == 0:
                                S.op("pe", lambda e, tg=tg, tsl=tsl: e.matmul(banks[6 + tg][:, :], lhsT=bd[:], rhs=cT[:, tsl],
                                                                              start=True, stop=True),
                                     reads=[tk["bd"], tk["cT"]], writes=[tb[6 + tg]])
                                S.op("act", lambda e, f=f, tg=tg, tsl=tsl: e.activation(out=acc[:, f, tsl], in_=banks[6 + tg][:, :],
                                                                                      func=AF.Identity),
                                     reads=[tb[6 + tg]], writes=[t_acc])
                            S.op("dve", lambda e, f=f, bk=bk, tsl=tsl: e.tensor_tensor(out=acc[:, f, tsl], in0=banks[bk][:, :],
                                                                                      in1=acc[:, f, tsl], op=ALU.add),
                                 reads=[tb[bk]], writes=[t_acc])
            S.barrier()
            with ExitStack() as es:
                sb = lambda name, shape, dt=F32: es.enter_context(nc.sbuf_tensor("fin%d_" % hb + name, shape, dt))
                xt = [sb("xt0", [128, D]), sb("xt1", [128, D])]
                G2, B2 = sb("G2", [128, D]), sb("B2", [128, D])
                st_, mv = sb("st", [128, 4, 6]), sb("mv", [128, 4])
                banks = [es.enter_context(nc.psum_tensor("fin%d_bk%d" % (hb, i), [128, 512], F32)) for i in range(4)]
                tb = bank_toks(4)
                tk = {n: Tok(n) for n in ("xt0", "xt1", "GB", "st")}
                t_out = Tok()
                S.dma("pool", "fin_c", G2[:], g.ln2_g[0:1, :].partition_broadcast(128), writes=[tk["GB"]])
                S.dma("pool", "fin_c", B2[:], g.ln2_b[0:1, :].partition_broadcast(128), writes=[tk["GB"]])
                for f in range(16):
                    S.op("dve", lambda e, f=f: e.tensor_scalar(out=acc[:, f, :], in0=acc[:, f, :], scalar1=g.mod[:, 80 + f, 0:1],
                                                               scalar2=None, op0=ALU.mult), reads=[g.t_mod], writes=[t_acc])
                for j in range(HB // 128):
                    b = j % 2
                    tok0 = hb * HB + j * 128
                    S.dma("sp", "fin_x%d" % b, xt[b][:], g.XMID[tok0:tok0 + 128, :], reads=[g.t_XMID], writes=[tk["xt%d" % b]])
                    for f in range(16):
                        S.op("pe", lambda e, f=f, j=j: e.transpose(
                            banks[f // 4][:, (f % 4) * 128:(f % 4 + 1) * 128], acc[:, f, j * 128:(j + 1) * 128], g.ident[:]),
                             reads=[t_acc, g.t_ident], writes=[tb[f // 4]])
                    for q in range(4):
                        S.op("dve", lambda e, q=q, b=b: e.scalar_tensor_tensor(
                            out=xt[b][:, q * 512:(q + 1) * 512], in0=xt[b][:, q * 512:(q + 1) * 512], scalar=ALPHA,
                            in1=banks[q][:, :], op0=ALU.mult, op1=ALU.add), reads=[tb[q]], writes=[tk["xt%d" % b]])
                    ln_rows(g, xt[b], tk["xt%d" % b], st_, mv, tk["st"])
                    S.op("dve", lambda e, b=b: e.tensor_tensor(out=xt[b][:], in0=xt[b][:], in1=G2[:], op=ALU.mult),
                         reads=[tk["GB"]], writes=[tk["xt%d" % b]])
                    S.op("pool", lambda e, b=b: e.tensor_tensor(out=xt[b][:], in0=xt[b][:], in1=B2[:], op=ALU.add),
                         reads=[tk["GB"]], writes=[tk["xt%d" % b]])
                    S.dma("sp", "fin_o%d" % b, out[tok0:tok0 + 128, :], xt[b][:], reads=[tk["xt%d" % b]],
                          writes=[t_out, tk["xt%d" % b]])
                g.out_toks.append(t_out)
            S.barrier()


def phase_placeholder_out(g, out):
    nc, S = g.nc, g.S
    with ExitStack() as es:
        bufs = [es.enter_context(nc.sbuf_tensor("po%d" % i, [128, D], F32)) for i in range(2)]
        toks = [Tok(), Tok()]
        t_out = Tok()
        for tt in range(NT):
            b = tt % 2
            S.dma("sp", "po_ld%d" % b, bufs[b][:], g.x[tt * 128:(tt + 1) * 128, :], writes=[toks[b]])
            S.dma("sp", "po_st%d" % b, out[tt * 128:(tt + 1) * 128, :], bufs[b][:], reads=[toks[b]], writes=[t_out])
        S.finish([t_out])


def make_in_maps(inp, ncores=8):
    maps = []
    for b in range(ncores):
        m = {}
        m["x"] = np.ascontiguousarray(inp["x"][b])
        m["ctx"] = np.ascontiguousarray(inp["ctx"][b])
        cc = np.stack([inp["c"][b], inp["c_ctx"]], axis=-1)
        m["cc"] = np.ascontiguousarray(cc.reshape(16, 128, 2).transpose(1, 0, 2))
        m["w_ada"] = np.ascontiguousarray(inp["w_ada"][0])
        m["b_ada"] = np.ascontiguousarray(inp["b_ada"][0].reshape(96, 128).T)
        m["w_in"] = np.ascontiguousarray(inp["w_in"][0])
        m["pv"] = make_pv(inp)
        m["g2"] = np.ascontiguousarray(inp["g2"][0])
        m["lnx_g"] = np.ascontiguousarray(inp["lnx_g"][0].reshape(16, 64))
        m["lnx_b"] = np.ascontiguousarray(inp["lnx_b"][0].reshape(16, 64))
        m["w_conv_o"] = np.ascontiguousarray(inp["w_conv_o"][0])
        m["w_rwkv_o"] = np.ascontiguousarray(inp["w_rwkv_o"][0])
        m["w_out"] = np.ascontiguousarray(inp["w_out"][0])
        for nm in ("ln1_g", "ln1_b", "ln2_g", "ln2_b", "b_router"):
            m[nm] = np.ascontiguousarray(inp[nm][0][None, :])
        m["w_router"] = np.ascontiguousarray(inp["w_router"][0].reshape(16, 128, 32).transpose(1, 0, 2))
        m["w_gu"] = np.ascontiguousarray(inp["w_gate_up"][0])
        m["b_gu"] = np.ascontiguousarray(inp["b_gate_up"][0].reshape(32, 32, 128).transpose(2, 0, 1).reshape(128, 1024))
        m["w_dn"] = np.ascontiguousarray(inp["w_down"][0])
        m["b_dn"] = np.ascontiguousarray(inp["b_down"][0])
        for nm, key in (("w2bd", "w2"), ("a2bd", "a2")):
            bd = np.zeros((16, 128, 128), np.float32)
            for h in range(16):
                for d in range(2):
                    bd[h, d * 64:(d + 1) * 64, d * 64:(d + 1) * 64] = inp[key][0, d, :, h * 64:(h + 1) * 64]
            m[nm] = bd
        maps.append(m)
    return maps


def kernel(**inputs):
    inp = {k: np.asarray(v) for k, v in inputs.items()}
    nc = build_program()
    in_maps = make_in_maps(inp)
    res = run_bass_kernel_spmd(nc, in_maps, core_ids=list(range(8)))
    return np.stack([r["out"] for r in res.results], axis=0)
```

```python
from contextlib import ExitStack
import numpy as np
import concourse.bass as bass
import concourse.mybir as mybir
from concourse.bass_utils import run_bass_kernel_spmd

F32 = mybir.dt.float32
BF16 = mybir.dt.bfloat16
AF = mybir.ActivationFunctionType
ALU = mybir.AluOpType
AX = mybir.AxisListType

D = 2048
T = 2048
TC = 256
NT = T // 128
NTC = TC // 128
TT = T + TC
P_IN = 9632
LN_EPS = 1e-5
ALPHA = 2.0 ** 0.25


class Tok:
    __slots__ = ("w", "r", "name", "excl")

    def __init__(self, name="", excl=False):
        self.w = {}
        self.r = {}
        self.name = name
        self.excl = excl


class Sched:
    EPOCH = 30000
    DMA_EPOCH = 1800

    def __init__(self, nc):
        self.nc = nc
        self.eng = {"pe": nc.tensor, "act": nc.scalar, "dve": nc.vector, "pool": nc.gpsimd, "sp": nc.sync}
        self.sem = {}
        self.cnt = {}
        self.seen = {e: {} for e in self.eng}
        self.nsem = 0
        self.dsem = {}
        self.dma_issued = {}
        self.ninst = 0
        for e in self.eng:
            self._new_sem(e)

    def _new_sem(self, e):
        self.sem[e] = self.nc.alloc_semaphore(name="se_%s_%d" % (e, self.nsem))
        self.nsem += 1
        self.cnt[e] = 0

    def _wait(self, e, deps):
        seen = self.seen[e]
        best = {}
        own = self.sem[e].num
        for (semh, val) in deps:
            k = semh.num
            if e == "pe" and k == own:
                continue
            if k in self.dma_issued:
                val = self.dma_issued[k]
            if seen.get(k, 0) >= val:
                continue
            if k not in best or best[k][1] < val:
                best[k] = (semh, val)
        for k, (semh, val) in best.items():
            self.eng[e].wait_ge(semh, val)
            seen[k] = val
            self.ninst += 1

    def _deps(self, reads, writes):
        deps = []
        for t in reads:
            deps.extend(t.w.values())
        for t in writes:
            deps.extend(t.w.values())
            deps.extend(t.r.values())
        return deps

    def pe_rg(self, rg):
        self.next_rg = rg

    def op(self, e, fn, reads=(), writes=()):
        if e == "pe":
            rg = getattr(self, "next_rg", 0)
            self.next_rg = 0
            if rg != getattr(self, "last_rg", 0) and self.cnt["pe"] > 0:
                self.eng["pe"].wait_ge(self.sem["pe"], self.cnt["pe"])
                self.ninst += 1
            self.last_rg = rg
        ex = [t for t in reads if t.excl]
        if ex:
            reads = [t for t in reads if not t.excl]
            writes = list(writes) + ex
        self._wait(e, self._deps(reads, writes))
        if self.cnt[e] >= self.EPOCH:
            self._new_sem(e)
        inst = fn(self.eng[e])
        self.cnt[e] += 1
        self.ninst += 1
        inst.then_inc(self.sem[e], 1)
        rec = (self.sem[e], self.cnt[e])
        k = self.sem[e].num
        for t in reads:
            t.r[k] = rec
        for t in writes:
            t.w[k] = rec
        return inst

    def dma(self, q, sname, out, in_, reads=(), writes=(), **kw):
        self._wait(q, self._deps(reads, writes))
        ent = self.dsem.get(sname)
        if ent is None or self.dma_issued[ent.num] >= 16 * self.DMA_EPOCH:
            ent = self.nc.alloc_semaphore(name="sd_%s_%d" % (sname, self.nsem))
            self.nsem += 1
            self.dsem[sname] = ent
            self.dma_issued[ent.num] = 0
        inst = self.eng[q].dma_start(out=out, in_=in_, **kw)
        self.ninst += 1
        self.dma_issued[ent.num] += 16
        inst.then_inc(ent, 16)
        rec = (ent, self.dma_issued[ent.num])
        for t in reads:
            t.r[ent.num] = rec
        for t in writes:
            t.w[ent.num] = rec
        return inst

    def barrier(self):
        for e in self.eng:
            deps = [(self.sem[o], self.cnt[o]) for o in self.eng if o != e and self.cnt[o] > 0]
            for k, ent in self.dsem.items():
                deps.append((ent, self.dma_issued[ent.num]))
            self._wait(e, deps)

    def finish(self, toks):
        deps = []
        for t in toks:
            deps.extend(t.w.values())
        self._wait("sp", deps)


class Ctx:
    pass


def build_program(debug=None):
    nc = bass.Bass("TRN2", target_bir_lowering=False)
    S = Sched(nc)
    g = Ctx()
    g.nc = nc
    g.S = S
    g.debug = debug
    if debug is not None and debug.startswith("rwkv1"):
        g.nheads = 1
        g.rw_stop = int(debug[5:6] or 9)
        g.no_bonus = debug.endswith("nb")
        debug = "rwkv"

    def dram_in(name, shape, dt=F32):
        return nc.dram_tensor(name, list(shape), dt, kind="ExternalInput").ap()

    def dram_out(name, shape, dt=F32):
        return nc.dram_tensor(name, list(shape), dt, kind="ExternalOutput").ap()

    g.x = dram_in("x", [T, D])
    g.ctx = dram_in("ctx", [TC, D])
    g.cc = dram_in("cc", [128, 16, 2])
    g.w_ada = dram_in("w_ada", [D, 6 * D])
    g.b_ada = dram_in("b_ada", [128, 96])
    g.w_in = dram_in("w_in", [D, P_IN])
    g.pv_in = dram_in("pv", [128, NPV])
    g.g2 = dram_in("g2", [160, 1024])
    g.w2bd = dram_in("w2bd", [16, 128, 128])
    g.a2bd = dram_in("a2bd", [16, 128, 128])
    g.lnx_g = dram_in("lnx_g", [16, 64])
    g.lnx_b = dram_in("lnx_b", [16, 64])
    g.w_conv_o = dram_in("w_conv_o", [1024, D])
    g.w_rwkv_o = dram_in("w_rwkv_o", [1024, D])
    g.w_out = dram_in("w_out", [D, D])
    g.ln1_g = dram_in("ln1_g", [1, D])
    g.ln1_b = dram_in("ln1_b", [1, D])
    g.ln2_g = dram_in("ln2_g", [1, D])
    g.ln2_b = dram_in("ln2_b", [1, D])
    g.w_router = dram_in("w_router", [128, 16, 32])
    g.b_router = dram_in("b_router", [1, 32])
    g.w_gu = dram_in("w_gu", [32, D, 2 * D])
    g.b_gu = dram_in("b_gu", [128, 1024])
    g.w_dn = dram_in("w_dn", [32, D, D])
    g.b_dn = dram_in("b_dn", [32, D])

    g.ident = nc.alloc_sbuf_tensor("ident", [128, 128], F32)
    g.anti = nc.alloc_sbuf_tensor("anti", [128, 128], F32)
    g.eps_ln = nc.alloc_sbuf_tensor("eps_ln", [128, 1], F32)
    g.t_const = Tok()
    S.op("pool", lambda e: e.memset(g.eps_ln[:], LN_EPS), writes=[g.t_const])
    g.t_ident = Tok()
    make_identity(g, g.ident, g.anti, g.t_ident)
    g.pv = nc.alloc_sbuf_tensor("pv_sb", [128, NPV], F32)
    g.t_pv = Tok()
    S.dma("sp", "ld_small", g.pv[:], g.pv_in, writes=[g.t_pv])
    phase_mod(g)
    phase_ln1(g, False)
    if debug == "ln1":
        o = dram_out("dbg_xmT", [128, 16, TT], BF16)

        tk = Tok()
        S.dma("sp", "dbg", o, g.xmT[:], reads=[g.t_xmT], writes=[tk])
        o2 = dram_out("dbg_mod", [128, 96, 2], F32)
        S.dma("sp", "dbg", o2, g.mod[:], reads=[g.t_mod], writes=[tk])
        S.finish([tk])
        return nc
    phase_proj(g, False)
    phase_ln1(g, True)
    phase_proj(g, True)
    g.es1.close()
    if debug == "proj":
        tk = Tok()
        for nm, src, tok in (("RKV", g.RKV, g.t_RKV), ("SGs", g.SGs, g.t_SGs), ("AGs", g.AGs, g.t_AGs),
                             ("Gs", g.Gs, g.t_Gs), ("ZC", g.ZC, g.t_ZC), ("SGZ", g.SGZ, g.t_SGZ)):
            o = dram_out("dbg_" + nm, list(src.shape), F32)
            S.dma("sp", "dbg", o, src, reads=[tok], writes=[tk])
        S.finish([tk])
        return nc
    phase_rwkv(g)
    if debug is None or debug == "full1":
        phase_mix(g)
        phase_out1(g)
        g.out_toks = []
        phase_moe(g, dram_out("out", [T, D], F32))
        S.finish(g.out_toks)
        return nc
    if debug == "rwkv":
        tk = Tok()
        o = dram_out("dbg_ORW", [1024, T], F32)
        S.dma("sp", "dbg", o, g.ORW, reads=[g.t_ORW], writes=[tk])
        for nm, buf in g.rw_dbg.items():
            o = dram_out("dbg_" + nm, list(buf.shape), F32)
            S.dma("sp", "dbg", o, buf, reads=[], writes=[tk])
        S.finish([tk])
        return nc
    return nc


def phase_mod(g):
    nc, S = g.nc, g.S
    g.mod = nc.alloc_sbuf_tensor("mod", [128, 96, 2], F32)
    g.t_mod = Tok("mod")
    g.modp = nc.alloc_sbuf_tensor("modp", [128, 96, 2], F32)
    sc = nc.alloc_sbuf_tensor("sc", [128, 16, 2], F32)
    t_sc = Tok()
    bada = nc.alloc_sbuf_tensor("bada", [128, 96], F32)
    t_b = Tok()
    S.dma("sp", "ld_small", sc[:], g.cc, writes=[t_sc])
    S.dma("sp", "ld_small", bada[:], g.b_ada, writes=[t_b])
    S.op("act", lambda e: e.activation(out=sc[:], in_=sc[:], func=AF.Silu), reads=[t_sc], writes=[t_sc])
    NG = 24
    wsrc = g.w_ada.rearrange("(kc p) n -> p kc n", p=128)
    with nc.sbuf_tensor("wada0", [128, 16, 512], F32) as wb0, nc.sbuf_tensor("wada1", [128, 16, 512], F32) as wb1, \
            nc.psum_tensor("ps_mod", [128, 96, 2], F32) as ps:
        wb = [wb0, wb1]
        t_wb = [Tok(), Tok()]
        t_ps = Tok(excl=True)
        for gi in range(NG):
            b = gi % 2
            S.dma("sp" if gi % 2 == 0 else "pool", "ld_wada%d" % b, wb[b][:], wsrc[:, :, gi * 512:(gi + 1) * 512],
                  writes=[t_wb[b]])
            for j in range(4):
                fo = gi * 4 + j
                for kc in range(16):
                    S.op("pe", lambda e, kc=kc, j=j, fo=fo, b=b: e.matmul(
                        ps[:, fo, :], lhsT=wb[b][:, kc, j * 128:(j + 1) * 128], rhs=sc[:, kc, :],
                        start=(kc == 0), stop=(kc == 15)),
                         reads=[t_wb[b], t_sc], writes=[t_ps])
        for i in range(2):
            S.op("dve", lambda e, i=i: e.tensor_tensor(out=g.mod[:, :, i], in0=ps[:, :, i], in1=bada[:], op=ALU.add),
                 reads=[t_ps, t_b], writes=[g.t_mod])
    S.op("dve", lambda e: e.tensor_scalar(out=g.modp[:], in0=g.mod[:], scalar1=1.0, scalar2=None, op0=ALU.add),
         reads=[g.t_mod], writes=[g.t_mod])
    S.barrier()


def phase_ln1(g, rev_pass):
    nc, S = g.nc, g.S
    if not rev_pass:
        g.es1 = ExitStack()
        g.xmT = g.es1.enter_context(nc.sbuf_tensor("xmT", [128, 16, TT], BF16))
        g.t_xmT = Tok("xmT")
    sfx = "r" if rev_pass else "f"
    with ExitStack() as es:
        sb = lambda name, shape, dt=F32: es.enter_context(nc.sbuf_tensor(name + sfx, shape, dt))
        psb = lambda name, shape, dt=F32: es.enter_context(nc.psum_tensor(name + sfx, shape, dt))
        xt = [sb("xt0", [128, D]), sb("xt1", [128, D])]
        st, mv = sb("ln_st", [128, 4, 6]), sb("ln_mv", [128, 4])
        pst = [psb("ps_tr%d" % i, [128, 4, 128]) for i in range(4)]
        t_xt = [Tok(), Tok()]
        t_st = Tok()
        t_pst = [Tok(excl=True) for _ in range(4)]
        for tt in range(NT + NTC):
            b = tt % 2
            if tt < NT:
                src = g.x[tt * 128:(tt + 1) * 128, :]
                col = 0
                pos = TC + (NT - 1 - tt) * 128 if rev_pass else TC + tt * 128
            else:
                src = g.ctx[(tt - NT) * 128:(tt - NT + 1) * 128, :]
                col = 1
                pos = (NTC - 1 - (tt - NT)) * 128 if rev_pass else (tt - NT) * 128
            S.dma("sp", "ld_x%d" % b, xt[b][:], src, writes=[t_xt[b]])
            ln_rows(g, xt[b], t_xt[b], st, mv, t_st)
            for fc in range(16):
                pb = fc // 4
                if rev_pass:
                    S.op("pe", lambda e, fc=fc, pb=pb, b=b: e.matmul(
                        pst[pb][:, fc % 4, :], lhsT=xt[b][:, fc * 128:(fc + 1) * 128], rhs=g.anti[:],
                        start=True, stop=True), reads=[t_xt[b], g.t_ident], writes=[t_pst[pb]])
                else:
                    S.op("pe", lambda e, fc=fc, pb=pb, b=b: e.transpose(
                        pst[pb][:, fc % 4, :], xt[b][:, fc * 128:(fc + 1) * 128], g.ident[:]),
                         reads=[t_xt[b], g.t_ident], writes=[t_pst[pb]])
                if fc % 4 == 3:
                    for f2 in range(fc - 3, fc + 1):
                        S.op("act", lambda e, f2=f2, pb=pb, pos=pos, col=col: e.activation(
                            out=g.xmT[:, f2, pos:pos + 128], in_=pst[pb][:, f2 % 4, :], func=AF.Identity,
                            bias=g.mod[:, f2, col:col + 1], scale=g.modp[:, 16 + f2, col:col + 1]),
                             reads=[t_pst[pb], g.t_mod], writes=[g.t_xmT])
    S.barrier()


def ln_rows(g, xt, t_x, st, mv, t_st, n=4):
    S = g.S
    for q in range(n):
        S.op("dve", lambda e, q=q: e.bn_stats(out=st[:, q, :], in_=xt[:, q * 512:(q + 1) * 512]),
             reads=[t_x], writes=[t_st])
    S.op("dve", lambda e: e.bn_aggr(out=mv[:, 0:2], in_=st[:, 0:n, :].rearrange("p a b -> p (a b)")),
         reads=[t_st], writes=[t_st])
    S.op("act", lambda e: e.activation(out=mv[:, 3:4], in_=mv[:, 1:2], func=AF.Sqrt, bias=g.eps_ln[:, 0:1],
                                       scale=1.0), reads=[t_st, g.t_const], writes=[t_st])
    S.op("dve", lambda e: e.reciprocal(out=mv[:, 2:3], in_=mv[:, 3:4]), reads=[t_st], writes=[t_st])
    S.op("dve", lambda e: e.tensor_scalar(out=xt[:, 0:n * 512], in0=xt[:, 0:n * 512], scalar1=mv[:, 0:1],
                                          scalar2=mv[:, 2:3], op0=ALU.subtract, op1=ALU.mult),
         reads=[t_st, t_x], writes=[t_x])


def make_identity(g, ident, anti, tok):
    nc, S = g.nc, g.S
    S.op("pool", lambda e: e.memset(ident[:], 0.0), writes=[tok])
    S.op("pool", lambda e: e.memset(anti[:], 0.0), writes=[tok])
    S.op("pool", lambda e: e.affine_select(out=ident[:], in_=ident[:], pattern=[[-1, 128]],
                                           compare_op=ALU.not_equal, fill=1.0, base=0, channel_multiplier=1),
         reads=[tok], writes=[tok])
    S.op("pool", lambda e: e.affine_select(out=anti[:], in_=anti[:], pattern=[[1, 128]],
                                           compare_op=ALU.not_equal, fill=1.0, base=-127, channel_multiplier=1),
         reads=[tok], writes=[tok])


def pv_layout():
    names = []
    for q in range(3):
        for h in range(16):
            names += ["b_%d_%d" % (q, h), "cp_%d_%d" % (q, h), "cn_%d_%d" % (q, h)]
    for nm in ("wd", "ad", "gd1", "gd2"):
        names += ["b_" + nm, "cp_" + nm, "cn_" + nm]
    for h in range(16):
        names += ["w0_%d" % h, "a0_%d" % h, "kk_%d" % h, "ka_%d" % h, "rk_%d" % h]
    for c in range(8):
        names += ["cba_%d" % c, "cbg_%d" % c, "convb_%d" % c, "clng_%d" % c, "clnb_%d" % c]
        names += ["cw_%d_%d" % (c, j) for j in range(31)]
    for c in range(32):
        names += ["bzg_%d" % c]
    for c in range(16):
        names += ["bco_%d" % c, "bout_%d" % c]
    return {n: i for i, n in enumerate(names)}


PVL = pv_layout()
NPV = len(PVL)


def make_pv(inp):
    pv = np.zeros((128, NPV), np.float32)
    b_in = inp["b_in"][0]
    mu = inp["shift_mu"][0]

    def dup(v):
        return np.concatenate([v, v])
    for q in range(3):
        for h in range(16):
            c0 = 2048 + q * 1024 + h * 64
            zc = c0 - 2048
            pv[:, PVL["b_%d_%d" % (q, h)]] = dup(b_in[c0:c0 + 64])
            pv[:, PVL["cp_%d_%d" % (q, h)]] = np.concatenate([mu[0, zc:zc + 64], mu[1, zc:zc + 64]])
            pv[:, PVL["cn_%d_%d" % (q, h)]] = np.concatenate([mu[1, zc:zc + 64], mu[0, zc:zc + 64]])
    for nm, c0 in (("wd", 5120), ("ad", 5248)):
        zc = c0 - 2048
        pv[:, PVL["b_" + nm]] = b_in[c0:c0 + 128]
        pv[:, PVL["cp_" + nm]] = np.concatenate([mu[0, zc:zc + 64], mu[1, zc + 64:zc + 128]])
        pv[:, PVL["cn_" + nm]] = np.concatenate([mu[1, zc:zc + 64], mu[0, zc + 64:zc + 128]])
    pv[:, PVL["b_gd1"]] = b_in[5376:5504]
    pv[:, PVL["cp_gd1"]] = mu[0, 5376 - 2048:5504 - 2048]
    pv[:, PVL["cn_gd1"]] = mu[1, 5376 - 2048:5504 - 2048]
    pv[:32, PVL["b_gd2"]] = b_in[5504:5536]
    pv[:32, PVL["cp_gd2"]] = mu[0, 5504 - 2048:5536 - 2048]
    pv[:32, PVL["cn_gd2"]] = mu[1, 5504 - 2048:5536 - 2048]
    for h in range(16):
        sl = slice(h * 64, h * 64 + 64)
        pv[:, PVL["w0_%d" % h]] = np.concatenate([inp["w0"][0, 0, sl], inp["w0"][0, 1, sl]])
        pv[:, PVL["a0_%d" % h]] = np.concatenate([inp["a0"][0, 0, sl], inp["a0"][0, 1, sl]])
        pv[:, PVL["kk_%d" % h]] = dup(inp["k_k"][0, sl])
        pv[:, PVL["ka_%d" % h]] = dup(inp["k_a"][0, sl])
        pv[:, PVL["rk_%d" % h]] = dup(inp["r_k"][0, h])
    for c in range(8):
        sl = slice(c * 128, c * 128 + 128)
        pv[:, PVL["cba_%d" % c]] = b_in[sl]
        pv[:, PVL["cbg_%d" % c]] = b_in[1024 + c * 128:1024 + c * 128 + 128]
        pv[:, PVL["convb_%d" % c]] = inp["conv_b"][0, sl]
        pv[:, PVL["clng_%d" % c]] = inp["conv_ln_g"][0, sl]
        pv[:, PVL["clnb_%d" % c]] = inp["conv_ln_b"][0, sl]
        for j in range(31):
            pv[:, PVL["cw_%d_%d" % (c, j)]] = inp["conv_w"][0, j, sl]
    for c in range(32):
        pv[:, PVL["bzg_%d" % c]] = b_in[5536 + c * 128:5536 + c * 128 + 128]
    for c in range(16):
        pv[:, PVL["bco_%d" % c]] = inp["b_conv_o"][0, c * 128:c * 128 + 128]
        pv[:, PVL["bout_%d" % c]] = inp["b_out"][0, c * 128:c * 128 + 128]
    return pv


def pvc(g, name):
    i = PVL[name]
    return g.pv[:, i:i + 1]


TG_ALL = [(0, 512), (512, 512), (1024, 512), (1536, 512), (2048, 256)]
TG_LAT = [(TC + i * 512, 512) for i in range(4)]


def phase_proj(g, rev_pass):
    nc, S = g.nc, g.S
    if not rev_pass:
        dr = lambda name, shape, dt=F32: nc.dram_tensor(name, list(shape), dt, kind="Internal").ap()
        g.RKV = dr("s_rkv", [3, 16, 128, TT])
        g.SGs = dr("s_sg", [16, 128, TT])
        g.AGs = dr("s_ag", [16, 128, TT])
        g.Gs = dr("s_g", [T, 1024])
        g.ZC = dr("s_zc", [1024, T])
        g.SGZ = dr("s_sgz", [4096, T])
        g.t_RKV, g.t_SGs, g.t_AGs, g.t_Gs, g.t_ZC, g.t_SGZ = (Tok() for _ in range(6))
        g.wdt = g.es1.enter_context(nc.sbuf_tensor("wdt", [128, TT], F32))
        g.ads = g.es1.enter_context(nc.sbuf_tensor("ads", [128, TT], F32))
        g.t_wdt, g.t_ads = Tok(), Tok()
    sfx = "r" if rev_pass else "f"
    wsrc = g.w_in.rearrange("(kc p) n -> p kc n", p=128)
    with ExitStack() as es:
        sb = lambda name, shape, dt=F32: es.enter_context(nc.sbuf_tensor(name + sfx, shape, dt))
        psb = lambda name, shape, dt=F32: es.enter_context(nc.psum_tensor(name + sfx, shape, dt))
        wf = [sb("wf0", [128, 16, 128]), sb("wf1", [128, 16, 128])]
        wb = [sb("wb0", [128, 16, 128], BF16), sb("wb1", [128, 16, 128], BF16)]
        zraw = sb("zraw", [128, TT])
        zs = [sb("zs0", [128, TT]), sb("zs1", [128, TT])]
        c0t = sb("c0t", [128, 1])
        lw2 = sb("lw2", [128, 2, 128])
        pp = [psb("pp%d" % i, [128, 512]) for i in range(4)]
        t_wf = [Tok(), Tok()]
        t_wb = [Tok(), Tok()]
        t_pp = [Tok(excl=True) for _ in range(4)]
        t_zraw, t_c0 = Tok(), Tok()
        t_zs = [Tok(), Tok()]
        st = {"wi": 0, "pi": 0, "zi": 0}

        def load_w(col0, M, dupl=False):
            i = st["wi"] % 2
            st["wi"] += 1
            S.dma("sp", "ld_w%d" % i, wf[i][:, :, 0:M], wsrc[:, :, col0:col0 + M], writes=[t_wf[i]])
            S.op("pool", lambda e: e.tensor_copy(out=wb[i][:, :, 0:M], in_=wf[i][:, :, 0:M]),
                 reads=[t_wf[i]], writes=[t_wb[i]])
            if dupl:
                S.op("pool", lambda e: e.tensor_copy(out=wb[i][:, :, M:2 * M], in_=wf[i][:, :, 0:M]),
                     reads=[t_wf[i]], writes=[t_wb[i]])
            return i

        def mm_group(i, M, p0, n):
            pi = st["pi"] % 4
            st["pi"] += 1
            for kc in range(16):
                S.op("pe", lambda e, kc=kc: e.matmul(pp[pi][0:M, 0:n], lhsT=wb[i][:, kc, 0:M],
                                                      rhs=g.xmT[:, kc, p0:p0 + n], start=(kc == 0), stop=(kc == 15)),
                     reads=[t_wb[i], g.t_xmT], writes=[t_pp[pi]])
            return pi

        def shift(zin, t_in, zout, t_out, plo, phi, cp, cn, segs):
            ps_ = slice(plo, phi)
            S.op("dve", lambda e: e.tensor_scalar(out=c0t[ps_, :], in0=cp[ps_, :], scalar1=cn[ps_, :], scalar2=-1.0,
                                                  op0=ALU.add, op1=ALU.mult), reads=[g.t_pv], writes=[t_c0])
            S.op("dve", lambda e: e.tensor_scalar(out=c0t[ps_, :], in0=c0t[ps_, :], scalar1=1.0, scalar2=None,
                                                  op0=ALU.add), reads=[t_c0], writes=[t_c0])
            lo, hi = segs[0][0], segs[-1][1]
            S.op("dve", lambda e: e.tensor_scalar(out=zout[ps_, lo:hi], in0=zin[ps_, lo:hi], scalar1=c0t[ps_, :],
                                                  scalar2=None, op0=ALU.mult), reads=[t_in, t_c0], writes=[t_out])
            for (a, b) in segs:
                S.op("dve", lambda e, a=a, b=b: e.scalar_tensor_tensor(
                    out=zout[ps_, a + 1:b], in0=zin[ps_, a:b - 1], scalar=cp[ps_, :], in1=zout[ps_, a + 1:b],
                    op0=ALU.mult, op1=ALU.add), reads=[t_in, g.t_pv], writes=[t_out])
                S.op("dve", lambda e, a=a, b=b: e.scalar_tensor_tensor(
                    out=zout[ps_, a:b - 1], in0=zin[ps_, a + 1:b], scalar=cn[ps_, :], in1=zout[ps_, a:b - 1],
                    op0=ALU.mult, op1=ALU.add), reads=[t_in, g.t_pv], writes=[t_out])

        SEG2 = [(0, TC), (TC, TT)]

        def one_pass(d, col0, M, dupl, bias, cp, cn, zout, t_out):
            i = load_w(col0, M, dupl)
            lo, hi = d * 64, d * 64 + 64
            for (p0, n) in TG_ALL:
                pi = mm_group(i, 128, p0, n)
                S.op("act", lambda e, pi=pi, p0=p0, n=n: e.activation(
                    out=zraw[lo:hi, p0:p0 + n], in_=pp[pi][lo:hi, 0:n], func=AF.Identity,
                    bias=bias[lo:hi, :], scale=1.0), reads=[t_pp[pi], g.t_pv], writes=[t_zraw])
            shift(zraw, t_zraw, zout, t_out, lo, hi, cp, cn, SEG2)

        def rkv_pass(d):
            lo, hi = d * 64, d * 64 + 64
            one_pass(d, 5120, 128, False, pvc(g, "b_wd"), pvc(g, "cp_wd"), pvc(g, "cn_wd"), g.wdt, g.t_wdt)
            S.op("act", lambda e: e.activation(out=g.wdt[lo:hi, :], in_=g.wdt[lo:hi, :], func=AF.Tanh),
                 reads=[g.t_wdt], writes=[g.t_wdt])
            one_pass(d, 5248, 128, False, pvc(g, "b_ad"), pvc(g, "cp_ad"), pvc(g, "cn_ad"), g.ads, g.t_ads)
            for q in range(3):
                for h in range(16):
                    zi = st["zi"] % 2
                    st["zi"] += 1
                    one_pass(d, 2048 + q * 1024 + h * 64, 64, True, pvc(g, "b_%d_%d" % (q, h)),
                             pvc(g, "cp_%d_%d" % (q, h)), pvc(g, "cn_%d_%d" % (q, h)), zs[zi], t_zs[zi])
                    S.dma("sp", "st_a%d" % zi, g.RKV[q, h, lo:hi, :], zs[zi][lo:hi, :], reads=[t_zs[zi]],
                          writes=[g.t_RKV])

        def lora_stage():
            t_lw = Tok()
            for h in range(16):
                S.dma("sp", "ld_small", lw2[:, 0, :], g.w2bd[h], writes=[t_lw])
                S.dma("sp", "ld_small", lw2[:, 1, :], g.a2bd[h], writes=[t_lw])
                for k, (src, t_src, bname, dstd, t_dst) in enumerate(
                        ((g.wdt, g.t_wdt, "w0_%d" % h, g.SGs, g.t_SGs), (g.ads, g.t_ads, "a0_%d" % h, g.AGs, g.t_AGs))):
                    zi = k
                    for (p0, n) in TG_ALL:
                        pi = st["pi"] % 4
                        st["pi"] += 1
                        S.op("pe", lambda e, k=k, pi=pi, p0=p0, n=n, src=src: e.matmul(
                            pp[pi][:, 0:n], lhsT=lw2[:, k, :], rhs=src[:, p0:p0 + n], start=True, stop=True),
                             reads=[t_lw, t_src], writes=[t_pp[pi]])
                        S.op("act", lambda e, pi=pi, p0=p0, n=n, zi=zi, bname=bname: e.activation(
                            out=zs[zi][:, p0:p0 + n], in_=pp[pi][:, 0:n], func=AF.Sigmoid, bias=pvc(g, bname),
                            scale=1.0), reads=[t_pp[pi], g.t_pv], writes=[t_zs[zi]])
                    S.dma("sp", "st_a%d" % zi, dstd[h], zs[zi][:], reads=[t_zs[zi]], writes=[t_dst])

        if rev_pass:
            rkv_pass(1)
            lora_stage()
        else:
            rkv_pass(0)
            sgd1, sgd2 = sb("sgd1", [128, T]), sb("sgd2", [32, T])
            g2a, g2b = sb("g2a", [128, 1024]), sb("g2b", [32, 1024])
            t_sgd, t_g2 = Tok(), Tok()
            for nm, col0, M, dst in (("gd1", 5376, 128, sgd1), ("gd2", 5504, 32, sgd2)):
                i = load_w(col0, M)
                for (p0, n) in TG_LAT:
                    pi = mm_group(i, M, p0, n)
                    S.op("act", lambda e, pi=pi, p0=p0, n=n, M=M, nm=nm: e.activation(
                        out=zraw[0:M, p0:p0 + n], in_=pp[pi][0:M, 0:n], func=AF.Identity,
                        bias=pvc(g, "b_" + nm)[0:M, :], scale=1.0), reads=[t_pp[pi], g.t_pv], writes=[t_zraw])
                shift(zraw, t_zraw, zs[0], t_zs[0], 0, M, pvc(g, "cp_" + nm), pvc(g, "cn_" + nm), [(TC, TT)])
                S.op("act", lambda e, M=M, dst=dst: e.activation(out=dst[0:M, :], in_=zs[0][0:M, TC:TT],
                                                                 func=AF.Sigmoid), reads=[t_zs[0]], writes=[t_sgd])
            S.dma("sp", "ld_small", g2a[:], g.g2[0:128, :], writes=[t_g2])
            S.dma("sp", "ld_small", g2b[:], g.g2[128:160, :], writes=[t_g2])
            for c in range(32):
                zi = c % 2
                for hf in range(2):
                    pi = st["pi"] % 4
                    st["pi"] += 1
                    S.op("pe", lambda e, c=c, hf=hf, pi=pi: e.matmul(
                        pp[pi][0:64, :], lhsT=sgd1[:, c * 64:(c + 1) * 64], rhs=g2a[:, hf * 512:(hf + 1) * 512],
                        start=True, stop=False), reads=[t_sgd, t_g2], writes=[t_pp[pi]])
                    S.op("pe", lambda e, c=c, hf=hf, pi=pi: e.matmul(
                        pp[pi][0:64, :], lhsT=sgd2[:, c * 64:(c + 1) * 64], rhs=g2b[:, hf * 512:(hf + 1) * 512],
                        start=False, stop=True), reads=[t_sgd, t_g2], writes=[t_pp[pi]])
                    S.op("dve", lambda e, hf=hf, pi=pi, zi=zi: e.tensor_copy(
                        out=zs[zi][0:64, hf * 512:(hf + 1) * 512], in_=pp[pi][0:64, :]),
                         reads=[t_pp[pi]], writes=[t_zs[zi]])
                S.dma("sp", "st_a%d" % zi, g.Gs[c * 64:(c + 1) * 64, :], zs[zi][0:64, 0:1024],
                      reads=[t_zs[zi]], writes=[g.t_Gs])
            for c in range(8):
                ia = load_w(c * 128, 128)
                ig = load_w(1024 + c * 128, 128)
                for ti, (p0, n) in enumerate(TG_LAT):
                    pa = mm_group(ia, 128, p0, n)
                    pg = mm_group(ig, 128, p0, n)
                    S.op("act", lambda e, pg=pg, ti=ti: e.activation(
                        out=zs[0][:, ti * 512:(ti + 1) * 512], in_=pp[pg][:, :], func=AF.Sigmoid,
                        bias=pvc(g, "cbg_%d" % c), scale=1.0), reads=[t_pp[pg], g.t_pv], writes=[t_zs[0]])
                    S.op("dve", lambda e, pa=pa, ti=ti: e.scalar_tensor_tensor(
                        out=zraw[:, ti * 512:(ti + 1) * 512], in0=pp[pa][:, :], scalar=pvc(g, "cba_%d" % c),
                        in1=zs[0][:, ti * 512:(ti + 1) * 512], op0=ALU.add, op1=ALU.mult),
                         reads=[t_pp[pa], t_zs[0], g.t_pv], writes=[t_zraw])
                hv = zraw[:, 0:T].rearrange("p (r w) -> p r w", w=64)
                ov = zs[1][:, 0:T].rearrange("p (r w) -> p r w", w=64)
                S.op("dve", lambda e: e.tensor_scalar(out=zs[1][:, 0:T], in0=zraw[:, 0:T],
                                                      scalar1=pvc(g, "cw_%d_15" % c), scalar2=pvc(g, "convb_%d" % c),
                                                      op0=ALU.mult, op1=ALU.add),
                     reads=[t_zraw, g.t_pv], writes=[t_zs[1]])
                for j in range(31):
                    o = j - 15
                    if o == 0:
                        continue
                    lo, hi = max(0, -o), min(64, 64 - o)
                    S.op("dve", lambda e, j=j, o=o, lo=lo, hi=hi: e.scalar_tensor_tensor(
                        out=ov[:, :, lo:hi], in0=hv[:, :, lo + o:hi + o], scalar=pvc(g, "cw_%d_%d" % (c, j)),
                        in1=ov[:, :, lo:hi], op0=ALU.mult, op1=ALU.add), reads=[t_zraw, g.t_pv], writes=[t_zs[1]])
                S.dma("sp", "st_a1", g.ZC[c * 128:(c + 1) * 128, :], zs[1][:, 0:T], reads=[t_zs[1]],
                      writes=[g.t_ZC])
            for c in range(32):
                i = load_w(5536 + c * 128, 128)
                zi = c % 2
                for ti, (p0, n) in enumerate(TG_LAT):
                    pi = mm_group(i, 128, p0, n)
                    S.op("act", lambda e, pi=pi, ti=ti, zi=zi: e.activation(
                        out=zs[zi][:, ti * 512:(ti + 1) * 512], in_=pp[pi][:, :], func=AF.Sigmoid,
                        bias=pvc(g, "bzg_%d" % c), scale=1.0), reads=[t_pp[pi], g.t_pv], writes=[t_zs[zi]])
                S.dma("sp", "st_a%d" % zi, g.SGZ[c * 128:(c + 1) * 128, :], zs[zi][:, 0:T],
                      reads=[t_zs[zi]], writes=[g.t_SGZ])
    S.barrier()


KAPPA = float(np.exp(-0.5))
GN_EPS = 64e-5
NCH = TT // 64


def phase_rwkv(g):
    nc, S = g.nc, g.S
    g.ORW = nc.dram_tensor("s_orw", [1024, T], F32, kind="Internal").ap()
    g.t_ORW = Tok()
    with ExitStack() as es:
        sb = lambda name, shape, dt=F32: es.enter_context(nc.sbuf_tensor("rw_" + name, shape, dt))
        psb = lambda name, shape, dt=F32: es.enter_context(nc.psum_tensor("rwp_" + name, shape, dt))
        R, Kt, Vt, SG, AG, KK, LI, TM = (sb(n, [128, TT]) for n in ("R", "Kt", "Vt", "SG", "AG", "KK", "LI", "TM"))
        QR, BK = sb("QR", [128, 2, TT]), sb("BK", [128, 2, TT])
        BKh = sb("BKh", [64, NCH, 2, 128])
        Vtm = sb("Vtm", [64, NCH, 128])
        Ys = sb("Ys", [64, 2, 32, 66])
        DC = sb("DC", [128, NCH])
        A2 = [sb("A0", [64, 2, 320]), sb("A1", [64, 2, 320])]
        W4 = [[sb("W00", [64, 2, 3, 64]), sb("W01", [64, 2, 3, 64])], [sb("W10", [64, 2, 3, 64]), sb("W11", [64, 2, 3, 64])]]
        tA = [Tok("A0"), Tok("A1")]
        tW = [[Tok(), Tok()], [Tok(), Tok()]]
        A_sb, W = A2[1], W4[1]
        P1s, UTs = sb("P1s", [64, 2, 64]), sb("UTs", [64, 2, 64])
        Sst = sb("Sst", [128, 64])
        maskc = sb("maskc", [128, TT])
        maskA = sb("maskA", [64, 320])
        onesbd = sb("onesbd", [128, 128])
        onesel = sb("onesel", [128, 2])
        omka = sb("omka", [128, 1])
        LG, LB = sb("LG", [64, 64]), sb("LB", [64, 64])
        st1, st2 = sb("st1", [64, 32]), sb("st2", [64, 32])
        epsg = sb("epsg", [64, 1])
        banks = [psb("bk%d" % i, [128, 512]) for i in range(8)]
        PA = [banks[0][0:64, 0:320], banks[1][0:64, 0:320]]
        PS = banks[2][0:64, 0:384].rearrange("p (d w) -> p d w", w=192)
        PQ = banks[3][0:64, 0:256].rearrange("p (a w) -> p a w", w=64)
        PSn = banks[4][:, 0:128].rearrange("p (d w) -> p d w", w=64)
        PT = banks[5]
        PT2 = banks[6]
        PU = banks[7][0:64, 0:128].rearrange("p (a w) -> p a w", w=64)
        t = {n: Tok(n) for n in ("R", "Kt", "Vt", "SG", "AG", "KK", "LI", "TM", "QR", "BK", "BKh", "Vtm", "Ys", "DC",
                                 "A", "W0", "W1", "P1s", "UTs", "Sst", "c", "LG", "st", "G")}
        for i in range(8):
            t["B%d" % i] = Tok("B%d" % i, excl=True)
        t["PA0"], t["PA1"], t["PS"], t["PQ1"], t["PQ3"], t["PSn"], t["PT"], t["PT2"], t["PQ2"] = (
            t["B0"], t["B1"], t["B2"], t["B3"], t["B3"], t["B4"], t["B5"], t["B6"], t["B7"])
        S.op("pool", lambda e: e.memset(maskc[:], 1.0), writes=[t["c"]])
        S.op("pool", lambda e: e.memset(maskc[:].rearrange("p (c w) -> p c w", w=64)[:, :, 0:1], 0.0),
             reads=[t["c"]], writes=[t["c"]])
        S.op("pool", lambda e: e.memset(onesbd[:], 0.0), writes=[t["c"]])
        S.op("pool", lambda e: e.memset(onesbd[0:64, 0:64], 1.0), reads=[t["c"]], writes=[t["c"]])
        S.op("pool", lambda e: e.memset(onesbd[64:128, 64:128], 1.0), reads=[t["c"]], writes=[t["c"]])
        S.op("pool", lambda e: e.memset(onesel[:], 0.0), writes=[t["c"]])
        S.op("pool", lambda e: e.memset(onesel[0:64, 0:1], 1.0), reads=[t["c"]], writes=[t["c"]])
        S.op("pool", lambda e: e.memset(onesel[64:128, 1:2], 1.0), reads=[t["c"]], writes=[t["c"]])
        S.op("pool", lambda e: e.memset(epsg[:], GN_EPS), writes=[t["c"]])
        S.op("pool", lambda e: e.memset(maskA[:], 1.0), writes=[t["c"]])
        for blk, (cm, base, pat) in enumerate(((-1, -1, 1), (-1, 0, 1), (-1, -1, 1), (-1, 0, 1), (1, -1, -1))):
            S.op("pool", lambda e, blk=blk, cm=cm, base=base, pat=pat: e.affine_select(
                out=maskA[:, blk * 64:(blk + 1) * 64], in_=maskA[:, blk * 64:(blk + 1) * 64], pattern=[[pat, 64]],
                compare_op=ALU.is_ge, fill=0.0, base=base, channel_multiplier=cm), reads=[t["c"]], writes=[t["c"]])
        I64 = g.ident[0:64, 0:64]
        J64 = g.anti[0:64, 64:128]
        g.rw_dbg = {}
        for h in range(getattr(g, "nheads", 16)):
            for buf, nm, src, tk in ((R, "R", g.RKV[0, h], g.t_RKV), (Kt, "Kt", g.RKV[1, h], g.t_RKV),
                                     (Vt, "Vt", g.RKV[2, h], g.t_RKV), (SG, "SG", g.SGs[h], g.t_SGs),
                                     (AG, "AG", g.AGs[h], g.t_AGs)):
                S.dma("sp", "ld_rw_" + nm, buf[:], src, reads=[tk], writes=[t[nm]])
            S.dma("pool", "ld_rw_lg", LG[:], g.lnx_g[h:h + 1, :].partition_broadcast(64), writes=[t["LG"]])
            S.dma("pool", "ld_rw_lg", LB[:], g.lnx_b[h:h + 1, :].partition_broadcast(64), writes=[t["LG"]])
            kkc, kac, rkc = pvc(g, "kk_%d" % h), pvc(g, "ka_%d" % h), pvc(g, "rk_%d" % h)
            S.op("dve", lambda e: e.tensor_scalar(out=omka[:], in0=kac, scalar1=-1.0, scalar2=1.0, op0=ALU.mult,
                                                  op1=ALU.add), reads=[g.t_pv], writes=[t["c"]])
            S.op("dve", lambda e: e.tensor_scalar(out=TM[:], in0=Kt[:], scalar1=kkc, scalar2=None, op0=ALU.mult),
                 reads=[t["Kt"], g.t_pv], writes=[t["TM"]])
            S.op("act", lambda e: e.activation(out=KK[:], in_=TM[:], func=AF.Square), reads=[t["TM"]], writes=[t["KK"]])
            for i, (p0, n) in enumerate(TG_ALL):
                pt, tn = (PT, "PT") if i % 2 == 0 else (PT2, "PT2")
                S.op("pe", lambda e, pt=pt, p0=p0, n=n: e.matmul(pt[:, 0:n], lhsT=onesbd[:], rhs=KK[:, p0:p0 + n],
                                                                start=True, stop=True),
                     reads=[t["KK"], t["c"]], writes=[t[tn]])
                S.op("act", lambda e, pt=pt, p0=p0, n=n: e.activation(out=LI[:, p0:p0 + n], in_=pt[:, 0:n],
                                                                      func=AF.Sqrt), reads=[t[tn]], writes=[t["LI"]])
            S.op("dve", lambda e: e.tensor_scalar(out=LI[:], in0=LI[:], scalar1=1e-12, scalar2=None, op0=ALU.max),
                 reads=[t["LI"]], writes=[t["LI"]])
            S.op("dve", lambda e: e.reciprocal(out=LI[:], in_=LI[:]), reads=[t["LI"]], writes=[t["LI"]])
            S.op("dve", lambda e: e.tensor_tensor(out=KK[:], in0=TM[:], in1=LI[:], op=ALU.mult),
                 reads=[t["TM"], t["LI"]], writes=[t["KK"]])
            if getattr(g, "rw_stop", 9) <= 1:
                continue
            S.op("dve", lambda e: e.tensor_tensor_scan(out=LI[:], data0=maskc[:], data1=SG[:], initial=0.0,
                                                       op0=ALU.mult, op1=ALU.add),
                 reads=[t["SG"], t["c"], t["KK"]], writes=[t["LI"]])
            S.op("dve", lambda e: e.tensor_tensor(out=SG[:], in0=LI[:], in1=SG[:], op=ALU.subtract),
                 reads=[t["LI"]], writes=[t["SG"]])
            LIv = LI[:].rearrange("p (c w) -> p c w", w=64)
            S.op("act", lambda e: e.activation(out=DC[:], in_=LIv[:, :, 63], func=AF.Exp, scale=-KAPPA),
                 reads=[t["LI"]], writes=[t["DC"]])
            S.op("act", lambda e: e.activation(out=TM[:], in_=LI[:], func=AF.Exp, scale=-KAPPA),
                 reads=[t["LI"], t["KK"]], writes=[t["TM"]])
            S.op("dve", lambda e: e.tensor_tensor(out=QR[:, 1, :], in0=R[:], in1=TM[:], op=ALU.mult),
                 reads=[t["R"], t["TM"]], writes=[t["QR"]])
            S.op("act", lambda e: e.activation(out=TM[:], in_=SG[:], func=AF.Exp, scale=-KAPPA),
                 reads=[t["SG"], t["QR"]], writes=[t["TM"]])
            S.op("dve", lambda e: e.scalar_tensor_tensor(out=QR[:, 0, :], in0=KK[:], scalar=-1.0, in1=TM[:],
                                                         op0=ALU.mult, op1=ALU.mult),
                 reads=[t["KK"], t["TM"]], writes=[t["QR"]])
            S.op("act", lambda e: e.activation(out=TM[:], in_=LI[:], func=AF.Exp, scale=KAPPA),
                 reads=[t["LI"], t["QR"]], writes=[t["TM"]])
            S.op("dve", lambda e: e.tensor_scalar(out=SG[:], in0=AG[:], scalar1=kac, scalar2=omka[:, 0:1],
                                                  op0=ALU.mult, op1=ALU.add),
                 reads=[t["AG"], t["c"], g.t_pv, t["TM"]], writes=[t["SG"]])
            S.op("dve", lambda e: e.tensor_tensor(out=Kt[:], in0=Kt[:], in1=SG[:], op=ALU.mult),
                 reads=[t["SG"], t["TM"]], writes=[t["Kt"]])
            S.op("dve", lambda e: e.tensor_tensor(out=AG[:], in0=KK[:], in1=AG[:], op=ALU.mult),
                 reads=[t["KK"]], writes=[t["AG"]])
            S.op("dve", lambda e: e.tensor_tensor(out=BK[:, 0, :], in0=AG[:], in1=TM[:], op=ALU.mult),
                 reads=[t["AG"], t["TM"]], writes=[t["BK"]])
            S.op("dve", lambda e: e.tensor_tensor(out=BK[:, 1, :], in0=Kt[:], in1=TM[:], op=ALU.mult),
                 reads=[t["Kt"], t["TM"]], writes=[t["BK"]])
            S.op("dve", lambda e: e.scalar_tensor_tensor(out=R[:], in0=R[:], scalar=rkc, in1=Kt[:], op0=ALU.mult,
                                                         op1=ALU.mult), reads=[t["Kt"], t["QR"], g.t_pv], writes=[t["R"]])
            TMv = TM[:].rearrange("p (c w) -> p c w", w=64)
            S.op("dve", lambda e: e.tensor_tensor(out=TMv, in0=TMv, in1=DC[:].unsqueeze(2).to_broadcast([128, NCH, 64]),
                                                  op=ALU.mult), reads=[t["DC"], t["BK"]], writes=[t["TM"]])
            S.op("dve", lambda e: e.tensor_tensor(out=SG[:], in0=AG[:], in1=TM[:], op=ALU.mult),
                 reads=[t["AG"], t["TM"], t["Kt"]], writes=[t["SG"]])
            S.op("dve", lambda e: e.tensor_tensor(out=LI[:], in0=Kt[:], in1=TM[:], op=ALU.mult),
                 reads=[t["Kt"], t["TM"]], writes=[t["LI"]])
            if getattr(g, "rw_stop", 9) <= 2:
                continue
            import os
            _f3 = os.environ.get("RW3", "abcd")
            for c in range(int(os.environ.get("RW3N", NCH))):
                csl = slice(c * 64, (c + 1) * 64)
                pt, tn = (PT, "PT") if c % 2 == 0 else (PT2, "PT2")
                ptv = pt[0:64, 0:384].rearrange("p (a b) -> p a b", b=128)
                for a, (src, tk) in enumerate(((SG, "SG"), (LI, "LI"), (Vt, "Vt"))):
                    if "a" not in _f3:
                        continue
                    S.op("pe", lambda e, a=a, src=src, ptv=ptv, csl=csl: e.matmul(ptv[:, a, :], lhsT=src[:, csl], rhs=g.ident[:], start=True, stop=True),
                         reads=[t[tk], g.t_ident], writes=[t[tn]])
                if "b" in _f3:
                    S.op("act", lambda e, c=c, ptv=ptv: e.activation(out=BKh[:, c, :, :], in_=ptv[:, 0:2, :], func=AF.Identity),
                         reads=[t[tn]], writes=[t["BKh"]])
                if "c" in _f3:
                    S.op("dve", lambda e, c=c, ptv=ptv: e.tensor_copy(out=Vtm[:, c, :], in_=ptv[:, 2, :]),
                         reads=[t[tn]], writes=[t["Vtm"]])
                if c >= 4 and "d" in _f3:
                    S.op("pe", lambda e, c=c, pt=pt, csl=csl: e.matmul(pt[0:64, 384:386], lhsT=R[:, csl], rhs=onesel[:],
                                                                       start=True, stop=True),
                         reads=[t["R"], t["c"]], writes=[t[tn]])
                    S.op("dve", lambda e, c=c, pt=pt: e.tensor_copy(out=Ys[:, :, c - 4, 64], in_=pt[0:64, 384:386]),
                         reads=[t[tn]], writes=[t["Ys"]])
            if getattr(g, "rw_stop", 9) <= 3:
                continue
            S.op("pool", lambda e: e.memset(Sst[:], 0.0), reads=[t["Sst"]], writes=[t["Sst"]])
            PSv = PS.rearrange("p d (b w) -> p d b w", w=64)

            def emit_A(c, sl):
                csl = slice(c * 64, (c + 1) * 64)
                A_ = A2[sl]
                for d in range(2):
                    ds = slice(d * 64, d * 64 + 64)
                    pn = "PA%d" % d
                    for (o0, o1, lt, li, rhs_fn) in ((0, 128, BK, 0, lambda ds=ds: QR[ds, :, csl]),
                                                     (128, 256, BK, 1, lambda ds=ds: QR[ds, :, csl]),
                                                     (256, 320, QR, 0, lambda ds=ds: BK[ds, 0, csl])):
                        S.pe_rg(d * 64)
                        S.op("pe", lambda e, d=d, ds=ds, o0=o0, o1=o1, lt=lt, li=li, rhs_fn=rhs_fn: e.matmul(
                            PA[d][:, o0:o1], lhsT=lt[ds, li, csl], rhs=rhs_fn(), start=True, stop=True),
                             reads=[t["BK"], t["QR"]], writes=[t[pn]])
                    S.op("dve", lambda e, d=d, A_=A_: e.tensor_tensor(out=A_[:, d, :], in0=PA[d][:, :], in1=maskA[:],
                                                                     op=ALU.mult), reads=[t[pn], t["c"]], writes=[tA[sl]])
                Av = A_[:].rearrange("p d (b w) -> p d b w", w=64)
                w0 = W4[sl][0]
                S.op("act", lambda e: e.activation(out=w0[:, :, 0, :], in_=Av[:, :, 0, :], func=AF.Identity),
                     reads=[tA[sl]], writes=[tW[sl][0]])
                S.op("act", lambda e: e.activation(out=w0[:, :, 2, :], in_=Av[:, :, 4, :], func=AF.Identity),
                     reads=[tA[sl]], writes=[tW[sl][0]])
                S.op("dve", lambda e: e.tensor_copy(out=w0[:, :, 1, :], in_=I64.unsqueeze(1).to_broadcast([64, 2, 64])),
                     reads=[g.t_ident], writes=[tW[sl][0]])

            def emit_stage(s_, sl):
                wc, wn = W4[sl][s_ % 2], W4[sl][(s_ + 1) % 2]
                tc_, tn_ = tW[sl][s_ % 2], tW[sl][(s_ + 1) % 2]
                for d in range(2):
                    S.op("pe", lambda e, d=d: e.matmul(PS[:, d, 0:128], lhsT=wc[:, d, 2, :], rhs=wc[:, d, 0:2, :],
                                                       start=True, stop=True), reads=[tc_], writes=[t["PS"]])
                    if s_ < 5:
                        S.op("pe", lambda e, d=d: e.matmul(PS[:, d, 128:192], lhsT=wc[:, d, 0, :], rhs=wc[:, d, 2, :],
                                                           start=True, stop=True), reads=[tc_], writes=[t["PS"]])
                if s_ < 5:
                    for blk in (0, 2):
                        S.op("act", lambda e, blk=blk: e.activation(out=wn[:, :, blk, :], in_=PSv[:, :, blk, :],
                                                                    func=AF.Identity), reads=[t["PS"]], writes=[tn_])
                S.op("dve", lambda e: e.tensor_tensor(out=wn[:, :, 1, :], in0=wc[:, :, 1, :], in1=PSv[:, :, 1, :],
                                                      op=ALU.add), reads=[t["PS"], tc_], writes=[tn_])

            def seq_parts(c, sl):
                csl = slice(c * 64, (c + 1) * 64)
                A_ = A2[sl]
                Wf, tWf = W4[sl][0], tW[sl][0]

                def p0():
                    for d in range(2):
                        ds = slice(d * 64, d * 64 + 64)
                        S.pe_rg(d * 64)
                        S.op("pe", lambda e, d=d, ds=ds: e.matmul(PQ[:, d, :], lhsT=QR[ds, 0, csl], rhs=Sst[ds, :],
                                                                  start=True, stop=True),
                             reads=[t["QR"], t["Sst"]], writes=[t["PQ1"]])
                        S.op("pe", lambda e, d=d, ds=ds: e.matmul(PQ[:, 2 + d, :], lhsT=A_[:, d, 128:192], rhs=Vtm[:, c, ds],
                                                                  start=True, stop=True),
                             reads=[tA[sl], t["Vtm"]], writes=[t["PQ1"]])
                    S.op("act", lambda e: e.activation(out=P1s[:], in_=PQ[:, 0:2, :], func=AF.Identity),
                         reads=[t["PQ1"]], writes=[t["P1s"]])
                    S.op("dve", lambda e: e.tensor_tensor(out=P1s[:], in0=P1s[:], in1=PQ[:, 2:4, :], op=ALU.add),
                         reads=[t["PQ1"]], writes=[t["P1s"]])

                def p1():
                    for d in range(2):
                        S.op("pe", lambda e, d=d: e.matmul(PU[:, d, :], lhsT=Wf[:, d, 1, :], rhs=P1s[:, d, :],
                                                           start=True, stop=True), reads=[tWf, t["P1s"]], writes=[t["PQ2"]])
                    S.op("dve", lambda e: e.tensor_copy(out=UTs[:], in_=PU[:, 0:2, :]), reads=[t["PQ2"]], writes=[t["UTs"]])

                def p2():
                    for d in range(2):
                        ds = slice(d * 64, d * 64 + 64)
                        S.op("pe", lambda e, d=d: e.matmul(PSn[:, d, :], lhsT=BKh[:, c, 0, :], rhs=UTs[:, d, :],
                                                           start=True, stop=False), reads=[t["BKh"], t["UTs"]],
                             writes=[t["PSn"]])
                        S.op("pe", lambda e, d=d, ds=ds: e.matmul(PSn[:, d, :], lhsT=BKh[:, c, 1, :], rhs=Vtm[:, c, ds],
                                                                  start=False, stop=True),
                             reads=[t["BKh"], t["Vtm"]], writes=[t["PSn"]])

                def p3():
                    if c < 4:
                        return
                    for d in range(2):
                        ds = slice(d * 64, d * 64 + 64)
                        S.pe_rg(d * 64)
                        S.op("pe", lambda e, d=d, ds=ds: e.matmul(PQ[:, d, :], lhsT=QR[ds, 1, csl], rhs=Sst[ds, :],
                                                                  start=True, stop=True),
                             reads=[t["QR"], t["Sst"], t["P1s"]], writes=[t["PQ3"]])
                        S.op("pe", lambda e, d=d: e.matmul(PQ[:, 2 + d, :], lhsT=A_[:, d, 64:128], rhs=UTs[:, d, :],
                                                           start=True, stop=False), reads=[tA[sl], t["UTs"]],
                             writes=[t["PQ3"]])
                        S.op("pe", lambda e, d=d, ds=ds: e.matmul(PQ[:, 2 + d, :], lhsT=A_[:, d, 192:256],
                                                                  rhs=Vtm[:, c, ds], start=False, stop=True),
                             reads=[tA[sl], t["Vtm"]], writes=[t["PQ3"]])
                    S.op("act", lambda e: e.activation(out=Ys[:, :, c - 4, 0:64], in_=PQ[:, 0:2, :], func=AF.Identity),
                         reads=[t["PQ3"]], writes=[t["Ys"]])
                    S.op("dve", lambda e: e.tensor_tensor(out=Ys[:, :, c - 4, 0:64], in0=Ys[:, :, c - 4, 0:64],
                                                          in1=PQ[:, 2:4, :], op=ALU.add),
                         reads=[t["PQ3"]], writes=[t["Ys"]])

                def p4():
                    for d in range(2):
                        ds = slice(d * 64, d * 64 + 64)
                        S.op("dve", lambda e, d=d, ds=ds: e.scalar_tensor_tensor(
                            out=Sst[ds, :], in0=Sst[ds, :], scalar=DC[ds, c:c + 1], in1=PSn[ds, d, :], op0=ALU.mult,
                            op1=ALU.add), reads=[t["PSn"], t["DC"], t["Sst"]], writes=[t["Sst"]])
                return [p0, p1, p2, p3, p4]

            emit_A(0, 0)
            for s_ in range(6):
                emit_stage(s_, 0)
            for c in range(NCH):
                sl = c % 2
                parts = seq_parts(c, sl)
                if c + 1 < NCH:
                    emit_A(c + 1, 1 - sl)
                for s_ in range(6):
                    if c + 1 < NCH:
                        emit_stage(s_, 1 - sl)
                    if s_ < len(parts):
                        parts[s_]()
            if getattr(g, "nheads", 16) == 1:
                g.rw_dbg = {"Ys": Ys[:], "QR": QR[:], "BK": BK[:], "BKh": BKh[:], "Vtm": Vtm[:], "DC": DC[:], "A": A_sb[:],
                            "Winv": W[0][:], "Sst": Sst[:]}
            if getattr(g, "rw_stop", 9) <= 4:
                continue
            Yt = TM[0:64, 0:32 * 66].rearrange("p (c w) -> p c w", w=66)
            Gt = KK[0:64, 0:2048].rearrange("p (c w) -> p c w", w=64)
            Yc = QR[0:64, 0, 0:2048].rearrange("p (c w) -> p c w", w=64)
            Y2 = QR[0:64, 1, 0:2048].rearrange("p (c w) -> p c w", w=64)
            S.dma("sp", "ld_rw_G", Gt, g.Gs[:, h * 64:(h + 1) * 64].rearrange("(c p) v -> p c v", p=64),
                  reads=[g.t_Gs, t["KK"]], writes=[t["KK"]])
            for c4 in range(8):
                pt, tn = (PT, "PT") if c4 % 2 == 0 else (PT2, "PT2")
                ptv = pt[0:64, 0:264].rearrange("p (a b) -> p a b", b=66)
                for a in range(4):
                    c = c4 * 4 + a
                    S.op("pe", lambda e, a=a, c=c, ptv=ptv: e.matmul(ptv[:, a, :], lhsT=I64, rhs=Ys[:, 0, c, :],
                                                                     start=True, stop=False),
                         reads=[t["Ys"], g.t_ident], writes=[t[tn]])
                    S.op("pe", lambda e, a=a, c=c, ptv=ptv: e.matmul(ptv[:, a, :], lhsT=J64, rhs=Ys[:, 1, 31 - c, :],
                                                                     start=False, stop=True),
                         reads=[t["Ys"], g.t_ident], writes=[t[tn]])
                S.op("act", lambda e, c4=c4, ptv=ptv: e.activation(out=Yt[:, c4 * 4:(c4 + 1) * 4, :], in_=ptv,
                                                                   func=AF.Identity), reads=[t[tn], t["LI"], t["SG"]],
                     writes=[t["TM"]])
            S.op("dve", lambda e: e.tensor_reduce(out=st1[:], in_=Yt[:, :, 0:64], axis=AX.X, op=ALU.add),
                 reads=[t["TM"]], writes=[t["st"]])
            S.op("dve", lambda e: e.tensor_scalar(out=st1[:], in0=st1[:], scalar1=1.0 / 64, scalar2=None, op0=ALU.mult),
                 reads=[t["st"]], writes=[t["st"]])
            S.op("dve", lambda e: e.tensor_tensor(out=Yc, in0=Yt[:, :, 0:64],
                                                  in1=st1[:].unsqueeze(2).to_broadcast([64, 32, 64]), op=ALU.subtract),
                 reads=[t["TM"], t["st"], t["BK"], t["Sst"]], writes=[t["QR"]])
            S.op("act", lambda e: e.activation(out=Y2, in_=Yc, func=AF.Square), reads=[t["QR"]], writes=[t["QR"]])
            S.op("dve", lambda e: e.tensor_reduce(out=st2[:], in_=Y2, axis=AX.X, op=ALU.add),
                 reads=[t["QR"]], writes=[t["st"]])
            S.op("act", lambda e: e.activation(out=st2[:], in_=st2[:], func=AF.Sqrt, bias=epsg[:, 0:1], scale=1.0 / 64),
                 reads=[t["st"], t["c"]], writes=[t["st"]])
            S.op("dve", lambda e: e.reciprocal(out=st2[:], in_=st2[:]), reads=[t["st"]], writes=[t["st"]])
            S.op("dve", lambda e: e.tensor_tensor(out=Yc, in0=Yc, in1=st2[:].unsqueeze(2).to_broadcast([64, 32, 64]),
                                                  op=ALU.mult), reads=[t["st"], t["QR"]], writes=[t["QR"]])
            S.op("dve", lambda e: e.tensor_tensor(out=Yc, in0=Yc, in1=LG[:].unsqueeze(1).to_broadcast([64, 32, 64]),
                                                  op=ALU.mult), reads=[t["LG"], t["QR"]], writes=[t["QR"]])
            S.op("dve", lambda e: e.tensor_tensor(out=Yc, in0=Yc, in1=LB[:].unsqueeze(1).to_broadcast([64, 32, 64]),
                                                  op=ALU.add), reads=[t["LG"], t["QR"]], writes=[t["QR"]])
            S.op("dve", lambda e: e.tensor_tensor(out=Y2, in0=Vtm[:, 4:36, 0:64],
                                                  in1=Yt[:, :, 64:65].to_broadcast([64, 32, 64]), op=ALU.mult),
                 reads=[t["Vtm"], t["TM"], t["QR"]], writes=[t["QR"]])
            S.op("dve", lambda e: e.tensor_tensor(out=Yc, in0=Yc, in1=Y2, op=ALU.add), reads=[t["QR"]], writes=[t["QR"]])
            S.op("dve", lambda e: e.tensor_tensor(out=Yc, in0=Yc, in1=Gt, op=ALU.mult), reads=[t["QR"], t["KK"]],
                 writes=[t["QR"]])
            Of = BK[0:64, 0, 0:2048]
            for c8 in range(4):
                pt, tn = (PT, "PT") if c8 % 2 == 0 else (PT2, "PT2")
                for a in range(8):
                    c = c8 * 8 + a
                    S.op("pe", lambda e, a=a, c=c, pt=pt: e.matmul(pt[0:64, a * 64:(a + 1) * 64], lhsT=Yc[:, c, :], rhs=I64, start=True, stop=True),
                         reads=[t["QR"], g.t_ident], writes=[t[tn]])
                S.op("act", lambda e, c8=c8, pt=pt: e.activation(out=Of[:, c8 * 512:(c8 + 1) * 512], in_=pt[0:64, :],
                                                                 func=AF.Identity), reads=[t[tn], t["A"]], writes=[t["BK"]])
            S.dma("sp", "st_rw", g.ORW[h * 64:(h + 1) * 64, :], Of, reads=[t["BK"]], writes=[g.t_ORW, t["BK"]])
    S.barrier()


def bank_toks(n):
    return [Tok("bank%d" % i, excl=True) for i in range(n)]


def phase_mix(g):
    nc, S = g.nc, g.S
    g.es2 = ExitStack()
    g.mixT = g.es2.enter_context(nc.sbuf_tensor("mixT", [128, 16, T], BF16))
    g.t_mixT = Tok()
    with ExitStack() as es:
        sb = lambda name, shape, dt=F32: es.enter_context(nc.sbuf_tensor("mx_" + name, shape, dt))
        HN, ORWb = sb("HN", [128, 8, T], BF16), sb("ORWb", [128, 8, T], BF16)
        ZCt, SQ = sb("ZCt", [128, 8, 512]), sb("SQ", [128, 8, 512])
        mu, rs, tmp = sb("mu", [128, 512]), sb("rs", [128, 512]), sb("tmp", [128, 512])
        onesM = sb("onesM", [128, 128])
        wf = [sb("wf0", [128, 8, 128]), sb("wf1", [128, 8, 128])]
        wb = [sb("wb0", [128, 8, 128], BF16), sb("wb1", [128, 8, 128], BF16)]
        sz = [sb("sz0", [128, 512]), sb("sz1", [128, 512])]
        t1, t2 = sb("t1", [128, 512]), sb("t2", [128, 512])
        banks = [es.enter_context(nc.psum_tensor("mx_bk%d" % i, [128, 512], F32)) for i in range(4)]
        tb = bank_toks(4)
        tk = {n: Tok(n) for n in ("HN", "ORWb", "ZCt", "SQ", "mu", "rs", "tmp", "c", "wf0", "wf1", "wb0", "wb1",
                                  "sz0", "sz1", "t1", "t2")}
        S.op("pool", lambda e: e.memset(onesM[:], 1.0 / 1024), writes=[tk["c"]])
        zcv = g.ZC.rearrange("(c p) t -> p c t", p=128)
        for tg in range(4):
            tsl = slice(tg * 512, (tg + 1) * 512)
            S.dma("sp", "mx_ld", ZCt[:], zcv[:, :, tsl], reads=[g.t_ZC], writes=[tk["ZCt"]])
            S.op("act", lambda e: e.activation(out=SQ[:], in_=ZCt[:], func=AF.Square), reads=[tk["ZCt"]], writes=[tk["SQ"]])
            for cc in range(8):
                S.op("pe", lambda e, cc=cc: e.matmul(banks[0][:, :], lhsT=onesM[:], rhs=ZCt[:, cc, :], start=(cc == 0),
                                                    stop=(cc == 7)), reads=[tk["ZCt"], tk["c"]], writes=[tb[0]])
            for cc in range(8):
                S.op("pe", lambda e, cc=cc: e.matmul(banks[1][:, :], lhsT=onesM[:], rhs=SQ[:, cc, :], start=(cc == 0),
                                                    stop=(cc == 7)), reads=[tk["SQ"], tk["c"]], writes=[tb[1]])
            S.op("act", lambda e: e.activation(out=mu[:], in_=banks[0][:, :], func=AF.Identity), reads=[tb[0]],
                 writes=[tk["mu"]])
            S.op("dve", lambda e: e.tensor_tensor(out=tmp[:], in0=mu[:], in1=mu[:], op=ALU.mult), reads=[tk["mu"]],
                 writes=[tk["tmp"]])
            S.op("dve", lambda e: e.tensor_tensor(out=rs[:], in0=banks[1][:, :], in1=tmp[:], op=ALU.subtract),
                 reads=[tb[1], tk["tmp"]], writes=[tk["rs"]])
            S.op("act", lambda e: e.activation(out=rs[:], in_=rs[:], func=AF.Sqrt, bias=g.eps_ln[:, 0:1], scale=1.0),
                 reads=[tk["rs"], g.t_const], writes=[tk["rs"]])
            S.op("dve", lambda e: e.reciprocal(out=rs[:], in_=rs[:]), reads=[tk["rs"]], writes=[tk["rs"]])
            for cc in range(8):
                S.op("dve", lambda e, cc=cc: e.tensor_tensor(out=tmp[:], in0=ZCt[:, cc, :], in1=mu[:], op=ALU.subtract),
                     reads=[tk["ZCt"], tk["mu"]], writes=[tk["tmp"]])
                S.op("dve", lambda e: e.tensor_tensor(out=tmp[:], in0=tmp[:], in1=rs[:], op=ALU.mult),
                     reads=[tk["rs"]], writes=[tk["tmp"]])
                S.op("act", lambda e, cc=cc, tsl=tsl: e.activation(out=HN[:, cc, tsl], in_=tmp[:], func=AF.Silu,
                                                                   bias=pvc(g, "clnb_%d" % cc), scale=pvc(g, "clng_%d" % cc)),
                     reads=[tk["tmp"], g.t_pv], writes=[tk["HN"]])
        for kc in range(8):
            for tg in range(4):
                i = (kc * 4 + tg) % 2
                tsl = slice(tg * 512, (tg + 1) * 512)
                S.dma("sp", "mx_ld%d" % i, sz[i][:], g.ORW[kc * 128:(kc + 1) * 128, tsl], reads=[g.t_ORW],
                      writes=[tk["sz%d" % i]])
                S.op("pool", lambda e, i=i, kc=kc, tsl=tsl: e.tensor_copy(out=ORWb[:, kc, tsl], in_=sz[i][:]),
                     reads=[tk["sz%d" % i]], writes=[tk["ORWb"]])
        wcv = g.w_conv_o.rearrange("(kc p) n -> p kc n", p=128)
        wrv = g.w_rwkv_o.rearrange("(kc p) n -> p kc n", p=128)
        for f in range(16):
            fsl = slice(f * 128, (f + 1) * 128)
            for i, wv in enumerate((wcv, wrv)):
                S.dma("sp", "mx_w%d" % i, wf[i][:], wv[:, :, fsl], writes=[tk["wf%d" % i]])
                S.op("pool", lambda e, i=i: e.tensor_copy(out=wb[i][:], in_=wf[i][:]), reads=[tk["wf%d" % i]],
                     writes=[tk["wb%d" % i]])
            for tg in range(4):
                tsl = slice(tg * 512, (tg + 1) * 512)
                ba, bb = (0, 1) if tg % 2 == 0 else (2, 3)
                for kc in range(8):
                    S.op("pe", lambda e, kc=kc, ba=ba, tsl=tsl: e.matmul(banks[ba][:, :], lhsT=wb[0][:, kc, :],
                                                                        rhs=HN[:, kc, tsl], start=(kc == 0), stop=(kc == 7)),
                         reads=[tk["wb0"], tk["HN"]], writes=[tb[ba]])
                for kc in range(8):
                    S.op("pe", lambda e, kc=kc, bb=bb, tsl=tsl: e.matmul(banks[bb][:, :], lhsT=wb[1][:, kc, :],
                                                                        rhs=ORWb[:, kc, tsl], start=(kc == 0), stop=(kc == 7)),
                         reads=[tk["wb1"], tk["ORWb"]], writes=[tb[bb]])
                S.dma("sp", "mx_sz0", sz[0][:], g.SGZ[f * 128:(f + 1) * 128, tsl], reads=[g.t_SGZ], writes=[tk["sz0"]])
                S.dma("sp", "mx_sz1", sz[1][:], g.SGZ[2048 + f * 128:2048 + (f + 1) * 128, tsl], reads=[g.t_SGZ],
                      writes=[tk["sz1"]])
                S.op("dve", lambda e, ba=ba: e.scalar_tensor_tensor(out=t1[:], in0=banks[ba][:, :], scalar=pvc(g, "bco_%d" % f),
                                                                    in1=sz[0][:], op0=ALU.add, op1=ALU.mult),
                     reads=[tb[ba], tk["sz0"], g.t_pv], writes=[tk["t1"]])
                S.op("dve", lambda e, bb=bb: e.tensor_tensor(out=t2[:], in0=banks[bb][:, :], in1=sz[1][:], op=ALU.mult),
                     reads=[tb[bb], tk["sz1"]], writes=[tk["t2"]])
                S.op("pool", lambda e, f=f, tsl=tsl: e.tensor_tensor(out=g.mixT[:, f, tsl], in0=t1[:], in1=t2[:], op=ALU.add),
                     reads=[tk["t1"], tk["t2"]], writes=[g.t_mixT])
    S.barrier()


def phase_out1(g):
    nc, S = g.nc, g.S
    dr = lambda name, shape, dt=F32: nc.dram_tensor(name, list(shape), dt, kind="Internal").ap()
    g.XMID = dr("s_xmid", [T, D])
    g.UT = dr("s_ut", [128, 16, T], BF16)
    g.CT = dr("s_ct", [32, T])
    g.t_XMID, g.t_UT, g.t_CT = Tok(), Tok(), Tok()
    with ExitStack() as es:
        sb = lambda name, shape, dt=F32: es.enter_context(nc.sbuf_tensor("o1_" + name, shape, dt))
        OF = sb("OF", [128, 16, 512])
        wf = [sb("wf0", [128, 16, 128]), sb("wf1", [128, 16, 128])]
        wb = [sb("wb0", [128, 16, 128], BF16), sb("wb1", [128, 16, 128], BF16)]
        xt, un = sb("xt", [128, D]), sb("un", [128, D])
        G1, B1 = sb("G1", [128, D]), sb("B1", [128, D])
        st, mv = sb("st", [128, 4, 6]), sb("mv", [128, 4])
        uTb, uTf = sb("uTb", [128, 16, 128], BF16), sb("uTf", [128, 16, 128])
        WR, BR = sb("WR", [128, 16, 32]), sb("BR", [128, 32])
        lg, ex, m8, sm = sb("lg", [128, 32]), sb("ex", [128, 32]), sb("m8", [128, 8]), sb("sm", [128, 4])
        cTt = sb("cTt", [32, 128])
        banks = [es.enter_context(nc.psum_tensor("o1_bk%d" % i, [128, 512], F32)) for i in range(7)]
        tb = bank_toks(7)
        tk = {n: Tok(n) for n in ("OF", "wf0", "wf1", "wb0", "wb1", "xt", "un", "GB", "st", "uTb", "uTf", "WR", "lg",
                                  "cTt")}
        S.dma("pool", "o1_c", G1[:], g.ln1_g[0:1, :].partition_broadcast(128), writes=[tk["GB"]])
        S.dma("pool", "o1_c", B1[:], g.ln1_b[0:1, :].partition_broadcast(128), writes=[tk["GB"]])
        S.dma("pool", "o1_c", BR[:], g.b_router[0:1, :].partition_broadcast(128), writes=[tk["WR"]])
        S.dma("sp", "o1_c2", WR[:], g.w_router, writes=[tk["WR"]])
        wov = g.w_out.rearrange("(kc p) n -> p kc n", p=128)
        for tg in range(4):
            tsl = slice(tg * 512, (tg + 1) * 512)
            for f in range(16):
                i = f % 2
                S.dma("sp", "o1_w%d" % i, wf[i][:], wov[:, :, f * 128:(f + 1) * 128], writes=[tk["wf%d" % i]])
                S.op("pool", lambda e, i=i: e.tensor_copy(out=wb[i][:], in_=wf[i][:]), reads=[tk["wf%d" % i]],
                     writes=[tk["wb%d" % i]])
                bk = 4 + i
                for kc in range(16):
                    S.op("pe", lambda e, kc=kc, i=i, bk=bk: e.matmul(banks[bk][:, :], lhsT=wb[i][:, kc, :],
                                                                    rhs=g.mixT[:, kc, tsl], start=(kc == 0), stop=(kc == 15)),
                         reads=[tk["wb%d" % i], g.t_mixT], writes=[tb[bk]])
                S.op("dve", lambda e, f=f, bk=bk: e.tensor_scalar(out=OF[:, f, :], in0=banks[bk][:, :],
                                                                  scalar1=pvc(g, "bout_%d" % f), scalar2=g.mod[:, 32 + f, 0:1],
                                                                  op0=ALU.add, op1=ALU.mult),
                     reads=[tb[bk], g.t_pv, g.t_mod], writes=[tk["OF"]])
            for j in range(4):
                tok0 = tg * 512 + j * 128
                S.dma("sp", "o1_x", xt[:], g.x[tok0:tok0 + 128, :], writes=[tk["xt"]])
                for f in range(16):
                    S.op("pe", lambda e, f=f, j=j: e.transpose(
                        banks[f // 4][:, (f % 4) * 128:(f % 4 + 1) * 128], OF[:, f, j * 128:(j + 1) * 128], g.ident[:]),
                         reads=[tk["OF"], g.t_ident], writes=[tb[f // 4]])
                for q in range(4):
                    S.op("dve", lambda e, q=q: e.scalar_tensor_tensor(
                        out=xt[:, q * 512:(q + 1) * 512], in0=xt[:, q * 512:(q + 1) * 512], scalar=ALPHA,
                        in1=banks[q][:, :], op0=ALU.mult, op1=ALU.add), reads=[tb[q]], writes=[tk["xt"]])
                ln_rows(g, xt, tk["xt"], st, mv, tk["st"])
                S.op("dve", lambda e: e.tensor_tensor(out=xt[:], in0=xt[:], in1=G1[:], op=ALU.mult),
                     reads=[tk["GB"]], writes=[tk["xt"]])
                S.op("pool", lambda e: e.tensor_tensor(out=xt[:], in0=xt[:], in1=B1[:], op=ALU.add),
                     reads=[tk["GB"]], writes=[tk["xt"]])
                S.dma("sp", "o1_xm", g.XMID[tok0:tok0 + 128, :], xt[:], reads=[tk["xt"]], writes=[g.t_XMID])
                S.op("pool", lambda e: e.tensor_copy(out=un[:], in_=xt[:]), reads=[tk["xt"]], writes=[tk["un"]])
                ln_rows(g, un, tk["un"], st, mv, tk["st"])
                for f in range(16):
                    S.op("pe", lambda e, f=f: e.transpose(banks[f // 4][:, (f % 4) * 128:(f % 4 + 1) * 128],
                                                          un[:, f * 128:(f + 1) * 128], g.ident[:]),
                         reads=[tk["un"], g.t_ident], writes=[tb[f // 4]])
                for f in range(16):
                    src = banks[f // 4][:, (f % 4) * 128:(f % 4 + 1) * 128]
                    S.op("act", lambda e, f=f, src=src: e.activation(out=uTb[:, f, :], in_=src, func=AF.Identity,
                                                                     bias=g.mod[:, 48 + f, 0:1], scale=g.modp[:, 64 + f, 0:1]),
                         reads=[tb[f // 4], g.t_mod], writes=[tk["uTb"]])
                    S.op("act", lambda e, f=f, src=src: e.activation(out=uTf[:, f, :], in_=src, func=AF.Identity,
                                                                     bias=g.mod[:, 48 + f, 0:1], scale=g.modp[:, 64 + f, 0:1]),
                         reads=[tb[f // 4], g.t_mod], writes=[tk["uTf"]])
                S.dma("sp", "o1_ut", g.UT[:, :, tok0:tok0 + 128], uTb[:], reads=[tk["uTb"]], writes=[g.t_UT])
                for kc in range(16):
                    S.op("pe", lambda e, kc=kc: e.matmul(banks[6][:, 0:32], lhsT=uTf[:, kc, :], rhs=WR[:, kc, :],
                                                        start=(kc == 0), stop=(kc == 15)),
                         reads=[tk["uTf"], tk["WR"]], writes=[tb[6]])
                S.op("dve", lambda e: e.tensor_tensor(out=lg[:], in0=banks[6][:, 0:32], in1=BR[:], op=ALU.add),
                     reads=[tb[6], tk["WR"]], writes=[tk["lg"]])
                S.op("dve", lambda e: e.max(out=m8[:], in_=lg[:]), reads=[tk["lg"]], writes=[tk["lg"]])
                S.op("dve", lambda e: e.tensor_scalar(out=sm[:, 0:1], in0=m8[:, 0:1], scalar1=-1.0, scalar2=None,
                                                      op0=ALU.mult), reads=[tk["lg"]], writes=[tk["lg"]])
                S.op("act", lambda e: e.activation(out=ex[:], in_=lg[:], func=AF.Exp, bias=sm[:, 0:1], scale=1.0),
                     reads=[tk["lg"]], writes=[tk["lg"]])
                S.op("dve", lambda e: e.tensor_scalar(out=lg[:], in0=lg[:], scalar1=m8[:, 3:4], scalar2=None,
                                                      op0=ALU.is_ge), reads=[tk["lg"]], writes=[tk["lg"]])
                S.op("dve", lambda e: e.tensor_tensor(out=ex[:], in0=ex[:], in1=lg[:], op=ALU.mult),
                     reads=[tk["lg"]], writes=[tk["lg"]])
                S.op("dve", lambda e: e.tensor_reduce(out=sm[:, 1:2], in_=ex[:], axis=AX.X, op=ALU.add),
                     reads=[tk["lg"]], writes=[tk["lg"]])
                S.op("dve", lambda e: e.reciprocal(out=sm[:, 2:3], in_=sm[:, 1:2]), reads=[tk["lg"]], writes=[tk["lg"]])
                S.op("dve", lambda e: e.tensor_scalar(out=ex[:], in0=ex[:], scalar1=sm[:, 2:3], scalar2=None,
                                                      op0=ALU.mult), reads=[tk["lg"]], writes=[tk["lg"]])
                S.op("pe", lambda e: e.matmul(banks[6][0:32, 128:256], lhsT=ex[:], rhs=g.ident[:], start=True, stop=True),
                     reads=[tk["lg"], g.t_ident], writes=[tb[6]])
                S.op("act", lambda e: e.activation(out=cTt[:], in_=banks[6][0:32, 128:256], func=AF.Identity),
                     reads=[tb[6]], writes=[tk["cTt"]])
                S.dma("sp", "o1_ct", g.CT[:, tok0:tok0 + 128], cTt[:], reads=[tk["cTt"]], writes=[g.t_CT])
    g.es2.close()
    S.barrier()


def phase_moe(g, out):
    nc, S = g.nc, g.S
    HB = 1024
    for hb in range(2):
        with ExitStack() as es0:
            acc = es0.enter_context(nc.sbuf_tensor("me_acc%d" % hb, [128, 16, HB], F32))
            t_acc = Tok()
            with ExitStack() as es:
                sb = lambda name, shape, dt=F32: es.enter_context(nc.sbuf_tensor("me%d_" % hb + name, shape, dt))
                uTh, hidT = sb("uTh", [128, 16, HB], BF16), sb("hidT", [128, 16, HB], BF16)
                cT, rhe, cb = sb("cT", [32, HB]), sb("rhe", [32, HB]), sb("cb", [128, HB])
                ones32 = sb("ones32", [32, 128])
                wf = [sb("wf0", [128, 16, 128]), sb("wf1", [128, 16, 128])]
                wb = [sb("wb%d" % i, [128, 16, 128], BF16) for i in range(4)]
                t1, t2, t3 = sb("t1", [128, 512]), sb("t2", [128, 512]), sb("t3", [128, 512])
                BGU = sb("BGU", [128, 1024])
                bd = sb("bd", [32, 128])
                banks = [es.enter_context(nc.psum_tensor("me%d_bk%d" % (hb, i), [128, 512], F32)) for i in range(8)]
                tb = bank_toks(8)
                tk = {n: Tok(n) for n in ("uTh", "hidT", "cT", "rhe", "cb", "c", "wf0", "wf1", "wb0", "wb1", "wb2", "wb3",
                                          "t1", "t2", "t3", "BGU", "bd")}
                hsl = slice(hb * HB, (hb + 1) * HB)
                S.dma("sp", "me_ld", uTh[:], g.UT[:, :, hsl], reads=[g.t_UT], writes=[tk["uTh"]])
                S.dma("sp", "me_ld", cT[:], g.CT[:, hsl], reads=[g.t_CT], writes=[tk["cT"]])
                S.dma("sp", "me_ld", BGU[:], g.b_gu, writes=[tk["BGU"]])
                S.op("pool", lambda e: e.memset(ones32[:], 1.0), writes=[tk["c"]])
                st = {"w": 0, "pb": 0}
                for ex_ in range(32):
                    S.op("dve", lambda e, ex_=ex_: e.tensor_scalar(out=rhe[:], in0=cT[:], scalar1=g.ident[0:32, ex_:ex_ + 1],
                                                                   scalar2=None, op0=ALU.mult),
                         reads=[tk["cT"], g.t_ident], writes=[tk["rhe"]])
                    for tg in range(2):
                        S.op("pe", lambda e, tg=tg: e.matmul(banks[6 + tg][:, :], lhsT=ones32[:],
                                                            rhs=rhe[:, tg * 512:(tg + 1) * 512], start=True, stop=True),
                             reads=[tk["rhe"], tk["c"]], writes=[tb[6 + tg]])
                        S.op("act", lambda e, tg=tg: e.activation(out=cb[:, tg * 512:(tg + 1) * 512], in_=banks[6 + tg][:, :],
                                                                  func=AF.Identity), reads=[tb[6 + tg]], writes=[tk["cb"]])
                    wgv = g.w_gu[ex_].rearrange("(kc p) n -> p kc n", p=128)
                    wdv = g.w_dn[ex_].rearrange("(kc p) n -> p kc n", p=128)
                    for j in range(16):
                        pair = (st["w"] % 2) * 2
                        st["w"] += 1
                        for i, c0 in enumerate((j * 128, 2048 + j * 128)):
                            S.dma("sp", "me_w%d" % i, wf[i][:], wgv[:, :, c0:c0 + 128], writes=[tk["wf%d" % i]])
                            S.op("act", lambda e, i=i, pair=pair: e.activation(out=wb[pair + i][:], in_=wf[i][:], func=AF.Identity),
                                 reads=[tk["wf%d" % i]], writes=[tk["wb%d" % (pair + i)]])
                        for tg in range(2):
                            tsl = slice(tg * 512, (tg + 1) * 512)
                            pb = (st["pb"] % 2) * 2
                            st["pb"] += 1
                            for i in range(2):
                                for kc in range(16):
                                    S.op("pe", lambda e, kc=kc, i=i, pb=pb, pair=pair, tsl=tsl: e.matmul(
                                        banks[pb + i][:, :], lhsT=wb[pair + i][:, kc, :], rhs=uTh[:, kc, tsl],
                                        start=(kc == 0), stop=(kc == 15)),
                                         reads=[tk["wb%d" % (pair + i)], tk["uTh"]], writes=[tb[pb + i]])
                            bgc = BGU[:, ex_ * 32 + j:ex_ * 32 + j + 1]
                            buc = BGU[:, ex_ * 32 + 16 + j:ex_ * 32 + 16 + j + 1]
                            S.op("dve", lambda e, pb=pb, bgc=bgc: e.tensor_scalar(out=t1[:], in0=banks[pb][:, :], scalar1=bgc,
                                                                                  scalar2=7.0, op0=ALU.add, op1=ALU.min),
                                 reads=[tb[pb], tk["BGU"]], writes=[tk["t1"]])
                            S.op("act", lambda e: e.activation(out=t2[:], in_=t1[:], func=AF.Sigmoid, scale=1.702),
                                 reads=[tk["t1"]], writes=[tk["t2"]])
                            S.op("dve", lambda e, pb=pb, buc=buc: e.tensor_scalar(out=t3[:], in0=banks[pb + 1][:, :],
                                                                                  scalar1=buc, scalar2=-7.0, op0=ALU.add,
                                                                                  op1=ALU.max),
                                 reads=[tb[pb + 1], tk["BGU"]], writes=[tk["t3"]])
                            S.op("dve", lambda e: e.tensor_scalar(out=t3[:], in0=t3[:], scalar1=7.0, scalar2=1.0, op0=ALU.min,
                                                                  op1=ALU.add), reads=[tk["t3"]], writes=[tk["t3"]])
                            S.op("dve", lambda e: e.tensor_tensor(out=t1[:], in0=t1[:], in1=t2[:], op=ALU.mult),
                                 reads=[tk["t2"]], writes=[tk["t1"]])
                            S.op("pool", lambda e: e.tensor_tensor(out=t1[:], in0=t1[:], in1=t3[:], op=ALU.mult),
                                 reads=[tk["t3"]], writes=[tk["t1"]])
                            S.op("dve", lambda e, j=j, tsl=tsl: e.tensor_tensor(out=hidT[:, j, tsl], in0=t1[:], in1=cb[:, tsl],
                                                                                op=ALU.mult),
                                 reads=[tk["t1"], tk["cb"]], writes=[tk["hidT"]])
                    for f in range(16):
                        wi = st["w"] % 4
                        st["w"] += 1
                        i = f % 2
                        S.dma("sp", "me_w%d" % i, wf[i][:], wdv[:, :, f * 128:(f + 1) * 128], writes=[tk["wf%d" % i]])
                        S.op("act", lambda e, i=i, wi=wi: e.activation(out=wb[wi][:], in_=wf[i][:], func=AF.Identity),
                             reads=[tk["wf%d" % i]], writes=[tk["wb%d" % wi]])
                        if ex_ == 0:
                            S.dma("sp", "me_bd", bd[:], g.b_dn[:, f * 128:(f + 1) * 128], writes=[tk["bd"]])
                        for tg in range(2):
                            tsl = slice(tg * 512, (tg + 1) * 512)
                            bk = 4 + (f * 2 + tg) % 2
                            for kc in range(16):
                                S.op("pe", lambda e, kc=kc, bk=bk, wi=wi, tsl=tsl: e.matmul(
                                    banks[bk][:, :], lhsT=wb[wi][:, kc, :], rhs=hidT[:, kc, tsl], start=(kc == 0),
                                    stop=(kc == 15)), reads=[tk["wb%d" % wi], tk["hidT"]], writes=[tb[bk]])
                            if ex_ == 0:
                                S.op("pe", lambda e, tg=tg, tsl=tsl: e.matmul(banks[6 + tg][:, :], lhsT=bd[:], rhs=cT[:, tsl],
                                                                              start=True, stop=True),
                                     reads=[tk["bd"], tk["cT"]], writes=[tb[6 + tg]])
                                S.op("act", lambda e, f=f, tg=tg, tsl=tsl: e.activation(out=acc[:, f, tsl], in_=banks[6 + tg][:, :],
                                                                                      func=AF.Identity),
                                     reads=[tb[6 + tg]], writes=[t_acc])
                            S.op("dve", lambda e, f=f, bk=bk, tsl=tsl: e.tensor_tensor(out=acc[:, f, tsl], in0=banks[bk][:, :],
                                                                                      in1=acc[:, f, tsl], op=ALU.add),
                                 reads=[tb[bk]], writes=[t_acc])
            S.barrier()
            with ExitStack() as es:
                sb = lambda name, shape, dt=F32: es.enter_context(nc.sbuf_tensor("fin%d_" % hb + name, shape, dt))
                xt = [sb("xt0", [128, D]), sb("xt1", [128, D])]
                G2, B2 = sb("G2", [128, D]), sb("B2", [128, D])
                st_, mv = sb("st", [128, 4, 6]), sb("mv", [128, 4])
                banks = [es.enter_context(nc.psum_tensor("fin%d_bk%d" % (hb, i), [128, 512], F32)) for i in range(4)]
                tb = bank_toks(4)
                tk = {n: Tok(n) for n in ("xt0", "xt1", "GB", "st")}
                t_out = Tok()
                S.dma("pool", "fin_c", G2[:], g.ln2_g[0:1, :].partition_broadcast(128), writes=[tk["GB"]])
                S.dma("pool", "fin_c", B2[:], g.ln2_b[0:1, :].partition_broadcast(128), writes=[tk["GB"]])
                for f in range(16):
                    S.op("dve", lambda e, f=f: e.tensor_scalar(out=acc[:, f, :], in0=acc[:, f, :], scalar1=g.mod[:, 80 + f, 0:1],
                                                               scalar2=None, op0=ALU.mult), reads=[g.t_mod], writes=[t_acc])
                for j in range(HB // 128):
                    b = j % 2
                    tok0 = hb * HB + j * 128
                    S.dma("sp", "fin_x%d" % b, xt[b][:], g.XMID[tok0:tok0 + 128, :], reads=[g.t_XMID], writes=[tk["xt%d" % b]])
                    for f in range(16):
                        S.op("pe", lambda e, f=f, j=j: e.transpose(
                            banks[f // 4][:, (f % 4) * 128:(f % 4 + 1) * 128], acc[:, f, j * 128:(j + 1) * 128], g.ident[:]),
                             reads=[t_acc, g.t_ident], writes=[tb[f // 4]])
                    for q in range(4):
                        S.op("dve", lambda e, q=q, b=b: e.scalar_tensor_tensor(
                            out=xt[b][:, q * 512:(q + 1) * 512], in0=xt[b][:, q * 512:(q + 1) * 512], scalar=ALPHA,
                            in1=banks[q][:, :], op0=ALU.mult, op1=ALU.add), reads=[tb[q]], writes=[tk["xt%d" % b]])
                    ln_rows(g, xt[b], tk["xt%d" % b], st_, mv, tk["st"])
                    S.op("dve", lambda e, b=b: e.tensor_tensor(out=xt[b][:], in0=xt[b][:], in1=G2[:], op=ALU.mult),
                         reads=[tk["GB"]], writes=[tk["xt%d" % b]])
                    S.op("pool", lambda e, b=b: e.tensor_tensor(out=xt[b][:], in0=xt[b][:], in1=B2[:], op=ALU.add),
                         reads=[tk["GB"]], writes=[tk["xt%d" % b]])
                    S.dma("sp", "fin_o%d" % b, out[tok0:tok0 + 128, :], xt[b][:], reads=[tk["xt%d" % b]],
                          writes=[t_out, tk["xt%d" % b]])
                g.out_toks.append(t_out)
            S.barrier()


def phase_placeholder_out(g, out):
    nc, S = g.nc, g.S
    with ExitStack() as es:
        bufs = [es.enter_context(nc.sbuf_tensor("po%d" % i, [128, D], F32)) for i in range(2)]
        toks = [Tok(), Tok()]
        t_out = Tok()
        for tt in range(NT):
            b = tt % 2
            S.dma("sp", "po_ld%d" % b, bufs[b][:], g.x[tt * 128:(tt + 1) * 128, :], writes=[toks[b]])
            S.dma("sp", "po_st%d" % b, out[tt * 128:(tt + 1) * 128, :], bufs[b][:], reads=[toks[b]], writes=[t_out])
        S.finish([t_out])


def make_in_maps(inp, ncores=8):
    maps = []
    for b in range(ncores):
        m = {}
        m["x"] = np.ascontiguousarray(inp["x"][b])
        m["ctx"] = np.ascontiguousarray(inp["ctx"][b])
        cc = np.stack([inp["c"][b], inp["c_ctx"]], axis=-1)
        m["cc"] = np.ascontiguousarray(cc.reshape(16, 128, 2).transpose(1, 0, 2))
        m["w_ada"] = np.ascontiguousarray(inp["w_ada"][0])
        m["b_ada"] = np.ascontiguousarray(inp["b_ada"][0].reshape(96, 128).T)
        m["w_in"] = np.ascontiguousarray(inp["w_in"][0])
        m["pv"] = make_pv(inp)
        m["g2"] = np.ascontiguousarray(inp["g2"][0])
        m["lnx_g"] = np.ascontiguousarray(inp["lnx_g"][0].reshape(16, 64))
        m["lnx_b"] = np.ascontiguousarray(inp["lnx_b"][0].reshape(16, 64))
        m["w_conv_o"] = np.ascontiguousarray(inp["w_conv_o"][0])
        m["w_rwkv_o"] = np.ascontiguousarray(inp["w_rwkv_o"][0])
        m["w_out"] = np.ascontiguousarray(inp["w_out"][0])
        for nm in ("ln1_g", "ln1_b", "ln2_g", "ln2_b", "b_router"):
            m[nm] = np.ascontiguousarray(inp[nm][0][None, :])
        m["w_router"] = np.ascontiguousarray(inp["w_router"][0].reshape(16, 128, 32).transpose(1, 0, 2))
        m["w_gu"] = np.ascontiguousarray(inp["w_gate_up"][0])
        m["b_gu"] = np.ascontiguousarray(inp["b_gate_up"][0].reshape(32, 32, 128).transpose(2, 0, 1).reshape(128, 1024))
        m["w_dn"] = np.ascontiguousarray(inp["w_down"][0])
        m["b_dn"] = np.ascontiguousarray(inp["b_down"][0])
        for nm, key in (("w2bd", "w2"), ("a2bd", "a2")):
            bd = np.zeros((16, 128, 128), np.float32)
            for h in range(16):
                for d in range(2):
                    bd[h, d * 64:(d + 1) * 64, d * 64:(d + 1) * 64] = inp[key][0, d, :, h * 64:(h + 1) * 64]
            m[nm] = bd
        maps.append(m)
    return maps


def kernel(**inputs):
    inp = {k: np.asarray(v) for k, v in inputs.items()}
    nc = build_program()
    in_maps = make_in_maps(inp)
    res = run_bass_kernel_spmd(nc, in_maps, core_ids=list(range(8)))
    return np.stack([r["out"] for r in res.results], axis=0)
```

```python
from contextlib import ExitStack
import numpy as np
import concourse.bass as bass
import concourse.mybir as mybir
from concourse.bass_utils import run_bass_kernel_spmd

F32 = mybir.dt.float32
BF16 = mybir.dt.bfloat16
AF = mybir.ActivationFunctionType
ALU = mybir.AluOpType
AX = mybir.AxisListType

D = 2048
T = 2048
TC = 256
NT = T // 128
NTC = TC // 128
TT = T + TC
P_IN = 9632
LN_EPS = 1e-5
ALPHA = 2.0 ** 0.25


class Tok:
    __slots__ = ("w", "r", "name", "excl")

    def __init__(self, name="", excl=False):
        self.w = {}
        self.r = {}
        self.name = name
        self.excl = excl


class Sched:
    EPOCH = 30000
    DMA_EPOCH = 1800

    def __init__(self, nc):
        self.nc = nc
        self.eng = {"pe": nc.tensor, "act": nc.scalar, "dve": nc.vector, "pool": nc.gpsimd, "sp": nc.sync}
        self.sem = {}
        self.cnt = {}
        self.seen = {e: {} for e in self.eng}
        self.nsem = 0
        self.dsem = {}
        self.dma_issued = {}
        self.ninst = 0
        for e in self.eng:
            self._new_sem(e)

    def _new_sem(self, e):
        self.sem[e] = self.nc.alloc_semaphore(name="se_%s_%d" % (e, self.nsem))
        self.nsem += 1
        self.cnt[e] = 0

    def _wait(self, e, deps):
        seen = self.seen[e]
        best = {}
        own = self.sem[e].num
        for (semh, val) in deps:
            k = semh.num
            if e == "pe" and k == own:
                continue
            if k in self.dma_issued:
                val = self.dma_issued[k]
            if seen.get(k, 0) >= val:
                continue
            if k not in best or best[k][1] < val:
                best[k] = (semh, val)
        for k, (semh, val) in best.items():
            self.eng[e].wait_ge(semh, val)
            seen[k] = val
            self.ninst += 1

    def _deps(self, reads, writes):
        deps = []
        for t in reads:
            deps.extend(t.w.values())
        for t in writes:
            deps.extend(t.w.values())
            deps.extend(t.r.values())
        return deps

    def pe_rg(self, rg):
        self.next_rg = rg

    def op(self, e, fn, reads=(), writes=()):
        if e == "pe":
            rg = getattr(self, "next_rg", 0)
            self.next_rg = 0
            if rg != getattr(self, "last_rg", 0) and self.cnt["pe"] > 0:
                self.eng["pe"].wait_ge(self.sem["pe"], self.cnt["pe"])
                self.ninst += 1
            self.last_rg = rg
        ex = [t for t in reads if t.excl]
        if ex:
            reads = [t for t in reads if not t.excl]
            writes = list(writes) + ex
        self._wait(e, self._deps(reads, writes))
        if self.cnt[e] >= self.EPOCH:
            self._new_sem(e)
        inst = fn(self.eng[e])
        self.cnt[e] += 1
        self.ninst += 1
        inst.then_inc(self.sem[e], 1)
        rec = (self.sem[e], self.cnt[e])
        k = self.sem[e].num
        for t in reads:
            t.r[k] = rec
        for t in writes:
            t.w[k] = rec
        return inst

    def dma(self, q, sname, out, in_, reads=(), writes=(), **kw):
        self._wait(q, self._deps(reads, writes))
        ent = self.dsem.get(sname)
        if ent is None or self.dma_issued[ent.num] >= 16 * self.DMA_EPOCH:
            ent = self.nc.alloc_semaphore(name="sd_%s_%d" % (sname, self.nsem))
            self.nsem += 1
            self.dsem[sname] = ent
            self.dma_issued[ent.num] = 0
        inst = self.eng[q].dma_start(out=out, in_=in_, **kw)
        self.ninst += 1
        self.dma_issued[ent.num] += 16
        inst.then_inc(ent, 16)
        rec = (ent, self.dma_issued[ent.num])
        for t in reads:
            t.r[ent.num] = rec
        for t in writes:
            t.w[ent.num] = rec
        return inst

    def barrier(self):
        for e in self.eng:
            deps = [(self.sem[o], self.cnt[o]) for o in self.eng if o != e and self.cnt[o] > 0]
            for k, ent in self.dsem.items():
                deps.append((ent, self.dma_issued[ent.num]))
            self._wait(e, deps)

    def finish(self, toks):
        deps = []
        for t in toks:
            deps.extend(t.w.values())
        self._wait("sp", deps)


class Ctx:
    pass


def build_program(debug=None):
    nc = bass.Bass("TRN2", target_bir_lowering=False)
    S = Sched(nc)
    g = Ctx()
    g.nc = nc
    g.S = S
    g.debug = debug
    if debug is not None and debug.startswith("rwkv1"):
        g.nheads = 1
        g.rw_stop = int(debug[5:6] or 9)
        g.no_bonus = debug.endswith("nb")
        debug = "rwkv"

    def dram_in(name, shape, dt=F32):
        return nc.dram_tensor(name, list(shape), dt, kind="ExternalInput").ap()

    def dram_out(name, shape, dt=F32):
        return nc.dram_tensor(name, list(shape), dt, kind="ExternalOutput").ap()

    g.x = dram_in("x", [T, D])
    g.ctx = dram_in("ctx", [TC, D])
    g.cc = dram_in("cc", [128, 16, 2])
    g.w_ada = dram_in("w_ada", [D, 6 * D])
    g.b_ada = dram_in("b_ada", [128, 96])
    g.w_in = dram_in("w_in", [D, P_IN])
    g.pv_in = dram_in("pv", [128, NPV])
    g.g2 = dram_in("g2", [160, 1024])
    g.w2bd = dram_in("w2bd", [16, 128, 128])
    g.a2bd = dram_in("a2bd", [16, 128, 128])
    g.lnx_g = dram_in("lnx_g", [16, 64])
    g.lnx_b = dram_in("lnx_b", [16, 64])
    g.w_conv_o = dram_in("w_conv_o", [1024, D])
    g.w_rwkv_o = dram_in("w_rwkv_o", [1024, D])
    g.w_out = dram_in("w_out", [D, D])
    g.ln1_g = dram_in("ln1_g", [1, D])
    g.ln1_b = dram_in("ln1_b", [1, D])
    g.ln2_g = dram_in("ln2_g", [1, D])
    g.ln2_b = dram_in("ln2_b", [1, D])
    g.w_router = dram_in("w_router", [128, 16, 32])
    g.b_router = dram_in("b_router", [1, 32])
    g.w_gu = dram_in("w_gu", [32, D, 2 * D])
    g.b_gu = dram_in("b_gu", [128, 1024])
    g.w_dn = dram_in("w_dn", [32, D, D])
    g.b_dn = dram_in("b_dn", [32, D])

    g.ident = nc.alloc_sbuf_tensor("ident", [128, 128], F32)
    g.anti = nc.alloc_sbuf_tensor("anti", [128, 128], F32)
    g.eps_ln = nc.alloc_sbuf_tensor("eps_ln", [128, 1], F32)
    g.t_const = Tok()
    S.op("pool", lambda e: e.memset(g.eps_ln[:], LN_EPS), writes=[g.t_const])
    g.t_ident = Tok()
    make_identity(g, g.ident, g.anti, g.t_ident)
    g.pv = nc.alloc_sbuf_tensor("pv_sb", [128, NPV], F32)
    g.t_pv = Tok()
    S.dma("sp", "ld_small", g.pv[:], g.pv_in, writes=[g.t_pv])
    phase_mod(g)
    phase_ln1(g, False)
    if debug == "ln1":
        o = dram_out("dbg_xmT", [128, 16, TT], BF16)

        tk = Tok()
        S.dma("sp", "dbg", o, g.xmT[:], reads=[g.t_xmT], writes=[tk])
        o2 = dram_out("dbg_mod", [128, 96, 2], F32)
        S.dma("sp", "dbg", o2, g.mod[:], reads=[g.t_mod], writes=[tk])
        S.finish([tk])
        return nc
    phase_proj(g, False)
    phase_ln1(g, True)
    phase_proj(g, True)
    g.es1.close()
    if debug == "proj":
        tk = Tok()
        for nm, src, tok in (("RKV", g.RKV, g.t_RKV), ("SGs", g.SGs, g.t_SGs), ("AGs", g.AGs, g.t_AGs),
                             ("Gs", g.Gs, g.t_Gs), ("ZC", g.ZC, g.t_ZC), ("SGZ", g.SGZ, g.t_SGZ)):
            o = dram_out("dbg_" + nm, list(src.shape), F32)
            S.dma("sp", "dbg", o, src, reads=[tok], writes=[tk])
        S.finish([tk])
        return nc
    phase_rwkv(g)
    if debug is None or debug == "full1":
        phase_mix(g)
        phase_out1(g)
        g.out_toks = []
        phase_moe(g, dram_out("out", [T, D], F32))
        S.finish(g.out_toks)
        return nc
    if debug == "rwkv":
        tk = Tok()
        o = dram_out("dbg_ORW", [1024, T], F32)
        S.dma("sp", "dbg", o, g.ORW, reads=[g.t_ORW], writes=[tk])
        for nm, buf in g.rw_dbg.items():
            o = dram_out("dbg_" + nm, list(buf.shape), F32)
            S.dma("sp", "dbg", o, buf, reads=[], writes=[tk])
        S.finish([tk])
        return nc
    return nc


def phase_mod(g):
    nc, S = g.nc, g.S
    g.mod = nc.alloc_sbuf_tensor("mod", [128, 96, 2], F32)
    g.t_mod = Tok("mod")
    g.modp = nc.alloc_sbuf_tensor("modp", [128, 96, 2], F32)
    sc = nc.alloc_sbuf_tensor("sc", [128, 16, 2], F32)
    t_sc = Tok()
    bada = nc.alloc_sbuf_tensor("bada", [128, 96], F32)
    t_b = Tok()
    S.dma("sp", "ld_small", sc[:], g.cc, writes=[t_sc])
    S.dma("sp", "ld_small", bada[:], g.b_ada, writes=[t_b])
    S.op("act", lambda e: e.activation(out=sc[:], in_=sc[:], func=AF.Silu), reads=[t_sc], writes=[t_sc])
    NG = 24
    wsrc = g.w_ada.rearrange("(kc p) n -> p kc n", p=128)
    with nc.sbuf_tensor("wada0", [128, 16, 512], F32) as wb0, nc.sbuf_tensor("wada1", [128, 16, 512], F32) as wb1, \
            nc.psum_tensor("ps_mod", [128, 96, 2], F32) as ps:
        wb = [wb0, wb1]
        t_wb = [Tok(), Tok()]
        t_ps = Tok(excl=True)
        for gi in range(NG):
            b = gi % 2
            S.dma("sp" if gi % 2 == 0 else "pool", "ld_wada%d" % b, wb[b][:], wsrc[:, :, gi * 512:(gi + 1) * 512],
                  writes=[t_wb[b]])
            for j in range(4):
                fo = gi * 4 + j
                for kc in range(16):
                    S.op("pe", lambda e, kc=kc, j=j, fo=fo, b=b: e.matmul(
                        ps[:, fo, :], lhsT=wb[b][:, kc, j * 128:(j + 1) * 128], rhs=sc[:, kc, :],
                        start=(kc == 0), stop=(kc == 15)),
                         reads=[t_wb[b], t_sc], writes=[t_ps])
        for i in range(2):
            S.op("dve", lambda e, i=i: e.tensor_tensor(out=g.mod[:, :, i], in0=ps[:, :, i], in1=bada[:], op=ALU.add),
                 reads=[t_ps, t_b], writes=[g.t_mod])
    S.op("dve", lambda e: e.tensor_scalar(out=g.modp[:], in0=g.mod[:], scalar1=1.0, scalar2=None, op0=ALU.add),
         reads=[g.t_mod], writes=[g.t_mod])
    S.barrier()


def phase_ln1(g, rev_pass):
    nc, S = g.nc, g.S
    if not rev_pass:
        g.es1 = ExitStack()
        g.xmT = g.es1.enter_context(nc.sbuf_tensor("xmT", [128, 16, TT], BF16))
        g.t_xmT = Tok("xmT")
    sfx = "r" if rev_pass else "f"
    with ExitStack() as es:
        sb = lambda name, shape, dt=F32: es.enter_context(nc.sbuf_tensor(name + sfx, shape, dt))
        psb = lambda name, shape, dt=F32: es.enter_context(nc.psum_tensor(name + sfx, shape, dt))
        xt = [sb("xt0", [128, D]), sb("xt1", [128, D])]
        st, mv = sb("ln_st", [128, 4, 6]), sb("ln_mv", [128, 4])
        pst = [psb("ps_tr%d" % i, [128, 4, 128]) for i in range(4)]
        t_xt = [Tok(), Tok()]
        t_st = Tok()
        t_pst = [Tok(excl=True) for _ in range(4)]
        for tt in range(NT + NTC):
            b = tt % 2
            if tt < NT:
                src = g.x[tt * 128:(tt + 1) * 128, :]
                col = 0
                pos = TC + (NT - 1 - tt) * 128 if rev_pass else TC + tt * 128
            else:
                src = g.ctx[(tt - NT) * 128:(tt - NT + 1) * 128, :]
                col = 1
                pos = (NTC - 1 - (tt - NT)) * 128 if rev_pass else (tt - NT) * 128
            S.dma("sp", "ld_x%d" % b, xt[b][:], src, writes=[t_xt[b]])
            ln_rows(g, xt[b], t_xt[b], st, mv, t_st)
            for fc in range(16):
                pb = fc // 4
                if rev_pass:
                    S.op("pe", lambda e, fc=fc, pb=pb, b=b: e.matmul(
                        pst[pb][:, fc % 4, :], lhsT=xt[b][:, fc * 128:(fc + 1) * 128], rhs=g.anti[:],
                        start=True, stop=True), reads=[t_xt[b], g.t_ident], writes=[t_pst[pb]])
                else:
                    S.op("pe", lambda e, fc=fc, pb=pb, b=b: e.transpose(
                        pst[pb][:, fc % 4, :], xt[b][:, fc * 128:(fc + 1) * 128], g.ident[:]),
                         reads=[t_xt[b], g.t_ident], writes=[t_pst[pb]])
                if fc % 4 == 3:
                    for f2 in range(fc - 3, fc + 1):
                        S.op("act", lambda e, f2=f2, pb=pb, pos=pos, col=col: e.activation(
                            out=g.xmT[:, f2, pos:pos + 128], in_=pst[pb][:, f2 % 4, :], func=AF.Identity,
                            bias=g.mod[:, f2, col:col + 1], scale=g.modp[:, 16 + f2, col:col + 1]),
                             reads=[t_pst[pb], g.t_mod], writes=[g.t_xmT])
    S.barrier()


def ln_rows(g, xt, t_x, st, mv, t_st, n=4):
    S = g.S
    for q in range(n):
        S.op("dve", lambda e, q=q: e.bn_stats(out=st[:, q, :], in_=xt[:, q * 512:(q + 1) * 512]),
             reads=[t_x], writes=[t_st])
    S.op("dve", lambda e: e.bn_aggr(out=mv[:, 0:2], in_=st[:, 0:n, :].rearrange("p a b -> p (a b)")),
         reads=[t_st], writes=[t_st])
    S.op("act", lambda e: e.activation(out=mv[:, 3:4], in_=mv[:, 1:2], func=AF.Sqrt, bias=g.eps_ln[:, 0:1],
                                       scale=1.0), reads=[t_st, g.t_const], writes=[t_st])
    S.op("dve", lambda e: e.reciprocal(out=mv[:, 2:3], in_=mv[:, 3:4]), reads=[t_st], writes=[t_st])
    S.op("dve", lambda e: e.tensor_scalar(out=xt[:, 0:n * 512], in0=xt[:, 0:n * 512], scalar1=mv[:, 0:1],
                                          scalar2=mv[:, 2:3], op0=ALU.subtract, op1=ALU.mult),
         reads=[t_st, t_x], writes=[t_x])


def make_identity(g, ident, anti, tok):
    nc, S = g.nc, g.S
    S.op("pool", lambda e: e.memset(ident[:], 0.0), writes=[tok])
    S.op("pool", lambda e: e.memset(anti[:], 0.0), writes=[tok])
    S.op("pool", lambda e: e.affine_select(out=ident[:], in_=ident[:], pattern=[[-1, 128]],
                                           compare_op=ALU.not_equal, fill=1.0, base=0, channel_multiplier=1),
         reads=[tok], writes=[tok])
    S.op("pool", lambda e: e.affine_select(out=anti[:], in_=anti[:], pattern=[[1, 128]],
                                           compare_op=ALU.not_equal, fill=1.0, base=-127, channel_multiplier=1),
         reads=[tok], writes=[tok])


def pv_layout():
    names = []
    for q in range(3):
        for h in range(16):
            names += ["b_%d_%d" % (q, h), "cp_%d_%d" % (q, h), "cn_%d_%d" % (q, h)]
    for nm in ("wd", "ad", "gd1", "gd2"):
        names += ["b_" + nm, "cp_" + nm, "cn_" + nm]
    for h in range(16):
        names += ["w0_%d" % h, "a0_%d" % h, "kk_%d" % h, "ka_%d" % h, "rk_%d" % h]
    for c in range(8):
        names += ["cba_%d" % c, "cbg_%d" % c, "convb_%d" % c, "clng_%d" % c, "clnb_%d" % c]
        names += ["cw_%d_%d" % (c, j) for j in range(31)]
    for c in range(32):
        names += ["bzg_%d" % c]
    for c in range(16):
        names += ["bco_%d" % c, "bout_%d" % c]
    return {n: i for i, n in enumerate(names)}


PVL = pv_layout()
NPV = len(PVL)


def make_pv(inp):
    pv = np.zeros((128, NPV), np.float32)
    b_in = inp["b_in"][0]
    mu = inp["shift_mu"][0]

    def dup(v):
        return np.concatenate([v, v])
    for q in range(3):
        for h in range(16):
            c0 = 2048 + q * 1024 + h * 64
            zc = c0 - 2048
            pv[:, PVL["b_%d_%d" % (q, h)]] = dup(b_in[c0:c0 + 64])
            pv[:, PVL["cp_%d_%d" % (q, h)]] = np.concatenate([mu[0, zc:zc + 64], mu[1, zc:zc + 64]])
            pv[:, PVL["cn_%d_%d" % (q, h)]] = np.concatenate([mu[1, zc:zc + 64], mu[0, zc:zc + 64]])
    for nm, c0 in (("wd", 5120), ("ad", 5248)):
        zc = c0 - 2048
        pv[:, PVL["b_" + nm]] = b_in[c0:c0 + 128]
        pv[:, PVL["cp_" + nm]] = np.concatenate([mu[0, zc:zc + 64], mu[1, zc + 64:zc + 128]])
        pv[:, PVL["cn_" + nm]] = np.concatenate([mu[1, zc:zc + 64], mu[0, zc + 64:zc + 128]])
    pv[:, PVL["b_gd1"]] = b_in[5376:5504]
    pv[:, PVL["cp_gd1"]] = mu[0, 5376 - 2048:5504 - 2048]
    pv[:, PVL["cn_gd1"]] = mu[1, 5376 - 2048:5504 - 2048]
    pv[:32, PVL["b_gd2"]] = b_in[5504:5536]
    pv[:32, PVL["cp_gd2"]] = mu[0, 5504 - 2048:5536 - 2048]
    pv[:32, PVL["cn_gd2"]] = mu[1, 5504 - 2048:5536 - 2048]
    for h in range(16):
        sl = slice(h * 64, h * 64 + 64)
        pv[:, PVL["w0_%d" % h]] = np.concatenate([inp["w0"][0, 0, sl], inp["w0"][0, 1, sl]])
        pv[:, PVL["a0_%d" % h]] = np.concatenate([inp["a0"][0, 0, sl], inp["a0"][0, 1, sl]])
        pv[:, PVL["kk_%d" % h]] = dup(inp["k_k"][0, sl])
        pv[:, PVL["ka_%d" % h]] = dup(inp["k_a"][0, sl])
        pv[:, PVL["rk_%d" % h]] = dup(inp["r_k"][0, h])
    for c in range(8):
        sl = slice(c * 128, c * 128 + 128)
        pv[:, PVL["cba_%d" % c]] = b_in[sl]
        pv[:, PVL["cbg_%d" % c]] = b_in[1024 + c * 128:1024 + c * 128 + 128]
        pv[:, PVL["convb_%d" % c]] = inp["conv_b"][0, sl]
        pv[:, PVL["clng_%d" % c]] = inp["conv_ln_g"][0, sl]
        pv[:, PVL["clnb_%d" % c]] = inp["conv_ln_b"][0, sl]
        for j in range(31):
            pv[:, PVL["cw_%d_%d" % (c, j)]] = inp["conv_w"][0, j, sl]
    for c in range(32):
        pv[:, PVL["bzg_%d" % c]] = b_in[5536 + c * 128:5536 + c * 128 + 128]
    for c in range(16):
        pv[:, PVL["bco_%d" % c]] = inp["b_conv_o"][0, c * 128:c * 128 + 128]
        pv[:, PVL["bout_%d" % c]] = inp["b_out"][0, c * 128:c * 128 + 128]
    return pv


def pvc(g, name):
    i = PVL[name]
    return g.pv[:, i:i + 1]


TG_ALL = [(0, 512), (512, 512), (1024, 512), (1536, 512), (2048, 256)]
TG_LAT = [(TC + i * 512, 512) for i in range(4)]


def phase_proj(g, rev_pass):
    nc, S = g.nc, g.S
    if not rev_pass:
        dr = lambda name, shape, dt=F32: nc.dram_tensor(name, list(shape), dt, kind="Internal").ap()
        g.RKV = dr("s_rkv", [3, 16, 128, TT])
        g.SGs = dr("s_sg", [16, 128, TT])
        g.AGs = dr("s_ag", [16, 128, TT])
        g.Gs = dr("s_g", [T, 1024])
        g.ZC = dr("s_zc", [1024, T])
        g.SGZ = dr("s_sgz", [4096, T])
        g.t_RKV, g.t_SGs, g.t_AGs, g.t_Gs, g.t_ZC, g.t_SGZ = (Tok() for _ in range(6))
        g.wdt = g.es1.enter_context(nc.sbuf_tensor("wdt", [128, TT], F32))
        g.ads = g.es1.enter_context(nc.sbuf_tensor("ads", [128, TT], F32))
        g.t_wdt, g.t_ads = Tok(), Tok()
    sfx = "r" if rev_pass else "f"
    wsrc = g.w_in.rearrange("(kc p) n -> p kc n", p=128)
    with ExitStack() as es:
        sb = lambda name, shape, dt=F32: es.enter_context(nc.sbuf_tensor(name + sfx, shape, dt))
        psb = lambda name, shape, dt=F32: es.enter_context(nc.psum_tensor(name + sfx, shape, dt))
        wf = [sb("wf0", [128, 16, 128]), sb("wf1", [128, 16, 128])]
        wb = [sb("wb0", [128, 16, 128], BF16), sb("wb1", [128, 16, 128], BF16)]
        zraw = sb("zraw", [128, TT])
        zs = [sb("zs0", [128, TT]), sb("zs1", [128, TT])]
        c0t = sb("c0t", [128, 1])
        lw2 = sb("lw2", [128, 2, 128])
        pp = [psb("pp%d" % i, [128, 512]) for i in range(4)]
        t_wf = [Tok(), Tok()]
        t_wb = [Tok(), Tok()]
        t_pp = [Tok(excl=True) for _ in range(4)]
        t_zraw, t_c0 = Tok(), Tok()
        t_zs = [Tok(), Tok()]
        st = {"wi": 0, "pi": 0, "zi": 0}

        def load_w(col0, M, dupl=False):
            i = st["wi"] % 2
            st["wi"] += 1
            S.dma("sp", "ld_w%d" % i, wf[i][:, :, 0:M], wsrc[:, :, col0:col0 + M], writes=[t_wf[i]])
            S.op("pool", lambda e: e.tensor_copy(out=wb[i][:, :, 0:M], in_=wf[i][:, :, 0:M]),
                 reads=[t_wf[i]], writes=[t_wb[i]])
            if dupl:
                S.op("pool", lambda e: e.tensor_copy(out=wb[i][:, :, M:2 * M], in_=wf[i][:, :, 0:M]),
                     reads=[t_wf[i]], writes=[t_wb[i]])
            return i

        def mm_group(i, M, p0, n):
            pi = st["pi"] % 4
            st["pi"] += 1
            for kc in range(16):
                S.op("pe", lambda e, kc=kc: e.matmul(pp[pi][0:M, 0:n], lhsT=wb[i][:, kc, 0:M],
                                                      rhs=g.xmT[:, kc, p0:p0 + n], start=(kc == 0), stop=(kc == 15)),
                     reads=[t_wb[i], g.t_xmT], writes=[t_pp[pi]])
            return pi

        def shift(zin, t_in, zout, t_out, plo, phi, cp, cn, segs):
            ps_ = slice(plo, phi)
            S.op("dve", lambda e: e.tensor_scalar(out=c0t[ps_, :], in0=cp[ps_, :], scalar1=cn[ps_, :], scalar2=-1.0,
                                                  op0=ALU.add, op1=ALU.mult), reads=[g.t_pv], writes=[t_c0])
            S.op("dve", lambda e: e.tensor_scalar(out=c0t[ps_, :], in0=c0t[ps_, :], scalar1=1.0, scalar2=None,
                                                  op0=ALU.add), reads=[t_c0], writes=[t_c0])
            lo, hi = segs[0][0], segs[-1][1]
            S.op("dve", lambda e: e.tensor_scalar(out=zout[ps_, lo:hi], in0=zin[ps_, lo:hi], scalar1=c0t[ps_, :],
                                                  scalar2=None, op0=ALU.mult), reads=[t_in, t_c0], writes=[t_out])
            for (a, b) in segs:
                S.op("dve", lambda e, a=a, b=b: e.scalar_tensor_tensor(
                    out=zout[ps_, a + 1:b], in0=zin[ps_, a:b - 1], scalar=cp[ps_, :], in1=zout[ps_, a + 1:b],
                    op0=ALU.mult, op1=ALU.add), reads=[t_in, g.t_pv], writes=[t_out])
                S.op("dve", lambda e, a=a, b=b: e.scalar_tensor_tensor(
                    out=zout[ps_, a:b - 1], in0=zin[ps_, a + 1:b], scalar=cn[ps_, :], in1=zout[ps_, a:b - 1],
                    op0=ALU.mult, op1=ALU.add), reads=[t_in, g.t_pv], writes=[t_out])

        SEG2 = [(0, TC), (TC, TT)]

        def one_pass(d, col0, M, dupl, bias, cp, cn, zout, t_out):
            i = load_w(col0, M, dupl)
            lo, hi = d * 64, d * 64 + 64
            for (p0, n) in TG_ALL:
                pi = mm_group(i, 128, p0, n)
                S.op("act", lambda e, pi=pi, p0=p0, n=n: e.activation(
                    out=zraw[lo:hi, p0:p0 + n], in_=pp[pi][lo:hi, 0:n], func=AF.Identity,
                    bias=bias[lo:hi, :], scale=1.0), reads=[t_pp[pi], g.t_pv], writes=[t_zraw])
            shift(zraw, t_zraw, zout, t_out, lo, hi, cp, cn, SEG2)

        def rkv_pass(d):
            lo, hi = d * 64, d * 64 + 64
            one_pass(d, 5120, 128, False, pvc(g, "b_wd"), pvc(g, "cp_wd"), pvc(g, "cn_wd"), g.wdt, g.t_wdt)
            S.op("act", lambda e: e.activation(out=g.wdt[lo:hi, :], in_=g.wdt[lo:hi, :], func=AF.Tanh),
                 reads=[g.t_wdt], writes=[g.t_wdt])
            one_pass(d, 5248, 128, False, pvc(g, "b_ad"), pvc(g, "cp_ad"), pvc(g, "cn_ad"), g.ads, g.t_ads)
            for q in range(3):
                for h in range(16):
                    zi = st["zi"] % 2
                    st["zi"] += 1
                    one_pass(d, 2048 + q * 1024 + h * 64, 64, True, pvc(g, "b_%d_%d" % (q, h)),
                             pvc(g, "cp_%d_%d" % (q, h)), pvc(g, "cn_%d_%d" % (q, h)), zs[zi], t_zs[zi])
                    S.dma("act", "st_a%d" % zi, g.RKV[q, h, lo:hi, :], zs[zi][lo:hi, :], reads=[t_zs[zi]],
                          writes=[g.t_RKV])

        def lora_stage():
            t_lw = Tok()
            for h in range(16):
                S.dma("sp", "ld_small", lw2[:, 0, :], g.w2bd[h], writes=[t_lw])
                S.dma("sp", "ld_small", lw2[:, 1, :], g.a2bd[h], writes=[t_lw])
                for k, (src, t_src, bname, dstd, t_dst) in enumerate(
                        ((g.wdt, g.t_wdt, "w0_%d" % h, g.SGs, g.t_SGs), (g.ads, g.t_ads, "a0_%d" % h, g.AGs, g.t_AGs))):
                    zi = k
                    for (p0, n) in TG_ALL:
                        pi = st["pi"] % 4
                        st["pi"] += 1
                        S.op("pe", lambda e, k=k, pi=pi, p0=p0, n=n, src=src: e.matmul(
                            pp[pi][:, 0:n], lhsT=lw2[:, k, :], rhs=src[:, p0:p0 + n], start=True, stop=True),
                             reads=[t_lw, t_src], writes=[t_pp[pi]])
                        S.op("act", lambda e, pi=pi, p0=p0, n=n, zi=zi, bname=bname: e.activation(
                            out=zs[zi][:, p0:p0 + n], in_=pp[pi][:, 0:n], func=AF.Sigmoid, bias=pvc(g, bname),
                            scale=1.0), reads=[t_pp[pi], g.t_pv], writes=[t_zs[zi]])
                    S.dma("act", "st_a%d" % zi, dstd[h], zs[zi][:], reads=[t_zs[zi]], writes=[t_dst])

        if rev_pass:
            rkv_pass(1)
            lora_stage()
        else:
            rkv_pass(0)
            sgd1, sgd2 = sb("sgd1", [128, T]), sb("sgd2", [32, T])
            g2a, g2b = sb("g2a", [128, 1024]), sb("g2b", [32, 1024])
            t_sgd, t_g2 = Tok(), Tok()
            for nm, col0, M, dst in (("gd1", 5376, 128, sgd1), ("gd2", 5504, 32, sgd2)):
                i = load_w(col0, M)
                for (p0, n) in TG_LAT:
                    pi = mm_group(i, M, p0, n)
                    S.op("act", lambda e, pi=pi, p0=p0, n=n, M=M, nm=nm: e.activation(
                        out=zraw[0:M, p0:p0 + n], in_=pp[pi][0:M, 0:n], func=AF.Identity,
                        bias=pvc(g, "b_" + nm)[0:M, :], scale=1.0), reads=[t_pp[pi], g.t_pv], writes=[t_zraw])
                shift(zraw, t_zraw, zs[0], t_zs[0], 0, M, pvc(g, "cp_" + nm), pvc(g, "cn_" + nm), [(TC, TT)])
                S.op("act", lambda e, M=M, dst=dst: e.activation(out=dst[0:M, :], in_=zs[0][0:M, TC:TT],
                                                                 func=AF.Sigmoid), reads=[t_zs[0]], writes=[t_sgd])
            S.dma("sp", "ld_small", g2a[:], g.g2[0:128, :], writes=[t_g2])
            S.dma("sp", "ld_small", g2b[:], g.g2[128:160, :], writes=[t_g2])
            for c in range(32):
                zi = c % 2
                for hf in range(2):
                    pi = st["pi"] % 4
                    st["pi"] += 1
                    S.op("pe", lambda e, c=c, hf=hf, pi=pi: e.matmul(
                        pp[pi][0:64, :], lhsT=sgd1[:, c * 64:(c + 1) * 64], rhs=g2a[:, hf * 512:(hf + 1) * 512],
                        start=True, stop=False), reads=[t_sgd, t_g2], writes=[t_pp[pi]])
                    S.op("pe", lambda e, c=c, hf=hf, pi=pi: e.matmul(
                        pp[pi][0:64, :], lhsT=sgd2[:, c * 64:(c + 1) * 64], rhs=g2b[:, hf * 512:(hf + 1) * 512],
                        start=False, stop=True), reads=[t_sgd, t_g2], writes=[t_pp[pi]])
                    S.op("dve", lambda e, hf=hf, pi=pi, zi=zi: e.tensor_copy(
                        out=zs[zi][0:64, hf * 512:(hf + 1) * 512], in_=pp[pi][0:64, :]),
                         reads=[t_pp[pi]], writes=[t_zs[zi]])
                S.dma("act", "st_a%d" % zi, g.Gs[c * 64:(c + 1) * 64, :], zs[zi][0:64, 0:1024],
                      reads=[t_zs[zi]], writes=[g.t_Gs])
            for c in range(8):
                ia = load_w(c * 128, 128)
                ig = load_w(1024 + c * 128, 128)
                for ti, (p0, n) in enumerate(TG_LAT):
                    pa = mm_group(ia, 128, p0, n)
                    pg = mm_group(ig, 128, p0, n)
                    S.op("act", lambda e, pg=pg, ti=ti: e.activation(
                        out=zs[0][:, ti * 512:(ti + 1) * 512], in_=pp[pg][:, :], func=AF.Sigmoid,
                        bias=pvc(g, "cbg_%d" % c), scale=1.0), reads=[t_pp[pg], g.t_pv], writes=[t_zs[0]])
                    S.op("dve", lambda e, pa=pa, ti=ti: e.scalar_tensor_tensor(
                        out=zraw[:, ti * 512:(ti + 1) * 512], in0=pp[pa][:, :], scalar=pvc(g, "cba_%d" % c),
                        in1=zs[0][:, ti * 512:(ti + 1) * 512], op0=ALU.add, op1=ALU.mult),
                         reads=[t_pp[pa], t_zs[0], g.t_pv], writes=[t_zraw])
                hv = zraw[:, 0:T].rearrange("p (r w) -> p r w", w=64)
                ov = zs[1][:, 0:T].rearrange("p (r w) -> p r w", w=64)
                S.op("dve", lambda e: e.tensor_scalar(out=zs[1][:, 0:T], in0=zraw[:, 0:T],
                                                      scalar1=pvc(g, "cw_%d_15" % c), scalar2=pvc(g, "convb_%d" % c),
                                                      op0=ALU.mult, op1=ALU.add),
                     reads=[t_zraw, g.t_pv], writes=[t_zs[1]])
                for j in range(31):
                    o = j - 15
                    if o == 0:
                        continue
                    lo, hi = max(0, -o), min(64, 64 - o)
                    S.op("dve", lambda e, j=j, o=o, lo=lo, hi=hi: e.scalar_tensor_tensor(
                        out=ov[:, :, lo:hi], in0=hv[:, :, lo + o:hi + o], scalar=pvc(g, "cw_%d_%d" % (c, j)),
                        in1=ov[:, :, lo:hi], op0=ALU.mult, op1=ALU.add), reads=[t_zraw, g.t_pv], writes=[t_zs[1]])
                S.dma("act", "st_a1", g.ZC[c * 128:(c + 1) * 128, :], zs[1][:, 0:T], reads=[t_zs[1]],
                      writes=[g.t_ZC])
            for c in range(32):
                i = load_w(5536 + c * 128, 128)
                zi = c % 2
                for ti, (p0, n) in enumerate(TG_LAT):
                    pi = mm_group(i, 128, p0, n)
                    S.op("act", lambda e, pi=pi, ti=ti, zi=zi: e.activation(
                        out=zs[zi][:, ti * 512:(ti + 1) * 512], in_=pp[pi][:, :], func=AF.Sigmoid,
                        bias=pvc(g, "bzg_%d" % c), scale=1.0), reads=[t_pp[pi], g.t_pv], writes=[t_zs[zi]])
                S.dma("act", "st_a%d" % zi, g.SGZ[c * 128:(c + 1) * 128, :], zs[zi][:, 0:T],
                      reads=[t_zs[zi]], writes=[g.t_SGZ])
    S.barrier()


KAPPA = float(np.exp(-0.5))
GN_EPS = 64e-5
NCH = TT // 64


def phase_rwkv(g):
    nc, S = g.nc, g.S
    g.ORW = nc.dram_tensor("s_orw", [1024, T], F32, kind="Internal").ap()
    g.t_ORW = Tok()
    with ExitStack() as es:
        sb = lambda name, shape, dt=F32: es.enter_context(nc.sbuf_tensor("rw_" + name, shape, dt))
        psb = lambda name, shape, dt=F32: es.enter_context(nc.psum_tensor("rwp_" + name, shape, dt))
        R, Kt, Vt, SG, AG, KK, LI, TM = (sb(n, [128, TT]) for n in ("R", "Kt", "Vt", "SG", "AG", "KK", "LI", "TM"))
        QR, BK = sb("QR", [128, 2, TT]), sb("BK", [128, 2, TT])
        BKh = sb("BKh", [64, NCH, 2, 128])
        Vtm = sb("Vtm", [64, NCH, 128])
        Ys = sb("Ys", [64, 2, 32, 66])
        DC = sb("DC", [128, NCH])
        A2 = [sb("A0", [64, 2, 320]), sb("A1", [64, 2, 320])]
        W4 = [[sb("W00", [64, 2, 3, 64]), sb("W01", [64, 2, 3, 64])], [sb("W10", [64, 2, 3, 64]), sb("W11", [64, 2, 3, 64])]]
        tA = [Tok("A0"), Tok("A1")]
        tW = [[Tok(), Tok()], [Tok(), Tok()]]
        A_sb, W = A2[1], W4[1]
        P1s, UTs = sb("P1s", [64, 2, 64]), sb("UTs", [64, 2, 64])
        Sst = sb("Sst", [128, 64])
        maskc = sb("maskc", [128, TT])
        maskA = sb("maskA", [64, 320])
        onesbd = sb("onesbd", [128, 128])
        onesel = sb("onesel", [128, 2])
        omka = sb("omka", [128, 1])
        LG, LB = sb("LG", [64, 64]), sb("LB", [64, 64])
        st1, st2 = sb("st1", [64, 32]), sb("st2", [64, 32])
        epsg = sb("epsg", [64, 1])
        banks = [psb("bk%d" % i, [128, 512]) for i in range(8)]
        PA = [banks[0][0:64, 0:320], banks[1][0:64, 0:320]]
        PS = banks[2][0:64, 0:384].rearrange("p (d w) -> p d w", w=192)
        PQ = banks[3][0:64, 0:256].rearrange("p (a w) -> p a w", w=64)
        PSn = banks[4][:, 0:128].rearrange("p (d w) -> p d w", w=64)
        PT = banks[5]
        PT2 = banks[6]
        PU = banks[7][0:64, 0:128].rearrange("p (a w) -> p a w", w=64)
        t = {n: Tok(n) for n in ("R", "Kt", "Vt", "SG", "AG", "KK", "LI", "TM", "QR", "BK", "BKh", "Vtm", "Ys", "DC",
                                 "A", "W0", "W1", "P1s", "UTs", "Sst", "c", "LG", "st", "G")}
        for i in range(8):
            t["B%d" % i] = Tok("B%d" % i, excl=True)
        t["PA0"], t["PA1"], t["PS"], t["PQ1"], t["PQ3"], t["PSn"], t["PT"], t["PT2"], t["PQ2"] = (
            t["B0"], t["B1"], t["B2"], t["B3"], t["B3"], t["B4"], t["B5"], t["B6"], t["B7"])
        S.op("pool", lambda e: e.memset(maskc[:], 1.0), writes=[t["c"]])
        S.op("pool", lambda e: e.memset(maskc[:].rearrange("p (c w) -> p c w", w=64)[:, :, 0:1], 0.0),
             reads=[t["c"]], writes=[t["c"]])
        S.op("pool", lambda e: e.memset(onesbd[:], 0.0), writes=[t["c"]])
        S.op("pool", lambda e: e.memset(onesbd[0:64, 0:64], 1.0), reads=[t["c"]], writes=[t["c"]])
        S.op("pool", lambda e: e.memset(onesbd[64:128, 64:128], 1.0), reads=[t["c"]], writes=[t["c"]])
        S.op("pool", lambda e: e.memset(onesel[:], 0.0), writes=[t["c"]])
        S.op("pool", lambda e: e.memset(onesel[0:64, 0:1], 1.0), reads=[t["c"]], writes=[t["c"]])
        S.op("pool", lambda e: e.memset(onesel[64:128, 1:2], 1.0), reads=[t["c"]], writes=[t["c"]])
        S.op("pool", lambda e: e.memset(epsg[:], GN_EPS), writes=[t["c"]])
        S.op("pool", lambda e: e.memset(maskA[:], 1.0), writes=[t["c"]])
        for blk, (cm, base, pat) in enumerate(((-1, -1, 1), (-1, 0, 1), (-1, -1, 1), (-1, 0, 1), (1, -1, -1))):
            S.op("pool", lambda e, blk=blk, cm=cm, base=base, pat=pat: e.affine_select(
                out=maskA[:, blk * 64:(blk + 1) * 64], in_=maskA[:, blk * 64:(blk + 1) * 64], pattern=[[pat, 64]],
                compare_op=ALU.is_ge, fill=0.0, base=base, channel_multiplier=cm), reads=[t["c"]], writes=[t["c"]])
        I64 = g.ident[0:64, 0:64]
        J64 = g.anti[0:64, 64:128]
        g.rw_dbg = {}
        def emit_loads(h):
            for buf, nm, src, tk in ((R, "R", g.RKV[0, h], g.t_RKV), (Kt, "Kt", g.RKV[1, h], g.t_RKV),
                                     (Vt, "Vt", g.RKV[2, h], g.t_RKV), (SG, "SG", g.SGs[h], g.t_SGs),
                                     (AG, "AG", g.AGs[h], g.t_AGs)):
                S.dma("sp", "ld_rw_" + nm, buf[:], src, reads=[tk], writes=[t[nm]])

        for h in range(getattr(g, "nheads", 16)):
            if h == 0:
                emit_loads(0)
            S.dma("pool", "ld_rw_lg", LG[:], g.lnx_g[h:h + 1, :].partition_broadcast(64), writes=[t["LG"]])
            S.dma("pool", "ld_rw_lg", LB[:], g.lnx_b[h:h + 1, :].partition_broadcast(64), writes=[t["LG"]])
            kkc, kac, rkc = pvc(g, "kk_%d" % h), pvc(g, "ka_%d" % h), pvc(g, "rk_%d" % h)
            S.op("dve", lambda e: e.tensor_scalar(out=omka[:], in0=kac, scalar1=-1.0, scalar2=1.0, op0=ALU.mult,
                                                  op1=ALU.add), reads=[g.t_pv], writes=[t["c"]])
            S.op("dve", lambda e: e.tensor_scalar(out=TM[:], in0=Kt[:], scalar1=kkc, scalar2=None, op0=ALU.mult),
                 reads=[t["Kt"], g.t_pv], writes=[t["TM"]])
            S.op("act", lambda e: e.activation(out=KK[:], in_=TM[:], func=AF.Square), reads=[t["TM"]], writes=[t["KK"]])
            for i, (p0, n) in enumerate(TG_ALL):
                pt, tn = (PT, "PT") if i % 2 == 0 else (PT2, "PT2")
                S.op("pe", lambda e, pt=pt, p0=p0, n=n: e.matmul(pt[:, 0:n], lhsT=onesbd[:], rhs=KK[:, p0:p0 + n],
                                                                start=True, stop=True),
                     reads=[t["KK"], t["c"]], writes=[t[tn]])
                S.op("act", lambda e, pt=pt, p0=p0, n=n: e.activation(out=LI[:, p0:p0 + n], in_=pt[:, 0:n],
                                                                      func=AF.Sqrt), reads=[t[tn]], writes=[t["LI"]])
            S.op("dve", lambda e: e.tensor_scalar(out=LI[:], in0=LI[:], scalar1=1e-12, scalar2=None, op0=ALU.max),
                 reads=[t["LI"]], writes=[t["LI"]])
            S.op("dve", lambda e: e.reciprocal(out=LI[:], in_=LI[:]), reads=[t["LI"]], writes=[t["LI"]])
            S.op("dve", lambda e: e.tensor_tensor(out=KK[:], in0=TM[:], in1=LI[:], op=ALU.mult),
                 reads=[t["TM"], t["LI"]], writes=[t["KK"]])
            if getattr(g, "rw_stop", 9) <= 1:
                continue
            S.op("dve", lambda e: e.tensor_tensor_scan(out=LI[:], data0=maskc[:], data1=SG[:], initial=0.0,
                                                       op0=ALU.mult, op1=ALU.add),
                 reads=[t["SG"], t["c"], t["KK"]], writes=[t["LI"]])
            S.op("dve", lambda e: e.tensor_tensor(out=SG[:], in0=LI[:], in1=SG[:], op=ALU.subtract),
                 reads=[t["LI"]], writes=[t["SG"]])
            LIv = LI[:].rearrange("p (c w) -> p c w", w=64)
            S.op("act", lambda e: e.activation(out=DC[:], in_=LIv[:, :, 63], func=AF.Exp, scale=-KAPPA),
                 reads=[t["LI"]], writes=[t["DC"]])
            S.op("act", lambda e: e.activation(out=TM[:], in_=LI[:], func=AF.Exp, scale=-KAPPA),
                 reads=[t["LI"], t["KK"]], writes=[t["TM"]])
            S.op("dve", lambda e: e.tensor_tensor(out=QR[:, 1, :], in0=R[:], in1=TM[:], op=ALU.mult),
                 reads=[t["R"], t["TM"]], writes=[t["QR"]])
            S.op("act", lambda e: e.activation(out=TM[:], in_=SG[:], func=AF.Exp, scale=-KAPPA),
                 reads=[t["SG"], t["QR"]], writes=[t["TM"]])
            S.op("dve", lambda e: e.scalar_tensor_tensor(out=QR[:, 0, :], in0=KK[:], scalar=-1.0, in1=TM[:],
                                                         op0=ALU.mult, op1=ALU.mult),
                 reads=[t["KK"], t["TM"]], writes=[t["QR"]])
            S.op("act", lambda e: e.activation(out=TM[:], in_=LI[:], func=AF.Exp, scale=KAPPA),
                 reads=[t["LI"], t["QR"]], writes=[t["TM"]])
            S.op("dve", lambda e: e.tensor_scalar(out=SG[:], in0=AG[:], scalar1=kac, scalar2=omka[:, 0:1],
                                                  op0=ALU.mult, op1=ALU.add),
                 reads=[t["AG"], t["c"], g.t_pv, t["TM"]], writes=[t["SG"]])
            S.op("dve", lambda e: e.tensor_tensor(out=Kt[:], in0=Kt[:], in1=SG[:], op=ALU.mult),
                 reads=[t["SG"], t["TM"]], writes=[t["Kt"]])
            S.op("dve", lambda e: e.tensor_tensor(out=AG[:], in0=KK[:], in1=AG[:], op=ALU.mult),
                 reads=[t["KK"]], writes=[t["AG"]])
            S.op("dve", lambda e: e.tensor_tensor(out=BK[:, 0, :], in0=AG[:], in1=TM[:], op=ALU.mult),
                 reads=[t["AG"], t["TM"]], writes=[t["BK"]])
            S.op("dve", lambda e: e.tensor_tensor(out=BK[:, 1, :], in0=Kt[:], in1=TM[:], op=ALU.mult),
                 reads=[t["Kt"], t["TM"]], writes=[t["BK"]])
            S.op("dve", lambda e: e.scalar_tensor_tensor(out=R[:], in0=R[:], scalar=rkc, in1=Kt[:], op0=ALU.mult,
                                                         op1=ALU.mult), reads=[t["Kt"], t["QR"], g.t_pv], writes=[t["R"]])
            TMv = TM[:].rearrange("p (c w) -> p c w", w=64)
            S.op("dve", lambda e: e.tensor_tensor(out=TMv, in0=TMv, in1=DC[:].unsqueeze(2).to_broadcast([128, NCH, 64]),
                                                  op=ALU.mult), reads=[t["DC"], t["BK"]], writes=[t["TM"]])
            S.op("dve", lambda e: e.tensor_tensor(out=SG[:], in0=AG[:], in1=TM[:], op=ALU.mult),
                 reads=[t["AG"], t["TM"], t["Kt"]], writes=[t["SG"]])
            S.op("dve", lambda e: e.tensor_tensor(out=LI[:], in0=Kt[:], in1=TM[:], op=ALU.mult),
                 reads=[t["Kt"], t["TM"]], writes=[t["LI"]])
            if getattr(g, "rw_stop", 9) <= 2:
                continue
            import os
            _f3 = os.environ.get("RW3", "abcd")
            for c in range(int(os.environ.get("RW3N", NCH))):
                csl = slice(c * 64, (c + 1) * 64)
                pt, tn = (PT, "PT") if c % 2 == 0 else (PT2, "PT2")
                ptv = pt[0:64, 0:384].rearrange("p (a b) -> p a b", b=128)
                for a, (src, tk) in enumerate(((SG, "SG"), (LI, "LI"), (Vt, "Vt"))):
                    if "a" not in _f3:
                        continue
                    S.op("pe", lambda e, a=a, src=src, ptv=ptv, csl=csl: e.matmul(ptv[:, a, :], lhsT=src[:, csl], rhs=g.ident[:], start=True, stop=True),
                         reads=[t[tk], g.t_ident], writes=[t[tn]])
                if "b" in _f3:
                    S.op("act", lambda e, c=c, ptv=ptv: e.activation(out=BKh[:, c, :, :], in_=ptv[:, 0:2, :], func=AF.Identity),
                         reads=[t[tn]], writes=[t["BKh"]])
                if "c" in _f3:
                    S.op("dve", lambda e, c=c, ptv=ptv: e.tensor_copy(out=Vtm[:, c, :], in_=ptv[:, 2, :]),
                         reads=[t[tn]], writes=[t["Vtm"]])
                if c >= 4 and "d" in _f3:
                    S.op("pe", lambda e, c=c, pt=pt, csl=csl: e.matmul(pt[0:64, 384:386], lhsT=R[:, csl], rhs=onesel[:],
                                                                       start=True, stop=True),
                         reads=[t["R"], t["c"]], writes=[t[tn]])
                    S.op("dve", lambda e, c=c, pt=pt: e.tensor_copy(out=Ys[:, :, c - 4, 64], in_=pt[0:64, 384:386]),
                         reads=[t[tn]], writes=[t["Ys"]])
            if getattr(g, "rw_stop", 9) <= 3:
                continue
            if h + 1 < getattr(g, "nheads", 16):
                emit_loads(h + 1)
            S.op("pool", lambda e: e.memset(Sst[:], 0.0), reads=[t["Sst"]], writes=[t["Sst"]])
            PSv = PS.rearrange("p d (b w) -> p d b w", w=64)

            def emit_A(c, sl):
                csl = slice(c * 64, (c + 1) * 64)
                A_ = A2[sl]
                for d in range(2):
                    ds = slice(d * 64, d * 64 + 64)
                    pn = "PA%d" % d
                    for (o0, o1, lt, li, rhs_fn) in ((0, 128, BK, 0, lambda ds=ds: QR[ds, :, csl]),
                                                     (128, 256, BK, 1, lambda ds=ds: QR[ds, :, csl]),
                                                     (256, 320, QR, 0, lambda ds=ds: BK[ds, 0, csl])):
                        S.pe_rg(d * 64)
                        S.op("pe", lambda e, d=d, ds=ds, o0=o0, o1=o1, lt=lt, li=li, rhs_fn=rhs_fn: e.matmul(
                            PA[d][:, o0:o1], lhsT=lt[ds, li, csl], rhs=rhs_fn(), start=True, stop=True),
                             reads=[t["BK"], t["QR"]], writes=[t[pn]])
                    S.op("dve", lambda e, d=d, A_=A_: e.tensor_tensor(out=A_[:, d, :], in0=PA[d][:, :], in1=maskA[:],
                                                                     op=ALU.mult), reads=[t[pn], t["c"]], writes=[tA[sl]])
                Av = A_[:].rearrange("p d (b w) -> p d b w", w=64)
                w0 = W4[sl][0]
                S.op("act", lambda e: e.activation(out=w0[:, :, 0, :], in_=Av[:, :, 0, :], func=AF.Identity),
                     reads=[tA[sl]], writes=[tW[sl][0]])
                S.op("act", lambda e: e.activation(out=w0[:, :, 2, :], in_=Av[:, :, 4, :], func=AF.Identity),
                     reads=[tA[sl]], writes=[tW[sl][0]])
                S.op("dve", lambda e: e.tensor_copy(out=w0[:, :, 1, :], in_=I64.unsqueeze(1).to_broadcast([64, 2, 64])),
                     reads=[g.t_ident], writes=[tW[sl][0]])

            def emit_stage(s_, sl):
                wc, wn = W4[sl][s_ % 2], W4[sl][(s_ + 1) % 2]
                tc_, tn_ = tW[sl][s_ % 2], tW[sl][(s_ + 1) % 2]
                for d in range(2):
                    S.op("pe", lambda e, d=d: e.matmul(PS[:, d, 0:128], lhsT=wc[:, d, 2, :], rhs=wc[:, d, 0:2, :],
                                                       start=True, stop=True), reads=[tc_], writes=[t["PS"]])
                    if s_ < 5:
                        S.op("pe", lambda e, d=d: e.matmul(PS[:, d, 128:192], lhsT=wc[:, d, 0, :], rhs=wc[:, d, 2, :],
                                                           start=True, stop=True), reads=[tc_], writes=[t["PS"]])
                if s_ < 5:
                    for blk in (0, 2):
                        S.op("act", lambda e, blk=blk: e.activation(out=wn[:, :, blk, :], in_=PSv[:, :, blk, :],
                                                                    func=AF.Identity), reads=[t["PS"]], writes=[tn_])
                S.op("dve", lambda e: e.tensor_tensor(out=wn[:, :, 1, :], in0=wc[:, :, 1, :], in1=PSv[:, :, 1, :],
                                                      op=ALU.add), reads=[t["PS"], tc_], writes=[tn_])

            def seq_parts(c, sl):
                csl = slice(c * 64, (c + 1) * 64)
                A_ = A2[sl]
                Wf, tWf = W4[sl][0], tW[sl][0]

                def p0():
                    for d in range(2):
                        ds = slice(d * 64, d * 64 + 64)
                        S.pe_rg(d * 64)
                        S.op("pe", lambda e, d=d, ds=ds: e.matmul(PQ[:, d, :], lhsT=QR[ds, 0, csl], rhs=Sst[ds, :],
                                                                  start=True, stop=True),
                             reads=[t["QR"], t["Sst"]], writes=[t["PQ1"]])
                        S.op("pe", lambda e, d=d, ds=ds: e.matmul(PQ[:, 2 + d, :], lhsT=A_[:, d, 128:192], rhs=Vtm[:, c, ds],
                                                                  start=True, stop=True),
                             reads=[tA[sl], t["Vtm"]], writes=[t["PQ1"]])
                    S.op("act", lambda e: e.activation(out=P1s[:], in_=PQ[:, 0:2, :], func=AF.Identity),
                         reads=[t["PQ1"]], writes=[t["P1s"]])
                    S.op("dve", lambda e: e.tensor_tensor(out=P1s[:], in0=P1s[:], in1=PQ[:, 2:4, :], op=ALU.add),
                         reads=[t["PQ1"]], writes=[t["P1s"]])

                def p1():
                    for d in range(2):
                        S.op("pe", lambda e, d=d: e.matmul(PU[:, d, :], lhsT=Wf[:, d, 1, :], rhs=P1s[:, d, :],
                                                           start=True, stop=True), reads=[tWf, t["P1s"]], writes=[t["PQ2"]])
                    S.op("dve", lambda e: e.tensor_copy(out=UTs[:], in_=PU[:, 0:2, :]), reads=[t["PQ2"]], writes=[t["UTs"]])

                def p2():
                    for d in range(2):
                        ds = slice(d * 64, d * 64 + 64)
                        S.op("pe", lambda e, d=d: e.matmul(PSn[:, d, :], lhsT=BKh[:, c, 0, :], rhs=UTs[:, d, :],
                                                           start=True, stop=False), reads=[t["BKh"], t["UTs"]],
                             writes=[t["PSn"]])
                        S.op("pe", lambda e, d=d, ds=ds: e.matmul(PSn[:, d, :], lhsT=BKh[:, c, 1, :], rhs=Vtm[:, c, ds],
                                                                  start=False, stop=True),
                             reads=[t["BKh"], t["Vtm"]], writes=[t["PSn"]])

                def p3():
                    if c < 4:
                        return
                    for d in range(2):
                        ds = slice(d * 64, d * 64 + 64)
                        S.pe_rg(d * 64)
                        S.op("pe", lambda e, d=d, ds=ds: e.matmul(PQ[:, d, :], lhsT=QR[ds, 1, csl], rhs=Sst[ds, :],
                                                                  start=True, stop=True),
                             reads=[t["QR"], t["Sst"], t["P1s"]], writes=[t["PQ3"]])
                        S.op("pe", lambda e, d=d: e.matmul(PQ[:, 2 + d, :], lhsT=A_[:, d, 64:128], rhs=UTs[:, d, :],
                                                           start=True, stop=False), reads=[tA[sl], t["UTs"]],
                             writes=[t["PQ3"]])
                        S.op("pe", lambda e, d=d, ds=ds: e.matmul(PQ[:, 2 + d, :], lhsT=A_[:, d, 192:256],
                                                                  rhs=Vtm[:, c, ds], start=False, stop=True),
                             reads=[tA[sl], t["Vtm"]], writes=[t["PQ3"]])
                    S.op("act", lambda e: e.activation(out=Ys[:, :, c - 4, 0:64], in_=PQ[:, 0:2, :], func=AF.Identity),
                         reads=[t["PQ3"]], writes=[t["Ys"]])
                    S.op("dve", lambda e: e.tensor_tensor(out=Ys[:, :, c - 4, 0:64], in0=Ys[:, :, c - 4, 0:64],
                                                          in1=PQ[:, 2:4, :], op=ALU.add),
                         reads=[t["PQ3"]], writes=[t["Ys"]])

                def p4():
                    for d in range(2):
                        ds = slice(d * 64, d * 64 + 64)
                        S.op("dve", lambda e, d=d, ds=ds: e.scalar_tensor_tensor(
                            out=Sst[ds, :], in0=Sst[ds, :], scalar=DC[ds, c:c + 1], in1=PSn[ds, d, :], op0=ALU.mult,
                            op1=ALU.add), reads=[t["PSn"], t["DC"], t["Sst"]], writes=[t["Sst"]])
                return [p0, p1, p2, p3, p4]

            emit_A(0, 0)
            for s_ in range(6):
                emit_stage(s_, 0)
            for c in range(NCH):
                sl = c % 2
                parts = seq_parts(c, sl)
                if c + 1 < NCH:
                    emit_A(c + 1, 1 - sl)
                for s_ in range(6):
                    if c + 1 < NCH:
                        emit_stage(s_, 1 - sl)
                    if s_ < len(parts):
                        parts[s_]()
            if getattr(g, "nheads", 16) == 1:
                g.rw_dbg = {"Ys": Ys[:], "QR": QR[:], "BK": BK[:], "BKh": BKh[:], "Vtm": Vtm[:], "DC": DC[:], "A": A_sb[:],
                            "Winv": W[0][:], "Sst": Sst[:]}
            if getattr(g, "rw_stop", 9) <= 4:
                continue
            Yt = TM[0:64, 0:32 * 66].rearrange("p (c w) -> p c w", w=66)
            Gt = KK[0:64, 0:2048].rearrange("p (c w) -> p c w", w=64)
            Yc = QR[0:64, 0, 0:2048].rearrange("p (c w) -> p c w", w=64)
            Y2 = QR[0:64, 1, 0:2048].rearrange("p (c w) -> p c w", w=64)
            S.dma("sp", "ld_rw_G", Gt, g.Gs[:, h * 64:(h + 1) * 64].rearrange("(c p) v -> p c v", p=64),
                  reads=[g.t_Gs, t["KK"]], writes=[t["KK"]])
            for c4 in range(8):
                pt, tn = (PT, "PT") if c4 % 2 == 0 else (PT2, "PT2")
                ptv = pt[0:64, 0:264].rearrange("p (a b) -> p a b", b=66)
                for a in range(4):
                    c = c4 * 4 + a
                    S.op("pe", lambda e, a=a, c=c, ptv=ptv: e.matmul(ptv[:, a, :], lhsT=I64, rhs=Ys[:, 0, c, :],
                                                                     start=True, stop=False),
                         reads=[t["Ys"], g.t_ident], writes=[t[tn]])
                    S.op("pe", lambda e, a=a, c=c, ptv=ptv: e.matmul(ptv[:, a, :], lhsT=J64, rhs=Ys[:, 1, 31 - c, :],
                                                                     start=False, stop=True),
                         reads=[t["Ys"], g.t_ident], writes=[t[tn]])
                S.op("act", lambda e, c4=c4, ptv=ptv: e.activation(out=Yt[:, c4 * 4:(c4 + 1) * 4, :], in_=ptv,
                                                                   func=AF.Identity), reads=[t[tn], t["LI"], t["SG"]],
                     writes=[t["TM"]])
            S.op("dve", lambda e: e.tensor_reduce(out=st1[:], in_=Yt[:, :, 0:64], axis=AX.X, op=ALU.add),
                 reads=[t["TM"]], writes=[t["st"]])
            S.op("dve", lambda e: e.tensor_scalar(out=st1[:], in0=st1[:], scalar1=1.0 / 64, scalar2=None, op0=ALU.mult),
                 reads=[t["st"]], writes=[t["st"]])
            S.op("dve", lambda e: e.tensor_tensor(out=Yc, in0=Yt[:, :, 0:64],
                                                  in1=st1[:].unsqueeze(2).to_broadcast([64, 32, 64]), op=ALU.subtract),
                 reads=[t["TM"], t["st"], t["BK"], t["Sst"]], writes=[t["QR"]])
            S.op("act", lambda e: e.activation(out=Y2, in_=Yc, func=AF.Square), reads=[t["QR"]], writes=[t["QR"]])
            S.op("dve", lambda e: e.tensor_reduce(out=st2[:], in_=Y2, axis=AX.X, op=ALU.add),
                 reads=[t["QR"]], writes=[t["st"]])
            S.op("act", lambda e: e.activation(out=st2[:], in_=st2[:], func=AF.Sqrt, bias=epsg[:, 0:1], scale=1.0 / 64),
                 reads=[t["st"], t["c"]], writes=[t["st"]])
            S.op("dve", lambda e: e.reciprocal(out=st2[:], in_=st2[:]), reads=[t["st"]], writes=[t["st"]])
            S.op("dve", lambda e: e.tensor_tensor(out=Yc, in0=Yc, in1=st2[:].unsqueeze(2).to_broadcast([64, 32, 64]),
                                                  op=ALU.mult), reads=[t["st"], t["QR"]], writes=[t["QR"]])
            S.op("dve", lambda e: e.tensor_tensor(out=Yc, in0=Yc, in1=LG[:].unsqueeze(1).to_broadcast([64, 32, 64]),
                                                  op=ALU.mult), reads=[t["LG"], t["QR"]], writes=[t["QR"]])
            S.op("dve", lambda e: e.tensor_tensor(out=Yc, in0=Yc, in1=LB[:].unsqueeze(1).to_broadcast([64, 32, 64]),
                                                  op=ALU.add), reads=[t["LG"], t["QR"]], writes=[t["QR"]])
            S.op("dve", lambda e: e.tensor_tensor(out=Y2, in0=Vtm[:, 4:36, 0:64],
                                                  in1=Yt[:, :, 64:65].to_broadcast([64, 32, 64]), op=ALU.mult),
                 reads=[t["Vtm"], t["TM"], t["QR"]], writes=[t["QR"]])
            S.op("dve", lambda e: e.tensor_tensor(out=Yc, in0=Yc, in1=Y2, op=ALU.add), reads=[t["QR"]], writes=[t["QR"]])
            S.op("dve", lambda e: e.tensor_tensor(out=Yc, in0=Yc, in1=Gt, op=ALU.mult), reads=[t["QR"], t["KK"]],
                 writes=[t["QR"]])
            Of = BK[0:64, 0, 0:2048]
            for c8 in range(4):
                pt, tn = (PT, "PT") if c8 % 2 == 0 else (PT2, "PT2")
                for a in range(8):
                    c = c8 * 8 + a
                    S.op("pe", lambda e, a=a, c=c, pt=pt: e.matmul(pt[0:64, a * 64:(a + 1) * 64], lhsT=Yc[:, c, :], rhs=I64, start=True, stop=True),
                         reads=[t["QR"], g.t_ident], writes=[t[tn]])
                S.op("act", lambda e, c8=c8, pt=pt: e.activation(out=Of[:, c8 * 512:(c8 + 1) * 512], in_=pt[0:64, :],
                                                                 func=AF.Identity), reads=[t[tn], t["A"]], writes=[t["BK"]])
            S.dma("sp", "st_rw", g.ORW[h * 64:(h + 1) * 64, :], Of, reads=[t["BK"]], writes=[g.t_ORW, t["BK"]])
    S.barrier()


def bank_toks(n):
    return [Tok("bank%d" % i, excl=True) for i in range(n)]


def phase_mix(g):
    nc, S = g.nc, g.S
    g.es2 = ExitStack()
    g.mixT = g.es2.enter_context(nc.sbuf_tensor("mixT", [128, 16, T], BF16))
    g.t_mixT = Tok()
    with ExitStack() as es:
        sb = lambda name, shape, dt=F32: es.enter_context(nc.sbuf_tensor("mx_" + name, shape, dt))
        HN, ORWb = sb("HN", [128, 8, T], BF16), sb("ORWb", [128, 8, T], BF16)
        ZCt, SQ = sb("ZCt", [128, 8, 512]), sb("SQ", [128, 8, 512])
        mu, rs, tmp = sb("mu", [128, 512]), sb("rs", [128, 512]), sb("tmp", [128, 512])
        onesM = sb("onesM", [128, 128])
        wf = [sb("wf0", [128, 8, 128]), sb("wf1", [128, 8, 128])]
        wb = [sb("wb0", [128, 8, 128], BF16), sb("wb1", [128, 8, 128], BF16)]
        sz = [sb("sz0", [128, 512]), sb("sz1", [128, 512])]
        t1, t2 = sb("t1", [128, 512]), sb("t2", [128, 512])
        banks = [es.enter_context(nc.psum_tensor("mx_bk%d" % i, [128, 512], F32)) for i in range(4)]
        tb = bank_toks(4)
        tk = {n: Tok(n) for n in ("HN", "ORWb", "ZCt", "SQ", "mu", "rs", "tmp", "c", "wf0", "wf1", "wb0", "wb1",
                                  "sz0", "sz1", "t1", "t2")}
        S.op("pool", lambda e: e.memset(onesM[:], 1.0 / 1024), writes=[tk["c"]])
        zcv = g.ZC.rearrange("(c p) t -> p c t", p=128)
        for tg in range(4):
            tsl = slice(tg * 512, (tg + 1) * 512)
            S.dma("sp", "mx_ld", ZCt[:], zcv[:, :, tsl], reads=[g.t_ZC], writes=[tk["ZCt"]])
            S.op("act", lambda e: e.activation(out=SQ[:], in_=ZCt[:], func=AF.Square), reads=[tk["ZCt"]], writes=[tk["SQ"]])
            for cc in range(8):
                S.op("pe", lambda e, cc=cc: e.matmul(banks[0][:, :], lhsT=onesM[:], rhs=ZCt[:, cc, :], start=(cc == 0),
                                                    stop=(cc == 7)), reads=[tk["ZCt"], tk["c"]], writes=[tb[0]])
            for cc in range(8):
                S.op("pe", lambda e, cc=cc: e.matmul(banks[1][:, :], lhsT=onesM[:], rhs=SQ[:, cc, :], start=(cc == 0),
                                                    stop=(cc == 7)), reads=[tk["SQ"], tk["c"]], writes=[tb[1]])
            S.op("act", lambda e: e.activation(out=mu[:], in_=banks[0][:, :], func=AF.Identity), reads=[tb[0]],
                 writes=[tk["mu"]])
            S.op("dve", lambda e: e.tensor_tensor(out=tmp[:], in0=mu[:], in1=mu[:], op=ALU.mult), reads=[tk["mu"]],
                 writes=[tk["tmp"]])
            S.op("dve", lambda e: e.tensor_tensor(out=rs[:], in0=banks[1][:, :], in1=tmp[:], op=ALU.subtract),
                 reads=[tb[1], tk["tmp"]], writes=[tk["rs"]])
            S.op("act", lambda e: e.activation(out=rs[:], in_=rs[:], func=AF.Sqrt, bias=g.eps_ln[:, 0:1], scale=1.0),
                 reads=[tk["rs"], g.t_const], writes=[tk["rs"]])
            S.op("dve", lambda e: e.reciprocal(out=rs[:], in_=rs[:]), reads=[tk["rs"]], writes=[tk["rs"]])
            for cc in range(8):
                S.op("dve", lambda e, cc=cc: e.tensor_tensor(out=tmp[:], in0=ZCt[:, cc, :], in1=mu[:], op=ALU.subtract),
                     reads=[tk["ZCt"], tk["mu"]], writes=[tk["tmp"]])
                S.op("dve", lambda e: e.tensor_tensor(out=tmp[:], in0=tmp[:], in1=rs[:], op=ALU.mult),
                     reads=[tk["rs"]], writes=[tk["tmp"]])
                S.op("act", lambda e, cc=cc, tsl=tsl: e.activation(out=HN[:, cc, tsl], in_=tmp[:], func=AF.Silu,
                                                                   bias=pvc(g, "clnb_%d" % cc), scale=pvc(g, "clng_%d" % cc)),
                     reads=[tk["tmp"], g.t_pv], writes=[tk["HN"]])
        for kc in range(8):
            for tg in range(4):
                i = (kc * 4 + tg) % 2
                tsl = slice(tg * 512, (tg + 1) * 512)
                S.dma("sp", "mx_ld%d" % i, sz[i][:], g.ORW[kc * 128:(kc + 1) * 128, tsl], reads=[g.t_ORW],
                      writes=[tk["sz%d" % i]])
                S.op("pool", lambda e, i=i, kc=kc, tsl=tsl: e.tensor_copy(out=ORWb[:, kc, tsl], in_=sz[i][:]),
                     reads=[tk["sz%d" % i]], writes=[tk["ORWb"]])
        wcv = g.w_conv_o.rearrange("(kc p) n -> p kc n", p=128)
        wrv = g.w_rwkv_o.rearrange("(kc p) n -> p kc n", p=128)
        for f in range(16):
            fsl = slice(f * 128, (f + 1) * 128)
            for i, wv in enumerate((wcv, wrv)):
                S.dma("sp", "mx_w%d" % i, wf[i][:], wv[:, :, fsl], writes=[tk["wf%d" % i]])
                S.op("pool", lambda e, i=i: e.tensor_copy(out=wb[i][:], in_=wf[i][:]), reads=[tk["wf%d" % i]],
                     writes=[tk["wb%d" % i]])
            for tg in range(4):
                tsl = slice(tg * 512, (tg + 1) * 512)
                ba, bb = (0, 1) if tg % 2 == 0 else (2, 3)
                for kc in range(8):
                    S.op("pe", lambda e, kc=kc, ba=ba, tsl=tsl: e.matmul(banks[ba][:, :], lhsT=wb[0][:, kc, :],
                                                                        rhs=HN[:, kc, tsl], start=(kc == 0), stop=(kc == 7)),
                         reads=[tk["wb0"], tk["HN"]], writes=[tb[ba]])
                for kc in range(8):
                    S.op("pe", lambda e, kc=kc, bb=bb, tsl=tsl: e.matmul(banks[bb][:, :], lhsT=wb[1][:, kc, :],
                                                                        rhs=ORWb[:, kc, tsl], start=(kc == 0), stop=(kc == 7)),
                         reads=[tk["wb1"], tk["ORWb"]], writes=[tb[bb]])
                S.dma("sp", "mx_sz0", sz[0][:], g.SGZ[f * 128:(f + 1) * 128, tsl], reads=[g.t_SGZ], writes=[tk["sz0"]])
                S.dma("sp", "mx_sz1", sz[1][:], g.SGZ[2048 + f * 128:2048 + (f + 1) * 128, tsl], reads=[g.t_SGZ],
                      writes=[tk["sz1"]])
                S.op("dve", lambda e, ba=ba: e.scalar_tensor_tensor(out=t1[:], in0=banks[ba][:, :], scalar=pvc(g, "bco_%d" % f),
                                                                    in1=sz[0][:], op0=ALU.add, op1=ALU.mult),
                     reads=[tb[ba], tk["sz0"], g.t_pv], writes=[tk["t1"]])
                S.op("dve", lambda e, bb=bb: e.tensor_tensor(out=t2[:], in0=banks[bb][:, :], in1=sz[1][:], op=ALU.mult),
                     reads=[tb[bb], tk["sz1"]], writes=[tk["t2"]])
                S.op("pool", lambda e, f=f, tsl=tsl: e.tensor_tensor(out=g.mixT[:, f, tsl], in0=t1[:], in1=t2[:], op=ALU.add),
                     reads=[tk["t1"], tk["t2"]], writes=[g.t_mixT])
    S.barrier()


def phase_out1(g):
    nc, S = g.nc, g.S
    dr = lambda name, shape, dt=F32: nc.dram_tensor(name, list(shape), dt, kind="Internal").ap()
    g.XMID = dr("s_xmid", [T, D])
    g.UT = dr("s_ut", [128, 16, T], BF16)
    g.CT = dr("s_ct", [32, T])
    g.t_XMID, g.t_UT, g.t_CT = Tok(), Tok(), Tok()
    with ExitStack() as es:
        sb = lambda name, shape, dt=F32: es.enter_context(nc.sbuf_tensor("o1_" + name, shape, dt))
        OF = sb("OF", [128, 16, 512])
        wf = [sb("wf0", [128, 16, 128]), sb("wf1", [128, 16, 128])]
        wb = [sb("wb0", [128, 16, 128], BF16), sb("wb1", [128, 16, 128], BF16)]
        xt, un = sb("xt", [128, D]), sb("un", [128, D])
        G1, B1 = sb("G1", [128, D]), sb("B1", [128, D])
        st, mv = sb("st", [128, 4, 6]), sb("mv", [128, 4])
        uTb, uTf = sb("uTb", [128, 16, 128], BF16), sb("uTf", [128, 16, 128])
        WR, BR = sb("WR", [128, 16, 32]), sb("BR", [128, 32])
        lg, ex, m8, sm = sb("lg", [128, 32]), sb("ex", [128, 32]), sb("m8", [128, 8]), sb("sm", [128, 4])
        cTt = sb("cTt", [32, 128])
        banks = [es.enter_context(nc.psum_tensor("o1_bk%d" % i, [128, 512], F32)) for i in range(7)]
        tb = bank_toks(7)
        tk = {n: Tok(n) for n in ("OF", "wf0", "wf1", "wb0", "wb1", "xt", "un", "GB", "st", "uTb", "uTf", "WR", "lg",
                                  "cTt")}
        S.dma("pool", "o1_c", G1[:], g.ln1_g[0:1, :].partition_broadcast(128), writes=[tk["GB"]])
        S.dma("pool", "o1_c", B1[:], g.ln1_b[0:1, :].partition_broadcast(128), writes=[tk["GB"]])
        S.dma("pool", "o1_c", BR[:], g.b_router[0:1, :].partition_broadcast(128), writes=[tk["WR"]])
        S.dma("sp", "o1_c2", WR[:], g.w_router, writes=[tk["WR"]])
        wov = g.w_out.rearrange("(kc p) n -> p kc n", p=128)
        for tg in range(4):
            tsl = slice(tg * 512, (tg + 1) * 512)
            for f in range(16):
                i = f % 2
                S.dma("sp", "o1_w%d" % i, wf[i][:], wov[:, :, f * 128:(f + 1) * 128], writes=[tk["wf%d" % i]])
                S.op("pool", lambda e, i=i: e.tensor_copy(out=wb[i][:], in_=wf[i][:]), reads=[tk["wf%d" % i]],
                     writes=[tk["wb%d" % i]])
                bk = 4 + i
                for kc in range(16):
                    S.op("pe", lambda e, kc=kc, i=i, bk=bk: e.matmul(banks[bk][:, :], lhsT=wb[i][:, kc, :],
                                                                    rhs=g.mixT[:, kc, tsl], start=(kc == 0), stop=(kc == 15)),
                         reads=[tk["wb%d" % i], g.t_mixT], writes=[tb[bk]])
                S.op("dve", lambda e, f=f, bk=bk: e.tensor_scalar(out=OF[:, f, :], in0=banks[bk][:, :],
                                                                  scalar1=pvc(g, "bout_%d" % f), scalar2=g.mod[:, 32 + f, 0:1],
                                                                  op0=ALU.add, op1=ALU.mult),
                     reads=[tb[bk], g.t_pv, g.t_mod], writes=[tk["OF"]])
            for j in range(4):
                tok0 = tg * 512 + j * 128
                S.dma("sp", "o1_x", xt[:], g.x[tok0:tok0 + 128, :], writes=[tk["xt"]])
                for f in range(16):
                    S.op("pe", lambda e, f=f, j=j: e.transpose(
                        banks[f // 4][:, (f % 4) * 128:(f % 4 + 1) * 128], OF[:, f, j * 128:(j + 1) * 128], g.ident[:]),
                         reads=[tk["OF"], g.t_ident], writes=[tb[f // 4]])
                for q in range(4):
                    S.op("dve", lambda e, q=q: e.scalar_tensor_tensor(
                        out=xt[:, q * 512:(q + 1) * 512], in0=xt[:, q * 512:(q + 1) * 512], scalar=ALPHA,
                        in1=banks[q][:, :], op0=ALU.mult, op1=ALU.add), reads=[tb[q]], writes=[tk["xt"]])
                ln_rows(g, xt, tk["xt"], st, mv, tk["st"])
                S.op("dve", lambda e: e.tensor_tensor(out=xt[:], in0=xt[:], in1=G1[:], op=ALU.mult),
                     reads=[tk["GB"]], writes=[tk["xt"]])
                S.op("pool", lambda e: e.tensor_tensor(out=xt[:], in0=xt[:], in1=B1[:], op=ALU.add),
                     reads=[tk["GB"]], writes=[tk["xt"]])
                S.dma("sp", "o1_xm", g.XMID[tok0:tok0 + 128, :], xt[:], reads=[tk["xt"]], writes=[g.t_XMID])
                S.op("pool", lambda e: e.tensor_copy(out=un[:], in_=xt[:]), reads=[tk["xt"]], writes=[tk["un"]])
                ln_rows(g, un, tk["un"], st, mv, tk["st"])
                for f in range(16):
                    S.op("pe", lambda e, f=f: e.transpose(banks[f // 4][:, (f % 4) * 128:(f % 4 + 1) * 128],
                                                          un[:, f * 128:(f + 1) * 128], g.ident[:]),
                         reads=[tk["un"], g.t_ident], writes=[tb[f // 4]])
                for f in range(16):
                    src = banks[f // 4][:, (f % 4) * 128:(f % 4 + 1) * 128]
                    S.op("act", lambda e, f=f, src=src: e.activation(out=uTb[:, f, :], in_=src, func=AF.Identity,
                                                                     bias=g.mod[:, 48 + f, 0:1], scale=g.modp[:, 64 + f, 0:1]),
                         reads=[tb[f // 4], g.t_mod], writes=[tk["uTb"]])
                    S.op("act", lambda e, f=f, src=src: e.activation(out=uTf[:, f, :], in_=src, func=AF.Identity,
                                                                     bias=g.mod[:, 48 + f, 0:1], scale=g.modp[:, 64 + f, 0:1]),
                         reads=[tb[f // 4], g.t_mod], writes=[tk["uTf"]])
                S.dma("sp", "o1_ut", g.UT[:, :, tok0:tok0 + 128], uTb[:], reads=[tk["uTb"]], writes=[g.t_UT])
                for kc in range(16):
                    S.op("pe", lambda e, kc=kc: e.matmul(banks[6][:, 0:32], lhsT=uTf[:, kc, :], rhs=WR[:, kc, :],
                                                        start=(kc == 0), stop=(kc == 15)),
                         reads=[tk["uTf"], tk["WR"]], writes=[tb[6]])
                S.op("dve", lambda e: e.tensor_tensor(out=lg[:], in0=banks[6][:, 0:32], in1=BR[:], op=ALU.add),
                     reads=[tb[6], tk["WR"]], writes=[tk["lg"]])
                S.op("dve", lambda e: e.max(out=m8[:], in_=lg[:]), reads=[tk["lg"]], writes=[tk["lg"]])
                S.op("dve", lambda e: e.tensor_scalar(out=sm[:, 0:1], in0=m8[:, 0:1], scalar1=-1.0, scalar2=None,
                                                      op0=ALU.mult), reads=[tk["lg"]], writes=[tk["lg"]])
                S.op("act", lambda e: e.activation(out=ex[:], in_=lg[:], func=AF.Exp, bias=sm[:, 0:1], scale=1.0),
                     reads=[tk["lg"]], writes=[tk["lg"]])
                S.op("dve", lambda e: e.tensor_scalar(out=lg[:], in0=lg[:], scalar1=m8[:, 3:4], scalar2=None,
                                                      op0=ALU.is_ge), reads=[tk["lg"]], writes=[tk["lg"]])
                S.op("dve", lambda e: e.tensor_tensor(out=ex[:], in0=ex[:], in1=lg[:], op=ALU.mult),
                     reads=[tk["lg"]], writes=[tk["lg"]])
                S.op("dve", lambda e: e.tensor_reduce(out=sm[:, 1:2], in_=ex[:], axis=AX.X, op=ALU.add),
                     reads=[tk["lg"]], writes=[tk["lg"]])
                S.op("dve", lambda e: e.reciprocal(out=sm[:, 2:3], in_=sm[:, 1:2]), reads=[tk["lg"]], writes=[tk["lg"]])
                S.op("dve", lambda e: e.tensor_scalar(out=ex[:], in0=ex[:], scalar1=sm[:, 2:3], scalar2=None,
                                                      op0=ALU.mult), reads=[tk["lg"]], writes=[tk["lg"]])
                S.op("pe", lambda e: e.matmul(banks[6][0:32, 128:256], lhsT=ex[:], rhs=g.ident[:], start=True, stop=True),
                     reads=[tk["lg"], g.t_ident], writes=[tb[6]])
                S.op("act", lambda e: e.activation(out=cTt[:], in_=banks[6][0:32, 128:256], func=AF.Identity),
                     reads=[tb[6]], writes=[tk["cTt"]])
                S.dma("sp", "o1_ct", g.CT[:, tok0:tok0 + 128], cTt[:], reads=[tk["cTt"]], writes=[g.t_CT])
    g.es2.close()
    S.barrier()


def phase_moe(g, out):
    nc, S = g.nc, g.S
    HB = 1024
    for hb in range(2):
        with ExitStack() as es0:
            acc = es0.enter_context(nc.sbuf_tensor("me_acc%d" % hb, [128, 16, HB], F32))
            t_acc = Tok()
            with ExitStack() as es:
                sb = lambda name, shape, dt=F32: es.enter_context(nc.sbuf_tensor("me%d_" % hb + name, shape, dt))
                uTh, hidT = sb("uTh", [128, 16, HB], BF16), sb("hidT", [128, 16, HB], BF16)
                cT, rhe, cb = sb("cT", [32, HB]), sb("rhe", [32, HB]), sb("cb", [128, HB])
                ones32 = sb("ones32", [32, 128])
                wf = [sb("wf0", [128, 16, 128]), sb("wf1", [128, 16, 128])]
                wb = [sb("wb%d" % i, [128, 16, 128], BF16) for i in range(4)]
                t1, t2, t3 = sb("t1", [128, 512]), sb("t2", [128, 512]), sb("t3", [128, 512])
                BGU = sb("BGU", [128, 1024])
                bd = sb("bd", [32, 128])
                banks = [es.enter_context(nc.psum_tensor("me%d_bk%d" % (hb, i), [128, 512], F32)) for i in range(8)]
                tb = bank_toks(8)
                tk = {n: Tok(n) for n in ("uTh", "hidT", "cT", "rhe", "cb", "c", "wf0", "wf1", "wb0", "wb1", "wb2", "wb3",
                                          "t1", "t2", "t3", "BGU", "bd")}
                hsl = slice(hb * HB, (hb + 1) * HB)
                S.dma("sp", "me_ld", uTh[:], g.UT[:, :, hsl], reads=[g.t_UT], writes=[tk["uTh"]])
                S.dma("sp", "me_ld", cT[:], g.CT[:, hsl], reads=[g.t_CT], writes=[tk["cT"]])
                S.dma("sp", "me_ld", BGU[:], g.b_gu, writes=[tk["BGU"]])
                S.op("pool", lambda e: e.memset(ones32[:], 1.0), writes=[tk["c"]])
                st = {"w": 0, "pb": 0}
                for ex_ in range(32):
                    S.op("dve", lambda e, ex_=ex_: e.tensor_scalar(out=rhe[:], in0=cT[:], scalar1=g.ident[0:32, ex_:ex_ + 1],
                                                                   scalar2=None, op0=ALU.mult),
                         reads=[tk["cT"], g.t_ident], writes=[tk["rhe"]])
                    for tg in range(2):
                        S.op("pe", lambda e, tg=tg: e.matmul(banks[6 + tg][:, :], lhsT=ones32[:],
                                                            rhs=rhe[:, tg * 512:(tg + 1) * 512], start=True, stop=True),
                             reads=[tk["rhe"], tk["c"]], writes=[tb[6 + tg]])
                        S.op("act", lambda e, tg=tg: e.activation(out=cb[:, tg * 512:(tg + 1) * 512], in_=banks[6 + tg][:, :],
                                                                  func=AF.Identity), reads=[tb[6 + tg]], writes=[tk["cb"]])
                    wgv = g.w_gu[ex_].rearrange("(kc p) n -> p kc n", p=128)
                    wdv = g.w_dn[ex_].rearrange("(kc p) n -> p kc n", p=128)
                    for j in range(16):
                        pair = (st["w"] % 2) * 2
                        st["w"] += 1
                        for i, c0 in enumerate((j * 128, 2048 + j * 128)):
                            S.dma("sp", "me_w%d" % i, wf[i][:], wgv[:, :, c0:c0 + 128], writes=[tk["wf%d" % i]])
                            S.op("act", lambda e, i=i, pair=pair: e.activation(out=wb[pair + i][:], in_=wf[i][:], func=AF.Identity),
                                 reads=[tk["wf%d" % i]], writes=[tk["wb%d" % (pair + i)]])
                        for tg in range(2):
                            tsl = slice(tg * 512, (tg + 1) * 512)
                            pb = (st["pb"] % 2) * 2
                            st["pb"] += 1
                            for i in range(2):
                                for kc in range(16):
                                    S.op("pe", lambda e, kc=kc, i=i, pb=pb, pair=pair, tsl=tsl: e.matmul(
                                        banks[pb + i][:, :], lhsT=wb[pair + i][:, kc, :], rhs=uTh[:, kc, tsl],
                                        start=(kc == 0), stop=(kc == 15)),
                                         reads=[tk["wb%d" % (pair + i)], tk["uTh"]], writes=[tb[pb + i]])
                            bgc = BGU[:, ex_ * 32 + j:ex_ * 32 + j + 1]
                            buc = BGU[:, ex_ * 32 + 16 + j:ex_ * 32 + 16 + j + 1]
                            S.op("dve", lambda e, pb=pb, bgc=bgc: e.tensor_scalar(out=t1[:], in0=banks[pb][:, :], scalar1=bgc,
                                                                                  scalar2=7.0, op0=ALU.add, op1=ALU.min),
                                 reads=[tb[pb], tk["BGU"]], writes=[tk["t1"]])
                            S.op("act", lambda e: e.activation(out=t2[:], in_=t1[:], func=AF.Sigmoid, scale=1.702),
                                 reads=[tk["t1"]], writes=[tk["t2"]])
                            S.op("dve", lambda e, pb=pb, buc=buc: e.tensor_scalar(out=t3[:], in0=banks[pb + 1][:, :],
                                                                                  scalar1=buc, scalar2=-7.0, op0=ALU.add,
                                                                                  op1=ALU.max),
                                 reads=[tb[pb + 1], tk["BGU"]], writes=[tk["t3"]])
                            S.op("dve", lambda e: e.tensor_scalar(out=t3[:], in0=t3[:], scalar1=7.0, scalar2=1.0, op0=ALU.min,
                                                                  op1=ALU.add), reads=[tk["t3"]], writes=[tk["t3"]])
                            S.op("dve", lambda e: e.tensor_tensor(out=t1[:], in0=t1[:], in1=t2[:], op=ALU.mult),
                                 reads=[tk["t2"]], writes=[tk["t1"]])
                            S.op("pool", lambda e: e.tensor_tensor(out=t1[:], in0=t1[:], in1=t3[:], op=ALU.mult),
                                 reads=[tk["t3"]], writes=[tk["t1"]])
                            S.op("dve", lambda e, j=j, tsl=tsl: e.tensor_tensor(out=hidT[:, j, tsl], in0=t1[:], in1=cb[:, tsl],
                                                                                op=ALU.mult),
                                 reads=[tk["t1"], tk["cb"]], writes=[tk["hidT"]])
                    for f in range(16):
                        wi = st["w"] % 4
                        st["w"] += 1
                        i = f % 2
                        S.dma("sp", "me_w%d" % i, wf[i][:], wdv[:, :, f * 128:(f + 1) * 128], writes=[tk["wf%d" % i]])
                        S.op("act", lambda e, i=i, wi=wi: e.activation(out=wb[wi][:], in_=wf[i][:], func=AF.Identity),
                             reads=[tk["wf%d" % i]], writes=[tk["wb%d" % wi]])
                        if ex_ == 0:
                            S.dma("sp", "me_bd", bd[:], g.b_dn[:, f * 128:(f + 1) * 128], writes=[tk["bd"]])
                        for tg in range(2):
                            tsl = slice(tg * 512, (tg + 1) * 512)
                            bk = 4 + (f * 2 + tg) % 2
                            for kc in range(16):
                                S.op("pe", lambda e, kc=kc, bk=bk, wi=wi, tsl=tsl: e.matmul(
                                    banks[bk][:, :], lhsT=wb[wi][:, kc, :], rhs=hidT[:, kc, tsl], start=(kc == 0),
                                    stop=(kc == 15)), reads=[tk["wb%d" % wi], tk["hidT"]], writes=[tb[bk]])
                            if ex_ == 0:
                                S.op("pe", lambda e, tg=tg, tsl=tsl: e.matmul(banks[6 + tg][:, :], lhsT=bd[:], rhs=cT[:, tsl],
                                                                              start=True, stop=True),
                                     reads=[tk["bd"], tk["cT"]], writes=[tb[6 + tg]])
                                S.op("act", lambda e, f=f, tg=tg, tsl=tsl: e.activation(out=acc[:, f, tsl], in_=banks[6 + tg][:, :],
                                                                                      func=AF.Identity),
                                     reads=[tb[6 + tg]], writes=[t_acc])
                            S.op("dve", lambda e, f=f, bk=bk, tsl=tsl: e.tensor_tensor(out=acc[:, f, tsl], in0=banks[bk][:, :],
                                                                                      in1=acc[:, f, tsl], op=ALU.add),
                                 reads=[tb[bk]], writes=[t_acc])
            S.barrier()
            with ExitStack() as es:
                sb = lambda name, shape, dt=F32: es.enter_context(nc.sbuf_tensor("fin%d_" % hb + name, shape, dt))
                xt = [sb("xt0", [128, D]), sb("xt1", [128, D])]
                G2, B2 = sb("G2", [128, D]), sb("B2", [128, D])
                st_, mv = sb("st", [128, 4, 6]), sb("mv", [128, 4])
                banks = [es.enter_context(nc.psum_tensor("fin%d_bk%d" % (hb, i), [128, 512], F32)) for i in range(4)]
                tb = bank_toks(4)
                tk = {n: Tok(n) for n in ("xt0", "xt1", "GB", "st")}
                t_out = Tok()
                S.dma("pool", "fin_c", G2[:], g.ln2_g[0:1, :].partition_broadcast(128), writes=[tk["GB"]])
                S.dma("pool", "fin_c", B2[:], g.ln2_b[0:1, :].partition_broadcast(128), writes=[tk["GB"]])
                for f in range(16):
                    S.op("dve", lambda e, f=f: e.tensor_scalar(out=acc[:, f, :], in0=acc[:, f, :], scalar1=g.mod[:, 80 + f, 0:1],
                                                               scalar2=None, op0=ALU.mult), reads=[g.t_mod], writes=[t_acc])
                for j in range(HB // 128):
                    b = j % 2
                    tok0 = hb * HB + j * 128
                    S.dma("sp", "fin_x%d" % b, xt[b][:], g.XMID[tok0:tok0 + 128, :], reads=[g.t_XMID], writes=[tk["xt%d" % b]])
                    for f in range(16):
                        S.op("pe", lambda e, f=f, j=j: e.transpose(
                            banks[f // 4][:, (f % 4) * 128:(f % 4 + 1) * 128], acc[:, f, j * 128:(j + 1) * 128], g.ident[:]),
                             reads=[t_acc, g.t_ident], writes=[tb[f // 4]])
                    for q in range(4):
                        S.op("dve", lambda e, q=q, b=b: e.scalar_tensor_tensor(
                            out=xt[b][:, q * 512:(q + 1) * 512], in0=xt[b][:, q * 512:(q + 1) * 512], scalar=ALPHA,
                            in1=banks[q][:, :], op0=ALU.mult, op1=ALU.add), reads=[tb[q]], writes=[tk["xt%d" % b]])
                    ln_rows(g, xt[b], tk["xt%d" % b], st_, mv, tk["st"])
                    S.op("dve", lambda e, b=b: e.tensor_tensor(out=xt[b][:], in0=xt[b][:], in1=G2[:], op=ALU.mult),
                         reads=[tk["GB"]], writes=[tk["xt%d" % b]])
                    S.op("pool", lambda e, b=b: e.tensor_tensor(out=xt[b][:], in0=xt[b][:], in1=B2[:], op=ALU.add),
                         reads=[tk["GB"]], writes=[tk["xt%d" % b]])
                    S.dma("sp", "fin_o%d" % b, out[tok0:tok0 + 128, :], xt[b][:], reads=[tk["xt%d" % b]],
                          writes=[t_out, tk["xt%d" % b]])
                g.out_toks.append(t_out)
            S.barrier()


def phase_placeholder_out(g, out):
    nc, S = g.nc, g.S
    with ExitStack() as es:
        bufs = [es.enter_context(nc.sbuf_tensor("po%d" % i, [128, D], F32)) for i in range(2)]
        toks = [Tok(), Tok()]
        t_out = Tok()
        for tt in range(NT):
            b = tt % 2
            S.dma("sp", "po_ld%d" % b, bufs[b][:], g.x[tt * 128:(tt + 1) * 128, :], writes=[toks[b]])
            S.dma("sp", "po_st%d" % b, out[tt * 128:(tt + 1) * 128, :], bufs[b][:], reads=[toks[b]], writes=[t_out])
        S.finish([t_out])


def make_in_maps(inp, ncores=8):
    maps = []
    for b in range(ncores):
        m = {}
        m["x"] = np.ascontiguousarray(inp["x"][b])
        m["ctx"] = np.ascontiguousarray(inp["ctx"][b])
        cc = np.stack([inp["c"][b], inp["c_ctx"]], axis=-1)
        m["cc"] = np.ascontiguousarray(cc.reshape(16, 128, 2).transpose(1, 0, 2))
        m["w_ada"] = np.ascontiguousarray(inp["w_ada"][0])
        m["b_ada"] = np.ascontiguousarray(inp["b_ada"][0].reshape(96, 128).T)
        m["w_in"] = np.ascontiguousarray(inp["w_in"][0])
        m["pv"] = make_pv(inp)
        m["g2"] = np.ascontiguousarray(inp["g2"][0])
        m["lnx_g"] = np.ascontiguousarray(inp["lnx_g"][0].reshape(16, 64))
        m["lnx_b"] = np.ascontiguousarray(inp["lnx_b"][0].reshape(16, 64))
        m["w_conv_o"] = np.ascontiguousarray(inp["w_conv_o"][0])
        m["w_rwkv_o"] = np.ascontiguousarray(inp["w_rwkv_o"][0])
        m["w_out"] = np.ascontiguousarray(inp["w_out"][0])
        for nm in ("ln1_g", "ln1_b", "ln2_g", "ln2_b", "b_router"):
            m[nm] = np.ascontiguousarray(inp[nm][0][None, :])
        m["w_router"] = np.ascontiguousarray(inp["w_router"][0].reshape(16, 128, 32).transpose(1, 0, 2))
        m["w_gu"] = np.ascontiguousarray(inp["w_gate_up"][0])
        m["b_gu"] = np.ascontiguousarray(inp["b_gate_up"][0].reshape(32, 32, 128).transpose(2, 0, 1).reshape(128, 1024))
        m["w_dn"] = np.ascontiguousarray(inp["w_down"][0])
        m["b_dn"] = np.ascontiguousarray(inp["b_down"][0])
        for nm, key in (("w2bd", "w2"), ("a2bd", "a2")):
            bd = np.zeros((16, 128, 128), np.float32)
            for h in range(16):
                for d in range(2):
                    bd[h, d * 64:(d + 1) * 64, d * 64:(d + 1) * 64] = inp[key][0, d, :, h * 64:(h + 1) * 64]
            m[nm] = bd
        maps.append(m)
    return maps


def kernel(**inputs):
    inp = {k: np.asarray(v) for k, v in inputs.items()}
    nc = build_program()
    in_maps = make_in_maps(inp)
    res = run_bass_kernel_spmd(nc, in_maps, core_ids=list(range(8)))
    return np.stack([r["out"] for r in res.results], axis=0)
```

```python
from contextlib import ExitStack
import numpy as np
import concourse.bass as bass
import concourse.mybir as mybir
from concourse.bass_utils import run_bass_kernel_spmd

F32 = mybir.dt.float32
BF16 = mybir.dt.bfloat16
AF = mybir.ActivationFunctionType
ALU = mybir.AluOpType
AX = mybir.AxisListType

D = 2048
T = 2048
TC = 256
NT = T // 128
NTC = TC // 128
TT = T + TC
P_IN = 9632
LN_EPS = 1e-5
ALPHA = 2.0 ** 0.25


class Tok:
    __slots__ = ("w", "r", "name", "excl")

    def __init__(self, name="", excl=False):
        self.w = {}
        self.r = {}
        self.name = name
        self.excl = excl


class Sched:
    EPOCH = 30000
    DMA_EPOCH = 1800

    def __init__(self, nc):
        self.nc = nc
        self.eng = {"pe": nc.tensor, "act": nc.scalar, "dve": nc.vector, "pool": nc.gpsimd, "sp": nc.sync}
        self.sem = {}
        self.cnt = {}
        self.seen = {e: {} for e in self.eng}
        self.nsem = 0
        self.dsem = {}
        self.dma_issued = {}
        self.ninst = 0
        for e in self.eng:
            self._new_sem(e)

    def _new_sem(self, e):
        self.sem[e] = self.nc.alloc_semaphore(name="se_%s_%d" % (e, self.nsem))
        self.nsem += 1
        self.cnt[e] = 0

    def _wait(self, e, deps):
        seen = self.seen[e]
        best = {}
        own = self.sem[e].num
        for (semh, val) in deps:
            k = semh.num
            if e == "pe" and k == own:
                continue
            if k in self.dma_issued:
                val = self.dma_issued[k]
            if seen.get(k, 0) >= val:
                continue
            if k not in best or best[k][1] < val:
                best[k] = (semh, val)
        for k, (semh, val) in best.items():
            self.eng[e].wait_ge(semh, val)
            seen[k] = val
            self.ninst += 1

    def _deps(self, reads, writes):
        deps = []
        for t in reads:
            deps.extend(t.w.values())
        for t in writes:
            deps.extend(t.w.values())
            deps.extend(t.r.values())
        return deps

    def pe_rg(self, rg):
        self.next_rg = rg

    def op(self, e, fn, reads=(), writes=()):
        if e == "pe":
            rg = getattr(self, "next_rg", 0)
            self.next_rg = 0
            if rg != getattr(self, "last_rg", 0) and self.cnt["pe"] > 0:
                self.eng["pe"].wait_ge(self.sem["pe"], self.cnt["pe"])
                self.ninst += 1
            self.last_rg = rg
        ex = [t for t in reads if t.excl]
        if ex:
            reads = [t for t in reads if not t.excl]
            writes = list(writes) + ex
        self._wait(e, self._deps(reads, writes))
        if self.cnt[e] >= self.EPOCH:
            self._new_sem(e)
        inst = fn(self.eng[e])
        self.cnt[e] += 1
        self.ninst += 1
        inst.then_inc(self.sem[e], 1)
        rec = (self.sem[e], self.cnt[e])
        k = self.sem[e].num
        for t in reads:
            t.r[k] = rec
        for t in writes:
            t.w[k] = rec
        return inst

    def dma(self, q, sname, out, in_, reads=(), writes=(), **kw):
        self._wait(q, self._deps(reads, writes))
        ent = self.dsem.get(sname)
        if ent is None or self.dma_issued[ent.num] >= 16 * self.DMA_EPOCH:
            ent = self.nc.alloc_semaphore(name="sd_%s_%d" % (sname, self.nsem))
            self.nsem += 1
            self.dsem[sname] = ent
            self.dma_issued[ent.num] = 0
        inst = self.eng[q].dma_start(out=out, in_=in_, **kw)
        self.ninst += 1
        self.dma_issued[ent.num] += 16
        inst.then_inc(ent, 16)
        rec = (ent, self.dma_issued[ent.num])
        for t in reads:
            t.r[ent.num] = rec
        for t in writes:
            t.w[ent.num] = rec
        return inst

    def barrier(self):
        for e in self.eng:
            deps = [(self.sem[o], self.cnt[o]) for o in self.eng if o != e and self.cnt[o] > 0]
            for k, ent in self.dsem.items():
                deps.append((ent, self.dma_issued[ent.num]))
            self._wait(e, deps)

    def finish(self, toks):
        deps = []
        for t in toks:
            deps.extend(t.w.values())
        self._wait("sp", deps)


class Ctx:
    pass


def build_program(debug=None):
    nc = bass.Bass("TRN2", target_bir_lowering=False)
    S = Sched(nc)
    g = Ctx()
    g.nc = nc
    g.S = S
    g.debug = debug
    if debug is not None and debug.startswith("rwkv1"):
        g.nheads = 1
        g.rw_stop = int(debug[5:6] or 9)
        g.no_bonus = debug.endswith("nb")
        debug = "rwkv"

    def dram_in(name, shape, dt=F32):
        return nc.dram_tensor(name, list(shape), dt, kind="ExternalInput").ap()

    def dram_out(name, shape, dt=F32):
        return nc.dram_tensor(name, list(shape), dt, kind="ExternalOutput").ap()

    g.x = dram_in("x", [T, D])
    g.ctx = dram_in("ctx", [TC, D])
    g.cc = dram_in("cc", [128, 16, 2])
    g.w_ada = dram_in("w_ada", [D, 6 * D])
    g.b_ada = dram_in("b_ada", [128, 96])
    g.w_in = dram_in("w_in", [D, P_IN])
    g.pv_in = dram_in("pv", [128, NPV])
    g.g2 = dram_in("g2", [160, 1024])
    g.w2bd = dram_in("w2bd", [16, 128, 128])
    g.a2bd = dram_in("a2bd", [16, 128, 128])
    g.lnx_g = dram_in("lnx_g", [16, 64])
    g.lnx_b = dram_in("lnx_b", [16, 64])
    g.w_conv_o = dram_in("w_conv_o", [1024, D])
    g.w_rwkv_o = dram_in("w_rwkv_o", [1024, D])
    g.w_out = dram_in("w_out", [D, D])
    g.ln1_g = dram_in("ln1_g", [1, D])
    g.ln1_b = dram_in("ln1_b", [1, D])
    g.ln2_g = dram_in("ln2_g", [1, D])
    g.ln2_b = dram_in("ln2_b", [1, D])
    g.w_router = dram_in("w_router", [128, 16, 32])
    g.b_router = dram_in("b_router", [1, 32])
    g.w_gu = dram_in("w_gu", [32, D, 2 * D])
    g.b_gu = dram_in("b_gu", [128, 1024])
    g.w_dn = dram_in("w_dn", [32, D, D])
    g.b_dn = dram_in("b_dn", [32, D])

    g.ident = nc.alloc_sbuf_tensor("ident", [128, 128], F32)
    g.anti = nc.alloc_sbuf_tensor("anti", [128, 128], F32)
    g.eps_ln = nc.alloc_sbuf_tensor("eps_ln", [128, 1], F32)
    g.t_const = Tok()
    S.op("pool", lambda e: e.memset(g.eps_ln[:], LN_EPS), writes=[g.t_const])
    g.t_ident = Tok()
    make_identity(g, g.ident, g.anti, g.t_ident)
    g.pv = nc.alloc_sbuf_tensor("pv_sb", [128, NPV], F32)
    g.t_pv = Tok()
    S.dma("sp", "ld_small", g.pv[:], g.pv_in, writes=[g.t_pv])
    phase_mod(g)
    phase_ln1(g, False)
    if debug == "ln1":
        o = dram_out("dbg_xmT", [128, 16, TT], BF16)

        tk = Tok()
        S.dma("sp", "dbg", o, g.xmT[:], reads=[g.t_xmT], writes=[tk])
        o2 = dram_out("dbg_mod", [128, 96, 2], F32)
        S.dma("sp", "dbg", o2, g.mod[:], reads=[g.t_mod], writes=[tk])
        S.finish([tk])
        return nc
    phase_proj(g, False)
    phase_ln1(g, True)
    phase_proj(g, True)
    g.es1.close()
    if debug == "proj":
        tk = Tok()
        for nm, src, tok in (("RKV", g.RKV, g.t_RKV), ("SGs", g.SGs, g.t_SGs), ("AGs", g.AGs, g.t_AGs),
                             ("Gs", g.Gs, g.t_Gs), ("ZC", g.ZC, g.t_ZC), ("SGZ", g.SGZ, g.t_SGZ)):
            o = dram_out("dbg_" + nm, list(src.shape), F32)
            S.dma("sp", "dbg", o, src, reads=[tok], writes=[tk])
        S.finish([tk])
        return nc
    phase_rwkv(g)
    if debug is None or debug == "full1":
        phase_mix(g)
        phase_out1(g)
        g.out_toks = []
        phase_moe(g, dram_out("out", [T, D], F32))
        S.finish(g.out_toks)
        return nc
    if debug == "rwkv":
        tk = Tok()
        o = dram_out("dbg_ORW", [1024, T], F32)
        S.dma("sp", "dbg", o, g.ORW, reads=[g.t_ORW], writes=[tk])
        for nm, buf in g.rw_dbg.items():
            o = dram_out("dbg_" + nm, list(buf.shape), F32)
            S.dma("sp", "dbg", o, buf, reads=[], writes=[tk])
        S.finish([tk])
        return nc
    return nc


def phase_mod(g):
    nc, S = g.nc, g.S
    g.mod = nc.alloc_sbuf_tensor("mod", [128, 96, 2], F32)
    g.t_mod = Tok("mod")
    g.modp = nc.alloc_sbuf_tensor("modp", [128, 96, 2], F32)
    sc = nc.alloc_sbuf_tensor("sc", [128, 16, 2], F32)
    t_sc = Tok()
    bada = nc.alloc_sbuf_tensor("bada", [128, 96], F32)
    t_b = Tok()
    S.dma("sp", "ld_small", sc[:], g.cc, writes=[t_sc])
    S.dma("sp", "ld_small", bada[:], g.b_ada, writes=[t_b])
    S.op("act", lambda e: e.activation(out=sc[:], in_=sc[:], func=AF.Silu), reads=[t_sc], writes=[t_sc])
    NG = 24
    wsrc = g.w_ada.rearrange("(kc p) n -> p kc n", p=128)
    with nc.sbuf_tensor("wada0", [128, 16, 512], F32) as wb0, nc.sbuf_tensor("wada1", [128, 16, 512], F32) as wb1, \
            nc.psum_tensor("ps_mod", [128, 96, 2], F32) as ps:
        wb = [wb0, wb1]
        t_wb = [Tok(), Tok()]
        t_ps = Tok(excl=True)
        for gi in range(NG):
            b = gi % 2
            S.dma("sp" if gi % 2 == 0 else "pool", "ld_wada%d" % b, wb[b][:], wsrc[:, :, gi * 512:(gi + 1) * 512],
                  writes=[t_wb[b]])
            for j in range(4):
                fo = gi * 4 + j
                for kc in range(16):
                    S.op("pe", lambda e, kc=kc, j=j, fo=fo, b=b: e.matmul(
                        ps[:, fo, :], lhsT=wb[b][:, kc, j * 128:(j + 1) * 128], rhs=sc[:, kc, :],
                        start=(kc == 0), stop=(kc == 15)),
                         reads=[t_wb[b], t_sc], writes=[t_ps])
        for i in range(2):
            S.op("dve", lambda e, i=i: e.tensor_tensor(out=g.mod[:, :, i], in0=ps[:, :, i], in1=bada[:], op=ALU.add),
                 reads=[t_ps, t_b], writes=[g.t_mod])
    S.op("dve", lambda e: e.tensor_scalar(out=g.modp[:], in0=g.mod[:], scalar1=1.0, scalar2=None, op0=ALU.add),
         reads=[g.t_mod], writes=[g.t_mod])
    S.barrier()


def phase_ln1(g, rev_pass):
    nc, S = g.nc, g.S
    if not rev_pass:
        g.es1 = ExitStack()
        g.xmT = g.es1.enter_context(nc.sbuf_tensor("xmT", [128, 16, TT], BF16))
        g.t_xmT = Tok("xmT")
    sfx = "r" if rev_pass else "f"
    with ExitStack() as es:
        sb = lambda name, shape, dt=F32: es.enter_context(nc.sbuf_tensor(name + sfx, shape, dt))
        psb = lambda name, shape, dt=F32: es.enter_context(nc.psum_tensor(name + sfx, shape, dt))
        xt = [sb("xt0", [128, D]), sb("xt1", [128, D])]
        st, mv = sb("ln_st", [128, 4, 6]), sb("ln_mv", [128, 4])
        pst = [psb("ps_tr%d" % i, [128, 4, 128]) for i in range(4)]
        t_xt = [Tok(), Tok()]
        t_st = Tok()
        t_pst = [Tok(excl=True) for _ in range(4)]
        for tt in range(NT + NTC):
            b = tt % 2
            if tt < NT:
                src = g.x[tt * 128:(tt + 1) * 128, :]
                col = 0
                pos = TC + (NT - 1 - tt) * 128 if rev_pass else TC + tt * 128
            else:
                src = g.ctx[(tt - NT) * 128:(tt - NT + 1) * 128, :]
                col = 1
                pos = (NTC - 1 - (tt - NT)) * 128 if rev_pass else (tt - NT) * 128
            S.dma("sp", "ld_x%d" % b, xt[b][:], src, writes=[t_xt[b]])
            ln_rows(g, xt[b], t_xt[b], st, mv, t_st)
            for fc in range(16):
                pb = fc // 4
                if rev_pass:
                    S.op("pe", lambda e, fc=fc, pb=pb, b=b: e.matmul(
                        pst[pb][:, fc % 4, :], lhsT=xt[b][:, fc * 128:(fc + 1) * 128], rhs=g.anti[:],
                        start=True, stop=True), reads=[t_xt[b], g.t_ident], writes=[t_pst[pb]])
                else:
                    S.op("pe", lambda e, fc=fc, pb=pb, b=b: e.transpose(
                        pst[pb][:, fc % 4, :], xt[b][:, fc * 128:(fc + 1) * 128], g.ident[:]),
                         reads=[t_xt[b], g.t_ident], writes=[t_pst[pb]])
                if fc % 4 == 3:
                    for f2 in range(fc - 3, fc + 1):
                        S.op("act", lambda e, f2=f2, pb=pb, pos=pos, col=col: e.activation(
                            out=g.xmT[:, f2, pos:pos + 128], in_=pst[pb][:, f2 % 4, :], func=AF.Identity,
                            bias=g.mod[:, f2, col:col + 1], scale=g.modp[:, 16 + f2, col:col + 1]),
                             reads=[t_pst[pb], g.t_mod], writes=[g.t_xmT])
    S.barrier()


def ln_rows(g, xt, t_x, st, mv, t_st, n=4):
    S = g.S
    for q in range(n):
        S.op("dve", lambda e, q=q: e.bn_stats(out=st[:, q, :], in_=xt[:, q * 512:(q + 1) * 512]),
             reads=[t_x], writes=[t_st])
    S.op("dve", lambda e: e.bn_aggr(out=mv[:, 0:2], in_=st[:, 0:n, :].rearrange("p a b -> p (a b)")),
         reads=[t_st], writes=[t_st])
    S.op("act", lambda e: e.activation(out=mv[:, 3:4], in_=mv[:, 1:2], func=AF.Sqrt, bias=g.eps_ln[:, 0:1],
                                       scale=1.0), reads=[t_st, g.t_const], writes=[t_st])
    S.op("dve", lambda e: e.reciprocal(out=mv[:, 2:3], in_=mv[:, 3:4]), reads=[t_st], writes=[t_st])
    S.op("dve", lambda e: e.tensor_scalar(out=xt[:, 0:n * 512], in0=xt[:, 0:n * 512], scalar1=mv[:, 0:1],
                                          scalar2=mv[:, 2:3], op0=ALU.subtract, op1=ALU.mult),
         reads=[t_st, t_x], writes=[t_x])


def make_identity(g, ident, anti, tok):
    nc, S = g.nc, g.S
    S.op("pool", lambda e: e.memset(ident[:], 0.0), writes=[tok])
    S.op("pool", lambda e: e.memset(anti[:], 0.0), writes=[tok])
    S.op("pool", lambda e: e.affine_select(out=ident[:], in_=ident[:], pattern=[[-1, 128]],
                                           compare_op=ALU.not_equal, fill=1.0, base=0, channel_multiplier=1),
         reads=[tok], writes=[tok])
    S.op("pool", lambda e: e.affine_select(out=anti[:], in_=anti[:], pattern=[[1, 128]],
                                           compare_op=ALU.not_equal, fill=1.0, base=-127, channel_multiplier=1),
         reads=[tok], writes=[tok])


def pv_layout():
    names = []
    for q in range(3):
        for h in range(16):
            names += ["b_%d_%d" % (q, h), "cp_%d_%d" % (q, h), "cn_%d_%d" % (q, h)]
    for nm in ("wd", "ad", "gd1", "gd2"):
        names += ["b_" + nm, "cp_" + nm, "cn_" + nm]
    for h in range(16):
        names += ["w0_%d" % h, "a0_%d" % h, "kk_%d" % h, "ka_%d" % h, "rk_%d" % h]
    for c in range(8):
        names += ["cba_%d" % c, "cbg_%d" % c, "convb_%d" % c, "clng_%d" % c, "clnb_%d" % c]
        names += ["cw_%d_%d" % (c, j) for j in range(31)]
    for c in range(32):
        names += ["bzg_%d" % c]
    for c in range(16):
        names += ["bco_%d" % c, "bout_%d" % c]
    return {n: i for i, n in enumerate(names)}


PVL = pv_layout()
NPV = len(PVL)


def make_pv(inp):
    pv = np.zeros((128, NPV), np.float32)
    b_in = inp["b_in"][0]
    mu = inp["shift_mu"][0]

    def dup(v):
        return np.concatenate([v, v])
    for q in range(3):
        for h in range(16):
            c0 = 2048 + q * 1024 + h * 64
            zc = c0 - 2048
            pv[:, PVL["b_%d_%d" % (q, h)]] = dup(b_in[c0:c0 + 64])
            pv[:, PVL["cp_%d_%d" % (q, h)]] = np.concatenate([mu[0, zc:zc + 64], mu[1, zc:zc + 64]])
            pv[:, PVL["cn_%d_%d" % (q, h)]] = np.concatenate([mu[1, zc:zc + 64], mu[0, zc:zc + 64]])
    for nm, c0 in (("wd", 5120), ("ad", 5248)):
        zc = c0 - 2048
        pv[:, PVL["b_" + nm]] = b_in[c0:c0 + 128]
        pv[:, PVL["cp_" + nm]] = np.concatenate([mu[0, zc:zc + 64], mu[1, zc + 64:zc + 128]])
        pv[:, PVL["cn_" + nm]] = np.concatenate([mu[1, zc:zc + 64], mu[0, zc + 64:zc + 128]])
    pv[:, PVL["b_gd1"]] = b_in[5376:5504]
    pv[:, PVL["cp_gd1"]] = mu[0, 5376 - 2048:5504 - 2048]
    pv[:, PVL["cn_gd1"]] = mu[1, 5376 - 2048:5504 - 2048]
    pv[:32, PVL["b_gd2"]] = b_in[5504:5536]
    pv[:32, PVL["cp_gd2"]] = mu[0, 5504 - 2048:5536 - 2048]
    pv[:32, PVL["cn_gd2"]] = mu[1, 5504 - 2048:5536 - 2048]
    for h in range(16):
        sl = slice(h * 64, h * 64 + 64)
        pv[:, PVL["w0_%d" % h]] = np.concatenate([inp["w0"][0, 0, sl], inp["w0"][0, 1, sl]])
        pv[:, PVL["a0_%d" % h]] = np.concatenate([inp["a0"][0, 0, sl], inp["a0"][0, 1, sl]])
        pv[:, PVL["kk_%d" % h]] = dup(inp["k_k"][0, sl])
        pv[:, PVL["ka_%d" % h]] = dup(inp["k_a"][0, sl])
        pv[:, PVL["rk_%d" % h]] = dup(inp["r_k"][0, h])
    for c in range(8):
        sl = slice(c * 128, c * 128 + 128)
        pv[:, PVL["cba_%d" % c]] = b_in[sl]
        pv[:, PVL["cbg_%d" % c]] = b_in[1024 + c * 128:1024 + c * 128 + 128]
        pv[:, PVL["convb_%d" % c]] = inp["conv_b"][0, sl]
        pv[:, PVL["clng_%d" % c]] = inp["conv_ln_g"][0, sl]
        pv[:, PVL["clnb_%d" % c]] = inp["conv_ln_b"][0, sl]
        for j in range(31):
            pv[:, PVL["cw_%d_%d" % (c, j)]] = inp["conv_w"][0, j, sl]
    for c in range(32):
        pv[:, PVL["bzg_%d" % c]] = b_in[5536 + c * 128:5536 + c * 128 + 128]
    for c in range(16):
        pv[:, PVL["bco_%d" % c]] = inp["b_conv_o"][0, c * 128:c * 128 + 128]
        pv[:, PVL["bout_%d" % c]] = inp["b_out"][0, c * 128:c * 128 + 128]
    return pv


def pvc(g, name):
    i = PVL[name]
    return g.pv[:, i:i + 1]


TG_ALL = [(0, 512), (512, 512), (1024, 512), (1536, 512), (2048, 256)]
TG_LAT = [(TC + i * 512, 512) for i in range(4)]


def phase_proj(g, rev_pass):
    nc, S = g.nc, g.S
    if not rev_pass:
        dr = lambda name, shape, dt=F32: nc.dram_tensor(name, list(shape), dt, kind="Internal").ap()
        g.RKV = dr("s_rkv", [3, 16, 128, TT])
        g.SGs = dr("s_sg", [16, 128, TT])
        g.AGs = dr("s_ag", [16, 128, TT])
        g.Gs = dr("s_g", [T, 1024])
        g.ZC = dr("s_zc", [1024, T])
        g.SGZ = dr("s_sgz", [4096, T])
        g.t_RKV, g.t_SGs, g.t_AGs, g.t_Gs, g.t_ZC, g.t_SGZ = (Tok() for _ in range(6))
        g.wdt = g.es1.enter_context(nc.sbuf_tensor("wdt", [128, TT], F32))
        g.ads = g.es1.enter_context(nc.sbuf_tensor("ads", [128, TT], F32))
        g.t_wdt, g.t_ads = Tok(), Tok()
    sfx = "r" if rev_pass else "f"
    wsrc = g.w_in.rearrange("(kc p) n -> p kc n", p=128)
    with ExitStack() as es:
        sb = lambda name, shape, dt=F32: es.enter_context(nc.sbuf_tensor(name + sfx, shape, dt))
        psb = lambda name, shape, dt=F32: es.enter_context(nc.psum_tensor(name + sfx, shape, dt))
        wf = [sb("wf0", [128, 16, 128]), sb("wf1", [128, 16, 128])]
        wb = [sb("wb0", [128, 16, 128], BF16), sb("wb1", [128, 16, 128], BF16)]
        zraw = sb("zraw", [128, TT])
        zs = [sb("zs0", [128, TT]), sb("zs1", [128, TT])]
        c0t = sb("c0t", [128, 1])
        lw2 = sb("lw2", [128, 2, 128])
        pp = [psb("pp%d" % i, [128, 512]) for i in range(4)]
        t_wf = [Tok(), Tok()]
        t_wb = [Tok(), Tok()]
        t_pp = [Tok(excl=True) for _ in range(4)]
        t_zraw, t_c0 = Tok(), Tok()
        t_zs = [Tok(), Tok()]
        st = {"wi": 0, "pi": 0, "zi": 0}

        def load_w(col0, M, dupl=False):
            i = st["wi"] % 2
            st["wi"] += 1
            S.dma("sp", "ld_w%d" % i, wf[i][:, :, 0:M], wsrc[:, :, col0:col0 + M], writes=[t_wf[i]])
            S.op("pool", lambda e: e.tensor_copy(out=wb[i][:, :, 0:M], in_=wf[i][:, :, 0:M]),
                 reads=[t_wf[i]], writes=[t_wb[i]])
            if dupl:
                S.op("pool", lambda e: e.tensor_copy(out=wb[i][:, :, M:2 * M], in_=wf[i][:, :, 0:M]),
                     reads=[t_wf[i]], writes=[t_wb[i]])
            return i

        def mm_group(i, M, p0, n):
            pi = st["pi"] % 4
            st["pi"] += 1
            for kc in range(16):
                S.op("pe", lambda e, kc=kc: e.matmul(pp[pi][0:M, 0:n], lhsT=wb[i][:, kc, 0:M],
                                                      rhs=g.xmT[:, kc, p0:p0 + n], start=(kc == 0), stop=(kc == 15)),
                     reads=[t_wb[i], g.t_xmT], writes=[t_pp[pi]])
            return pi

        def shift(zin, t_in, zout, t_out, plo, phi, cp, cn, segs):
            ps_ = slice(plo, phi)
            S.op("dve", lambda e: e.tensor_scalar(out=c0t[ps_, :], in0=cp[ps_, :], scalar1=cn[ps_, :], scalar2=-1.0,
                                                  op0=ALU.add, op1=ALU.mult), reads=[g.t_pv], writes=[t_c0])
            S.op("dve", lambda e: e.tensor_scalar(out=c0t[ps_, :], in0=c0t[ps_, :], scalar1=1.0, scalar2=None,
                                                  op0=ALU.add), reads=[t_c0], writes=[t_c0])
            lo, hi = segs[0][0], segs[-1][1]
            S.op("dve", lambda e: e.tensor_scalar(out=zout[ps_, lo:hi], in0=zin[ps_, lo:hi], scalar1=c0t[ps_, :],
                                                  scalar2=None, op0=ALU.mult), reads=[t_in, t_c0], writes=[t_out])
            for (a, b) in segs:
                S.op("dve", lambda e, a=a, b=b: e.scalar_tensor_tensor(
                    out=zout[ps_, a + 1:b], in0=zin[ps_, a:b - 1], scalar=cp[ps_, :], in1=zout[ps_, a + 1:b],
                    op0=ALU.mult, op1=ALU.add), reads=[t_in, g.t_pv], writes=[t_out])
                S.op("dve", lambda e, a=a, b=b: e.scalar_tensor_tensor(
                    out=zout[ps_, a:b - 1], in0=zin[ps_, a + 1:b], scalar=cn[ps_, :], in1=zout[ps_, a:b - 1],
                    op0=ALU.mult, op1=ALU.add), reads=[t_in, g.t_pv], writes=[t_out])

        SEG2 = [(0, TC), (TC, TT)]

        def one_pass(d, col0, M, dupl, bias, cp, cn, zout, t_out):
            i = load_w(col0, M, dupl)
            lo, hi = d * 64, d * 64 + 64
            for (p0, n) in TG_ALL:
                pi = mm_group(i, 128, p0, n)
                S.op("act", lambda e, pi=pi, p0=p0, n=n: e.activation(
                    out=zraw[lo:hi, p0:p0 + n], in_=pp[pi][lo:hi, 0:n], func=AF.Identity,
                    bias=bias[lo:hi, :], scale=1.0), reads=[t_pp[pi], g.t_pv], writes=[t_zraw])
            shift(zraw, t_zraw, zout, t_out, lo, hi, cp, cn, SEG2)

        def rkv_pass(d):
            lo, hi = d * 64, d * 64 + 64
            one_pass(d, 5120, 128, False, pvc(g, "b_wd"), pvc(g, "cp_wd"), pvc(g, "cn_wd"), g.wdt, g.t_wdt)
            S.op("act", lambda e: e.activation(out=g.wdt[lo:hi, :], in_=g.wdt[lo:hi, :], func=AF.Tanh),
                 reads=[g.t_wdt], writes=[g.t_wdt])
            one_pass(d, 5248, 128, False, pvc(g, "b_ad"), pvc(g, "cp_ad"), pvc(g, "cn_ad"), g.ads, g.t_ads)
            for q in range(3):
                for h in range(16):
                    zi = st["zi"] % 2
                    st["zi"] += 1
                    one_pass(d, 2048 + q * 1024 + h * 64, 64, True, pvc(g, "b_%d_%d" % (q, h)),
                             pvc(g, "cp_%d_%d" % (q, h)), pvc(g, "cn_%d_%d" % (q, h)), zs[zi], t_zs[zi])
                    S.dma("act", "st_a%d" % zi, g.RKV[q, h, lo:hi, :], zs[zi][lo:hi, :], reads=[t_zs[zi]],
                          writes=[g.t_RKV])

        def lora_stage():
            t_lw = Tok()
            for h in range(16):
                S.dma("sp", "ld_small", lw2[:, 0, :], g.w2bd[h], writes=[t_lw])
                S.dma("sp", "ld_small", lw2[:, 1, :], g.a2bd[h], writes=[t_lw])
                for k, (src, t_src, bname, dstd, t_dst) in enumerate(
                        ((g.wdt, g.t_wdt, "w0_%d" % h, g.SGs, g.t_SGs), (g.ads, g.t_ads, "a0_%d" % h, g.AGs, g.t_AGs))):
                    zi = k
                    for (p0, n) in TG_ALL:
                        pi = st["pi"] % 4
                        st["pi"] += 1
                        S.op("pe", lambda e, k=k, pi=pi, p0=p0, n=n, src=src: e.matmul(
                            pp[pi][:, 0:n], lhsT=lw2[:, k, :], rhs=src[:, p0:p0 + n], start=True, stop=True),
                             reads=[t_lw, t_src], writes=[t_pp[pi]])
                        S.op("act", lambda e, pi=pi, p0=p0, n=n, zi=zi, bname=bname: e.activation(
                            out=zs[zi][:, p0:p0 + n], in_=pp[pi][:, 0:n], func=AF.Sigmoid, bias=pvc(g, bname),
                            scale=1.0), reads=[t_pp[pi], g.t_pv], writes=[t_zs[zi]])
                    S.dma("act", "st_a%d" % zi, dstd[h], zs[zi][:], reads=[t_zs[zi]], writes=[t_dst])

        if rev_pass:
            rkv_pass(1)
            lora_stage()
        else:
            rkv_pass(0)
            sgd1, sgd2 = sb("sgd1", [128, T]), sb("sgd2", [32, T])
            g2a, g2b = sb("g2a", [128, 1024]), sb("g2b", [32, 1024])
            t_sgd, t_g2 = Tok(), Tok()
            for nm, col0, M, dst in (("gd1", 5376, 128, sgd1), ("gd2", 5504, 32, sgd2)):
                i = load_w(col0, M)
                for (p0, n) in TG_LAT:
                    pi = mm_group(i, M, p0, n)
                    S.op("act", lambda e, pi=pi, p0=p0, n=n, M=M, nm=nm: e.activation(
                        out=zraw[0:M, p0:p0 + n], in_=pp[pi][0:M, 0:n], func=AF.Identity,
                        bias=pvc(g, "b_" + nm)[0:M, :], scale=1.0), reads=[t_pp[pi], g.t_pv], writes=[t_zraw])
                shift(zraw, t_zraw, zs[0], t_zs[0], 0, M, pvc(g, "cp_" + nm), pvc(g, "cn_" + nm), [(TC, TT)])
                S.op("act", lambda e, M=M, dst=dst: e.activation(out=dst[0:M, :], in_=zs[0][0:M, TC:TT],
                                                                 func=AF.Sigmoid), reads=[t_zs[0]], writes=[t_sgd])
            S.dma("sp", "ld_small", g2a[:], g.g2[0:128, :], writes=[t_g2])
            S.dma("sp", "ld_small", g2b[:], g.g2[128:160, :], writes=[t_g2])
            for c in range(32):
                zi = c % 2
                for hf in range(2):
                    pi = st["pi"] % 4
                    st["pi"] += 1
                    S.op("pe", lambda e, c=c, hf=hf, pi=pi: e.matmul(
                        pp[pi][0:64, :], lhsT=sgd1[:, c * 64:(c + 1) * 64], rhs=g2a[:, hf * 512:(hf + 1) * 512],
                        start=True, stop=False), reads=[t_sgd, t_g2], writes=[t_pp[pi]])
                    S.op("pe", lambda e, c=c, hf=hf, pi=pi: e.matmul(
                        pp[pi][0:64, :], lhsT=sgd2[:, c * 64:(c + 1) * 64], rhs=g2b[:, hf * 512:(hf + 1) * 512],
                        start=False, stop=True), reads=[t_sgd, t_g2], writes=[t_pp[pi]])
                    S.op("dve", lambda e, hf=hf, pi=pi, zi=zi: e.tensor_copy(
                        out=zs[zi][0:64, hf * 512:(hf + 1) * 512], in_=pp[pi][0:64, :]),
                         reads=[t_pp[pi]], writes=[t_zs[zi]])
                S.dma("act", "st_a%d" % zi, g.Gs[c * 64:(c + 1) * 64, :], zs[zi][0:64, 0:1024],
                      reads=[t_zs[zi]], writes=[g.t_Gs])
            for c in range(8):
                ia = load_w(c * 128, 128)
                ig = load_w(1024 + c * 128, 128)
                for ti, (p0, n) in enumerate(TG_LAT):
                    pa = mm_group(ia, 128, p0, n)
                    pg = mm_group(ig, 128, p0, n)
                    S.op("act", lambda e, pg=pg, ti=ti: e.activation(
                        out=zs[0][:, ti * 512:(ti + 1) * 512], in_=pp[pg][:, :], func=AF.Sigmoid,
                        bias=pvc(g, "cbg_%d" % c), scale=1.0), reads=[t_pp[pg], g.t_pv], writes=[t_zs[0]])
                    S.op("dve", lambda e, pa=pa, ti=ti: e.scalar_tensor_tensor(
                        out=zraw[:, ti * 512:(ti + 1) * 512], in0=pp[pa][:, :], scalar=pvc(g, "cba_%d" % c),
                        in1=zs[0][:, ti * 512:(ti + 1) * 512], op0=ALU.add, op1=ALU.mult),
                         reads=[t_pp[pa], t_zs[0], g.t_pv], writes=[t_zraw])
                hv = zraw[:, 0:T].rearrange("p (r w) -> p r w", w=64)
                ov = zs[1][:, 0:T].rearrange("p (r w) -> p r w", w=64)
                S.op("dve", lambda e: e.tensor_scalar(out=zs[1][:, 0:T], in0=zraw[:, 0:T],
                                                      scalar1=pvc(g, "cw_%d_15" % c), scalar2=pvc(g, "convb_%d" % c),
                                                      op0=ALU.mult, op1=ALU.add),
                     reads=[t_zraw, g.t_pv], writes=[t_zs[1]])
                for j in range(31):
                    o = j - 15
                    if o == 0:
                        continue
                    lo, hi = max(0, -o), min(64, 64 - o)
                    S.op("dve", lambda e, j=j, o=o, lo=lo, hi=hi: e.scalar_tensor_tensor(
                        out=ov[:, :, lo:hi], in0=hv[:, :, lo + o:hi + o], scalar=pvc(g, "cw_%d_%d" % (c, j)),
                        in1=ov[:, :, lo:hi], op0=ALU.mult, op1=ALU.add), reads=[t_zraw, g.t_pv], writes=[t_zs[1]])
                S.dma("act", "st_a1", g.ZC[c * 128:(c + 1) * 128, :], zs[1][:, 0:T], reads=[t_zs[1]],
                      writes=[g.t_ZC])
            for c in range(32):
                i = load_w(5536 + c * 128, 128)
                zi = c % 2
                for ti, (p0, n) in enumerate(TG_LAT):
                    pi = mm_group(i, 128, p0, n)
                    S.op("act", lambda e, pi=pi, ti=ti, zi=zi: e.activation(
                        out=zs[zi][:, ti * 512:(ti + 1) * 512], in_=pp[pi][:, :], func=AF.Sigmoid,
                        bias=pvc(g, "bzg_%d" % c), scale=1.0), reads=[t_pp[pi], g.t_pv], writes=[t_zs[zi]])
                S.dma("act", "st_a%d" % zi, g.SGZ[c * 128:(c + 1) * 128, :], zs[zi][:, 0:T],
                      reads=[t_zs[zi]], writes=[g.t_SGZ])
    S.barrier()


KAPPA = float(np.exp(-0.5))
GN_EPS = 64e-5
NCH = TT // 64


def phase_rwkv(g):
    nc, S = g.nc, g.S
    g.ORW = nc.dram_tensor("s_orw", [1024, T], F32, kind="Internal").ap()
    g.t_ORW = Tok()
    with ExitStack() as es:
        sb = lambda name, shape, dt=F32: es.enter_context(nc.sbuf_tensor("rw_" + name, shape, dt))
        psb = lambda name, shape, dt=F32: es.enter_context(nc.psum_tensor("rwp_" + name, shape, dt))
        R, Kt, Vt, SG, AG, KK, LI, TM = (sb(n, [128, TT]) for n in ("R", "Kt", "Vt", "SG", "AG", "KK", "LI", "TM"))
        QR, BK = sb("QR", [128, 2, TT]), sb("BK", [128, 2, TT])
        BKh = sb("BKh", [64, NCH, 2, 128])
        Vtm = sb("Vtm", [64, NCH, 128])
        Ys = sb("Ys", [64, 2, 32, 66])
        DC = sb("DC", [128, NCH])
        A2 = [sb("A%d" % i, [64, 2, 320]) for i in range(3)]
        W4 = [[sb("W%d0" % i, [64, 2, 3, 64]), sb("W%d1" % i, [64, 2, 3, 64])] for i in range(2)]
        Pinv = [sb("Pi%d" % i, [64, 2, 64]) for i in range(3)]
        tA = [Tok("A%d" % i) for i in range(3)]
        tW = [[Tok(), Tok()] for _ in range(2)]
        tP = [Tok() for _ in range(3)]
        A_sb = A2[2]
        P1s, UTs = sb("P1s", [64, 2, 64]), sb("UTs", [64, 2, 64])
        Sst = sb("Sst", [128, 64])
        maskc = sb("maskc", [128, TT], BF16)
        maskA = sb("maskA", [64, 320])
        onesbd = sb("onesbd", [128, 128])
        onesel = sb("onesel", [128, 2])
        omka = sb("omka", [128, 1])
        LG, LB = sb("LG", [64, 64]), sb("LB", [64, 64])
        st1, st2 = sb("st1", [64, 32]), sb("st2", [64, 32])
        epsg = sb("epsg", [64, 1])
        banks = [psb("bk%d" % i, [128, 512]) for i in range(8)]
        PA = [banks[0][0:64, 0:320], banks[1][0:64, 0:320]]
        PS = banks[2][0:64, 0:384].rearrange("p (d w) -> p d w", w=192)
        PQ = banks[3][0:64, 0:256].rearrange("p (a w) -> p a w", w=64)
        PSn = banks[4][:, 0:128].rearrange("p (d w) -> p d w", w=64)
        PT = banks[5]
        PT2 = banks[6]
        PU = banks[7][0:64, 0:128].rearrange("p (a w) -> p a w", w=64)
        t = {n: Tok(n) for n in ("R", "Kt", "Vt", "SG", "AG", "KK", "LI", "TM", "QR", "BK", "BKh", "Vtm", "Ys", "DC",
                                 "A", "W0", "W1", "P1s", "UTs", "Sst", "c", "LG", "st", "G")}
        for i in range(8):
            t["B%d" % i] = Tok("B%d" % i, excl=True)
        t["PA0"], t["PA1"], t["PS"], t["PQ1"], t["PQ3"], t["PSn"], t["PT"], t["PT2"], t["PQ2"] = (
            t["B0"], t["B1"], t["B2"], t["B3"], t["B3"], t["B4"], t["B5"], t["B6"], t["B7"])
        S.op("pool", lambda e: e.memset(maskc[:], 1.0), writes=[t["c"]])
        S.op("pool", lambda e: e.memset(maskc[:].rearrange("p (c w) -> p c w", w=64)[:, :, 0:1], 0.0),
             reads=[t["c"]], writes=[t["c"]])
        S.op("pool", lambda e: e.memset(onesbd[:], 0.0), writes=[t["c"]])
        S.op("pool", lambda e: e.memset(onesbd[0:64, 0:64], 1.0), reads=[t["c"]], writes=[t["c"]])
        S.op("pool", lambda e: e.memset(onesbd[64:128, 64:128], 1.0), reads=[t["c"]], writes=[t["c"]])
        S.op("pool", lambda e: e.memset(onesel[:], 0.0), writes=[t["c"]])
        S.op("pool", lambda e: e.memset(onesel[0:64, 0:1], 1.0), reads=[t["c"]], writes=[t["c"]])
        S.op("pool", lambda e: e.memset(onesel[64:128, 1:2], 1.0), reads=[t["c"]], writes=[t["c"]])
        S.op("pool", lambda e: e.memset(epsg[:], GN_EPS), writes=[t["c"]])
        S.op("pool", lambda e: e.memset(maskA[:], 1.0), writes=[t["c"]])
        for blk, (cm, base, pat) in enumerate(((-1, -1, 1), (-1, 0, 1), (-1, -1, 1), (-1, 0, 1), (1, -1, -1))):
            S.op("pool", lambda e, blk=blk, cm=cm, base=base, pat=pat: e.affine_select(
                out=maskA[:, blk * 64:(blk + 1) * 64], in_=maskA[:, blk * 64:(blk + 1) * 64], pattern=[[pat, 64]],
                compare_op=ALU.is_ge, fill=0.0, base=base, channel_multiplier=cm), reads=[t["c"]], writes=[t["c"]])
        I64 = g.ident[0:64, 0:64]
        J64 = g.anti[0:64, 64:128]
        g.rw_dbg = {}
        def emit_loads(h):
            for buf, nm, src, tk in ((R, "R", g.RKV[0, h], g.t_RKV), (Kt, "Kt", g.RKV[1, h], g.t_RKV),
                                     (Vt, "Vt", g.RKV[2, h], g.t_RKV), (SG, "SG", g.SGs[h], g.t_SGs),
                                     (AG, "AG", g.AGs[h], g.t_AGs)):
                S.dma("sp", "ld_rw_" + nm, buf[:], src, reads=[tk], writes=[t[nm]])

        for h in range(getattr(g, "nheads", 16)):
            if h == 0:
                emit_loads(0)
            S.dma("pool", "ld_rw_lg", LG[:], g.lnx_g[h:h + 1, :].partition_broadcast(64), writes=[t["LG"]])
            S.dma("pool", "ld_rw_lg", LB[:], g.lnx_b[h:h + 1, :].partition_broadcast(64), writes=[t["LG"]])
            kkc, kac, rkc = pvc(g, "kk_%d" % h), pvc(g, "ka_%d" % h), pvc(g, "rk_%d" % h)
            S.op("dve", lambda e: e.tensor_scalar(out=omka[:], in0=kac, scalar1=-1.0, scalar2=1.0, op0=ALU.mult,
                                                  op1=ALU.add), reads=[g.t_pv], writes=[t["c"]])
            S.op("dve", lambda e: e.tensor_scalar(out=TM[:], in0=Kt[:], scalar1=kkc, scalar2=None, op0=ALU.mult),
                 reads=[t["Kt"], g.t_pv], writes=[t["TM"]])
            S.op("act", lambda e: e.activation(out=KK[:], in_=TM[:], func=AF.Square), reads=[t["TM"]], writes=[t["KK"]])
            for i, (p0, n) in enumerate(TG_ALL):
                pt, tn = (PT, "PT") if i % 2 == 0 else (PT2, "PT2")
                S.op("pe", lambda e, pt=pt, p0=p0, n=n: e.matmul(pt[:, 0:n], lhsT=onesbd[:], rhs=KK[:, p0:p0 + n],
                                                                start=True, stop=True),
                     reads=[t["KK"], t["c"]], writes=[t[tn]])
                S.op("act", lambda e, pt=pt, p0=p0, n=n: e.activation(out=LI[:, p0:p0 + n], in_=pt[:, 0:n],
                                                                      func=AF.Sqrt), reads=[t[tn]], writes=[t["LI"]])
            S.op("dve", lambda e: e.tensor_scalar(out=LI[:], in0=LI[:], scalar1=1e-12, scalar2=None, op0=ALU.max),
                 reads=[t["LI"]], writes=[t["LI"]])
            S.op("dve", lambda e: e.reciprocal(out=LI[:], in_=LI[:]), reads=[t["LI"]], writes=[t["LI"]])
            S.op("dve", lambda e: e.tensor_tensor(out=KK[:], in0=TM[:], in1=LI[:], op=ALU.mult),
                 reads=[t["TM"], t["LI"]], writes=[t["KK"]])
            if getattr(g, "rw_stop", 9) <= 1:
                continue
            S.op("dve", lambda e: e.tensor_tensor_scan(out=LI[:], data0=maskc[:], data1=SG[:], initial=0.0,
                                                       op0=ALU.mult, op1=ALU.add),
                 reads=[t["SG"], t["c"], t["KK"]], writes=[t["LI"]])
            S.op("dve", lambda e: e.tensor_tensor(out=SG[:], in0=LI[:], in1=SG[:], op=ALU.subtract),
                 reads=[t["LI"]], writes=[t["SG"]])
            LIv = LI[:].rearrange("p (c w) -> p c w", w=64)
            S.op("act", lambda e: e.activation(out=DC[:], in_=LIv[:, :, 63], func=AF.Exp, scale=-KAPPA),
                 reads=[t["LI"]], writes=[t["DC"]])
            S.op("act", lambda e: e.activation(out=TM[:], in_=LI[:], func=AF.Exp, scale=-KAPPA),
                 reads=[t["LI"], t["KK"]], writes=[t["TM"]])
            S.op("dve", lambda e: e.tensor_tensor(out=QR[:, 1, :], in0=R[:], in1=TM[:], op=ALU.mult),
                 reads=[t["R"], t["TM"]], writes=[t["QR"]])
            S.op("act", lambda e: e.activation(out=TM[:], in_=SG[:], func=AF.Exp, scale=-KAPPA),
                 reads=[t["SG"], t["QR"]], writes=[t["TM"]])
            S.op("dve", lambda e: e.scalar_tensor_tensor(out=QR[:, 0, :], in0=KK[:], scalar=-1.0, in1=TM[:],
                                                         op0=ALU.mult, op1=ALU.mult),
                 reads=[t["KK"], t["TM"]], writes=[t["QR"]])
            S.op("act", lambda e: e.activation(out=TM[:], in_=LI[:], func=AF.Exp, scale=KAPPA),
                 reads=[t["LI"], t["QR"]], writes=[t["TM"]])
            S.op("dve", lambda e: e.tensor_scalar(out=SG[:], in0=AG[:], scalar1=kac, scalar2=omka[:, 0:1],
                                                  op0=ALU.mult, op1=ALU.add),
                 reads=[t["AG"], t["c"], g.t_pv, t["TM"]], writes=[t["SG"]])
            S.op("dve", lambda e: e.tensor_tensor(out=Kt[:], in0=Kt[:], in1=SG[:], op=ALU.mult),
                 reads=[t["SG"], t["TM"]], writes=[t["Kt"]])
            S.op("dve", lambda e: e.tensor_tensor(out=AG[:], in0=KK[:], in1=AG[:], op=ALU.mult),
                 reads=[t["KK"]], writes=[t["AG"]])
            S.op("dve", lambda e: e.tensor_tensor(out=BK[:, 0, :], in0=AG[:], in1=TM[:], op=ALU.mult),
                 reads=[t["AG"], t["TM"]], writes=[t["BK"]])
            S.op("dve", lambda e: e.tensor_tensor(out=BK[:, 1, :], in0=Kt[:], in1=TM[:], op=ALU.mult),
                 reads=[t["Kt"], t["TM"]], writes=[t["BK"]])
            S.op("dve", lambda e: e.scalar_tensor_tensor(out=R[:], in0=R[:], scalar=rkc, in1=Kt[:], op0=ALU.mult,
                                                         op1=ALU.mult), reads=[t["Kt"], t["QR"], g.t_pv], writes=[t["R"]])
            TMv = TM[:].rearrange("p (c w) -> p c w", w=64)
            S.op("dve", lambda e: e.tensor_tensor(out=TMv, in0=TMv, in1=DC[:].unsqueeze(2).to_broadcast([128, NCH, 64]),
                                                  op=ALU.mult), reads=[t["DC"], t["BK"]], writes=[t["TM"]])
            S.op("dve", lambda e: e.tensor_tensor(out=SG[:], in0=AG[:], in1=TM[:], op=ALU.mult),
                 reads=[t["AG"], t["TM"], t["Kt"]], writes=[t["SG"]])
            S.op("dve", lambda e: e.tensor_tensor(out=LI[:], in0=Kt[:], in1=TM[:], op=ALU.mult),
                 reads=[t["Kt"], t["TM"]], writes=[t["LI"]])
            if getattr(g, "rw_stop", 9) <= 2:
                continue
            for c in range(NCH):
                csl = slice(c * 64, (c + 1) * 64)
                pt, tn = (PT, "PT") if c % 2 == 0 else (PT2, "PT2")
                ptv = pt[0:64, 0:384].rearrange("p (a b) -> p a b", b=128)
                for a, (src, tk) in enumerate(((SG, "SG"), (LI, "LI"), (Vt, "Vt"))):
                    S.op("pe", lambda e, a=a, src=src, ptv=ptv, csl=csl: e.matmul(ptv[:, a, :], lhsT=src[:, csl], rhs=g.ident[:], start=True, stop=True),
                         reads=[t[tk], g.t_ident], writes=[t[tn]])
                S.op("act", lambda e, c=c, ptv=ptv: e.activation(out=BKh[:, c, :, :], in_=ptv[:, 0:2, :], func=AF.Identity),
                     reads=[t[tn]], writes=[t["BKh"]])
                S.op("dve", lambda e, c=c, ptv=ptv: e.tensor_copy(out=Vtm[:, c, :], in_=ptv[:, 2, :]),
                     reads=[t[tn]], writes=[t["Vtm"]])
                if c >= 4:
                    S.op("pe", lambda e, c=c, pt=pt, csl=csl: e.matmul(pt[0:64, 384:386], lhsT=R[:, csl], rhs=onesel[:],
                                                                       start=True, stop=True),
                         reads=[t["R"], t["c"]], writes=[t[tn]])
                    S.op("dve", lambda e, c=c, pt=pt: e.tensor_copy(out=Ys[:, :, c - 4, 64], in_=pt[0:64, 384:386]),
                         reads=[t[tn]], writes=[t["Ys"]])
            if getattr(g, "rw_stop", 9) <= 3:
                continue
            if h + 1 < getattr(g, "nheads", 16):
                emit_loads(h + 1)
            S.op("pool", lambda e: e.memset(Sst[:], 0.0), reads=[t["Sst"]], writes=[t["Sst"]])
            PS0 = PS

            def emit_A(c, sl):
                csl = slice(c * 64, (c + 1) * 64)
                A_ = A2[sl]
                for d in range(2):
                    ds = slice(d * 64, d * 64 + 64)
                    pn = "PA%d" % d
                    for (o0, o1, lt, li, rhs_fn) in ((0, 128, BK, 0, lambda ds=ds: QR[ds, :, csl]),
                                                     (128, 256, BK, 1, lambda ds=ds: QR[ds, :, csl]),
                                                     (256, 320, QR, 0, lambda ds=ds: BK[ds, 0, csl])):
                        S.pe_rg(d * 64)
                        S.op("pe", lambda e, d=d, ds=ds, o0=o0, o1=o1, lt=lt, li=li, rhs_fn=rhs_fn: e.matmul(
                            PA[d][:, o0:o1], lhsT=lt[ds, li, csl], rhs=rhs_fn(), start=True, stop=True),
                             reads=[t["BK"], t["QR"]], writes=[t[pn]])
                    S.op("dve", lambda e, d=d, A_=A_: e.tensor_tensor(out=A_[:, d, :], in0=PA[d][:, :], in1=maskA[:],
                                                                     op=ALU.mult), reads=[t[pn], t["c"]], writes=[tA[sl]])
                Av = A_[:].rearrange("p d (b w) -> p d b w", w=64)
                w0 = W4[c % 2][0]
                S.op("act", lambda e: e.activation(out=w0[:, :, 0, :], in_=Av[:, :, 0, :], func=AF.Identity),
                     reads=[tA[sl]], writes=[tW[c % 2][0]])
                S.op("act", lambda e: e.activation(out=w0[:, :, 2, :], in_=Av[:, :, 4, :], func=AF.Identity),
                     reads=[tA[sl]], writes=[tW[c % 2][0]])
                S.op("dve", lambda e: e.tensor_copy(out=w0[:, :, 1, :], in_=I64.unsqueeze(1).to_broadcast([64, 2, 64])),
                     reads=[g.t_ident], writes=[tW[c % 2][0]])

            PS_alt = PT[0:64, 0:384].rearrange("p (d w) -> p d w", w=192)

            def emit_stage(s_, n, alt=False):
                wc, wn = W4[n % 2][s_ % 2], W4[n % 2][(s_ + 1) % 2]
                tc_, tn_ = tW[n % 2][s_ % 2], tW[n % 2][(s_ + 1) % 2]
                pout, tpout = (Pinv[n % 3][:, :, :], tP[n % 3]) if s_ == 5 else (wn[:, :, 1, :], tn_)
                PS, tPS = (PS_alt, t["PT"]) if alt else (PS0, t["PS"])
                PSv = PS.rearrange("p d (b w) -> p d b w", w=64)
                for d in range(2):
                    S.op("pe", lambda e, d=d: e.matmul(PS[:, d, 0:128], lhsT=wc[:, d, 2, :], rhs=wc[:, d, 0:2, :],
                                                       start=True, stop=True), reads=[tc_], writes=[tPS])
                    if s_ < 5:
                        S.op("pe", lambda e, d=d: e.matmul(PS[:, d, 128:192], lhsT=wc[:, d, 0, :], rhs=wc[:, d, 2, :],
                                                           start=True, stop=True), reads=[tc_], writes=[tPS])
                if s_ < 5:
                    for blk in (0, 2):
                        S.op("act", lambda e, blk=blk: e.activation(out=wn[:, :, blk, :], in_=PSv[:, :, blk, :],
                                                                    func=AF.Identity), reads=[tPS], writes=[tn_])
                S.op("dve", lambda e: e.tensor_tensor(out=pout, in0=wc[:, :, 1, :], in1=PSv[:, :, 1, :],
                                                      op=ALU.add), reads=[tPS, tc_], writes=[tpout])

            def seq_parts(c, sl):
                csl = slice(c * 64, (c + 1) * 64)
                A_ = A2[sl]
                Wf, tWf = Pinv[c % 3], tP[c % 3]

                def p0():
                    for d in range(2):
                        ds = slice(d * 64, d * 64 + 64)
                        S.pe_rg(d * 64)
                        S.op("pe", lambda e, d=d, ds=ds: e.matmul(PQ[:, d, :], lhsT=QR[ds, 0, csl], rhs=Sst[ds, :],
                                                                  start=True, stop=True),
                             reads=[t["QR"], t["Sst"]], writes=[t["PQ1"]])
                        S.op("pe", lambda e, d=d, ds=ds: e.matmul(PQ[:, 2 + d, :], lhsT=A_[:, d, 128:192], rhs=Vtm[:, c, ds],
                                                                  start=True, stop=True),
                             reads=[tA[sl], t["Vtm"]], writes=[t["PQ1"]])
                    S.op("act", lambda e: e.activation(out=P1s[:], in_=PQ[:, 0:2, :], func=AF.Identity),
                         reads=[t["PQ1"]], writes=[t["P1s"]])
                    S.op("dve", lambda e: e.tensor_tensor(out=P1s[:], in0=P1s[:], in1=PQ[:, 2:4, :], op=ALU.add),
                         reads=[t["PQ1"]], writes=[t["P1s"]])

                def p1():
                    for d in range(2):
                        S.op("pe", lambda e, d=d: e.matmul(PU[:, d, :], lhsT=Wf[:, d, :], rhs=P1s[:, d, :],
                                                           start=True, stop=True), reads=[tWf, t["P1s"]], writes=[t["PQ2"]])
                    S.op("dve", lambda e: e.tensor_copy(out=UTs[:], in_=PU[:, 0:2, :]), reads=[t["PQ2"]], writes=[t["UTs"]])

                def p2():
                    for d in range(2):
                        ds = slice(d * 64, d * 64 + 64)
                        S.op("pe", lambda e, d=d: e.matmul(PSn[:, d, :], lhsT=BKh[:, c, 0, :], rhs=UTs[:, d, :],
                                                           start=True, stop=False), reads=[t["BKh"], t["UTs"]],
                             writes=[t["PSn"]])
                        S.op("pe", lambda e, d=d, ds=ds: e.matmul(PSn[:, d, :], lhsT=BKh[:, c, 1, :], rhs=Vtm[:, c, ds],
                                                                  start=False, stop=True),
                             reads=[t["BKh"], t["Vtm"]], writes=[t["PSn"]])

                def p3():
                    if c < 4:
                        return
                    for d in range(2):
                        ds = slice(d * 64, d * 64 + 64)
                        S.pe_rg(d * 64)
                        S.op("pe", lambda e, d=d, ds=ds: e.matmul(PQ[:, d, :], lhsT=QR[ds, 1, csl], rhs=Sst[ds, :],
                                                                  start=True, stop=True),
                             reads=[t["QR"], t["Sst"], t["P1s"]], writes=[t["PQ3"]])
                        S.op("pe", lambda e, d=d: e.matmul(PQ[:, 2 + d, :], lhsT=A_[:, d, 64:128], rhs=UTs[:, d, :],
                                                           start=True, stop=False), reads=[tA[sl], t["UTs"]],
                             writes=[t["PQ3"]])
                        S.op("pe", lambda e, d=d, ds=ds: e.matmul(PQ[:, 2 + d, :], lhsT=A_[:, d, 192:256],
                                                                  rhs=Vtm[:, c, ds], start=False, stop=True),
                             reads=[tA[sl], t["Vtm"]], writes=[t["PQ3"]])
                    S.op("act", lambda e: e.activation(out=Ys[:, :, c - 4, 0:64], in_=PQ[:, 0:2, :], func=AF.Identity),
                         reads=[t["PQ3"]], writes=[t["Ys"]])
                    S.op("dve", lambda e: e.tensor_tensor(out=Ys[:, :, c - 4, 0:64], in0=Ys[:, :, c - 4, 0:64],
                                                          in1=PQ[:, 2:4, :], op=ALU.add),
                         reads=[t["PQ3"]], writes=[t["Ys"]])

                def p4():
                    for d in range(2):
                        ds = slice(d * 64, d * 64 + 64)
                        S.op("dve", lambda e, d=d, ds=ds: e.scalar_tensor_tensor(
                            out=Sst[ds, :], in0=Sst[ds, :], scalar=DC[ds, c:c + 1], in1=PSn[ds, d, :], op0=ALU.mult,
                            op1=ALU.add), reads=[t["PSn"], t["DC"], t["Sst"]], writes=[t["Sst"]])
                return [p0, p1, p2, p3, p4]

            emit_A(0, 0)
            for s_ in range(6):
                emit_stage(s_, 0)
            emit_A(1, 1)
            for s_ in range(3):
                emit_stage(s_, 1, alt=True)
            for c in range(NCH):
                parts = seq_parts(c, c % 3)
                n1, n2 = c + 1, c + 2
                if n2 < NCH:
                    emit_A(n2, n2 % 3)
                for k in range(3):
                    if n2 < NCH:
                        emit_stage(k, n2, alt=(n2 % 2 == 1))
                    parts[k]()
                    if n1 < NCH:
                        emit_stage(3 + k, n1, alt=(n1 % 2 == 1))
                parts[3]()
                parts[4]()
            if getattr(g, "nheads", 16) == 1:
                g.rw_dbg = {"Ys": Ys[:], "QR": QR[:], "BK": BK[:], "BKh": BKh[:], "Vtm": Vtm[:], "DC": DC[:], "A": A_sb[:],
                            "Winv": Pinv[2][:], "Sst": Sst[:]}
            if getattr(g, "rw_stop", 9) <= 4:
                continue
            Yt = TM[0:64, 0:32 * 66].rearrange("p (c w) -> p c w", w=66)
            Gt = KK[0:64, 0:2048].rearrange("p (c w) -> p c w", w=64)
            Yc = QR[0:64, 0, 0:2048].rearrange("p (c w) -> p c w", w=64)
            Y2 = QR[0:64, 1, 0:2048].rearrange("p (c w) -> p c w", w=64)
            S.dma("sp", "ld_rw_G", Gt, g.Gs[:, h * 64:(h + 1) * 64].rearrange("(c p) v -> p c v", p=64),
                  reads=[g.t_Gs, t["KK"]], writes=[t["KK"]])
            for c4 in range(8):
                pt, tn = (PT, "PT") if c4 % 2 == 0 else (PT2, "PT2")
                ptv = pt[0:64, 0:264].rearrange("p (a b) -> p a b", b=66)
                for a in range(4):
                    c = c4 * 4 + a
                    S.op("pe", lambda e, a=a, c=c, ptv=ptv: e.matmul(ptv[:, a, :], lhsT=I64, rhs=Ys[:, 0, c, :],
                                                                     start=True, stop=False),
                         reads=[t["Ys"], g.t_ident], writes=[t[tn]])
                    S.op("pe", lambda e, a=a, c=c, ptv=ptv: e.matmul(ptv[:, a, :], lhsT=J64, rhs=Ys[:, 1, 31 - c, :],
                                                                     start=False, stop=True),
                         reads=[t["Ys"], g.t_ident], writes=[t[tn]])
                S.op("act", lambda e, c4=c4, ptv=ptv: e.activation(out=Yt[:, c4 * 4:(c4 + 1) * 4, :], in_=ptv,
                                                                   func=AF.Identity), reads=[t[tn], t["LI"], t["SG"]],
                     writes=[t["TM"]])
            S.op("dve", lambda e: e.tensor_reduce(out=st1[:], in_=Yt[:, :, 0:64], axis=AX.X, op=ALU.add),
                 reads=[t["TM"]], writes=[t["st"]])
            S.op("dve", lambda e: e.tensor_scalar(out=st1[:], in0=st1[:], scalar1=1.0 / 64, scalar2=None, op0=ALU.mult),
                 reads=[t["st"]], writes=[t["st"]])
            S.op("dve", lambda e: e.tensor_tensor(out=Yc, in0=Yt[:, :, 0:64],
                                                  in1=st1[:].unsqueeze(2).to_broadcast([64, 32, 64]), op=ALU.subtract),
                 reads=[t["TM"], t["st"], t["BK"], t["Sst"]], writes=[t["QR"]])
            S.op("act", lambda e: e.activation(out=Y2, in_=Yc, func=AF.Square), reads=[t["QR"]], writes=[t["QR"]])
            S.op("dve", lambda e: e.tensor_reduce(out=st2[:], in_=Y2, axis=AX.X, op=ALU.add),
                 reads=[t["QR"]], writes=[t["st"]])
            S.op("act", lambda e: e.activation(out=st2[:], in_=st2[:], func=AF.Sqrt, bias=epsg[:, 0:1], scale=1.0 / 64),
                 reads=[t["st"], t["c"]], writes=[t["st"]])
            S.op("dve", lambda e: e.reciprocal(out=st2[:], in_=st2[:]), reads=[t["st"]], writes=[t["st"]])
            S.op("dve", lambda e: e.tensor_tensor(out=Yc, in0=Yc, in1=st2[:].unsqueeze(2).to_broadcast([64, 32, 64]),
                                                  op=ALU.mult), reads=[t["st"], t["QR"]], writes=[t["QR"]])
            S.op("dve", lambda e: e.tensor_tensor(out=Yc, in0=Yc, in1=LG[:].unsqueeze(1).to_broadcast([64, 32, 64]),
                                                  op=ALU.mult), reads=[t["LG"], t["QR"]], writes=[t["QR"]])
            S.op("dve", lambda e: e.tensor_tensor(out=Yc, in0=Yc, in1=LB[:].unsqueeze(1).to_broadcast([64, 32, 64]),
                                                  op=ALU.add), reads=[t["LG"], t["QR"]], writes=[t["QR"]])
            S.op("dve", lambda e: e.tensor_tensor(out=Y2, in0=Vtm[:, 4:36, 0:64],
                                                  in1=Yt[:, :, 64:65].to_broadcast([64, 32, 64]), op=ALU.mult),
                 reads=[t["Vtm"], t["TM"], t["QR"]], writes=[t["QR"]])
            S.op("dve", lambda e: e.tensor_tensor(out=Yc, in0=Yc, in1=Y2, op=ALU.add), reads=[t["QR"]], writes=[t["QR"]])
            S.op("dve", lambda e: e.tensor_tensor(out=Yc, in0=Yc, in1=Gt, op=ALU.mult), reads=[t["QR"], t["KK"]],
                 writes=[t["QR"]])
            Of = BK[0:64, 0, 0:2048]
            for c8 in range(4):
                pt, tn = (PT, "PT") if c8 % 2 == 0 else (PT2, "PT2")
                for a in range(8):
                    c = c8 * 8 + a
                    S.op("pe", lambda e, a=a, c=c, pt=pt: e.matmul(pt[0:64, a * 64:(a + 1) * 64], lhsT=Yc[:, c, :], rhs=I64, start=True, stop=True),
                         reads=[t["QR"], g.t_ident], writes=[t[tn]])
                S.op("act", lambda e, c8=c8, pt=pt: e.activation(out=Of[:, c8 * 512:(c8 + 1) * 512], in_=pt[0:64, :],
                                                                 func=AF.Identity), reads=[t[tn], t["A"]], writes=[t["BK"]])
            S.dma("sp", "st_rw", g.ORW[h * 64:(h + 1) * 64, :], Of, reads=[t["BK"]], writes=[g.t_ORW, t["BK"]])
    S.barrier()


def bank_toks(n):
    return [Tok("bank%d" % i, excl=True) for i in range(n)]


def phase_mix(g):
    nc, S = g.nc, g.S
    g.es2 = ExitStack()
    g.mixT = g.es2.enter_context(nc.sbuf_tensor("mixT", [128, 16, T], BF16))
    g.t_mixT = Tok()
    with ExitStack() as es:
        sb = lambda name, shape, dt=F32: es.enter_context(nc.sbuf_tensor("mx_" + name, shape, dt))
        HN, ORWb = sb("HN", [128, 8, T], BF16), sb("ORWb", [128, 8, T], BF16)
        ZCt, SQ = sb("ZCt", [128, 8, 512]), sb("SQ", [128, 8, 512])
        mu, rs, tmp = sb("mu", [128, 512]), sb("rs", [128, 512]), sb("tmp", [128, 512])
        onesM = sb("onesM", [128, 128])
        wf = [sb("wf0", [128, 8, 128]), sb("wf1", [128, 8, 128])]
        wb = [sb("wb0", [128, 8, 128], BF16), sb("wb1", [128, 8, 128], BF16)]
        sz = [sb("sz0", [128, 512]), sb("sz1", [128, 512])]
        t1, t2 = sb("t1", [128, 512]), sb("t2", [128, 512])
        banks = [es.enter_context(nc.psum_tensor("mx_bk%d" % i, [128, 512], F32)) for i in range(4)]
        tb = bank_toks(4)
        tk = {n: Tok(n) for n in ("HN", "ORWb", "ZCt", "SQ", "mu", "rs", "tmp", "c", "wf0", "wf1", "wb0", "wb1",
                                  "sz0", "sz1", "t1", "t2")}
        S.op("pool", lambda e: e.memset(onesM[:], 1.0 / 1024), writes=[tk["c"]])
        zcv = g.ZC.rearrange("(c p) t -> p c t", p=128)
        for tg in range(4):
            tsl = slice(tg * 512, (tg + 1) * 512)
            S.dma("sp", "mx_ld", ZCt[:], zcv[:, :, tsl], reads=[g.t_ZC], writes=[tk["ZCt"]])
            S.op("act", lambda e: e.activation(out=SQ[:], in_=ZCt[:], func=AF.Square), reads=[tk["ZCt"]], writes=[tk["SQ"]])
            for cc in range(8):
                S.op("pe", lambda e, cc=cc: e.matmul(banks[0][:, :], lhsT=onesM[:], rhs=ZCt[:, cc, :], start=(cc == 0),
                                                    stop=(cc == 7)), reads=[tk["ZCt"], tk["c"]], writes=[tb[0]])
            for cc in range(8):
                S.op("pe", lambda e, cc=cc: e.matmul(banks[1][:, :], lhsT=onesM[:], rhs=SQ[:, cc, :], start=(cc == 0),
                                                    stop=(cc == 7)), reads=[tk["SQ"], tk["c"]], writes=[tb[1]])
            S.op("act", lambda e: e.activation(out=mu[:], in_=banks[0][:, :], func=AF.Identity), reads=[tb[0]],
                 writes=[tk["mu"]])
            S.op("dve", lambda e: e.tensor_tensor(out=tmp[:], in0=mu[:], in1=mu[:], op=ALU.mult), reads=[tk["mu"]],
                 writes=[tk["tmp"]])
            S.op("dve", lambda e: e.tensor_tensor(out=rs[:], in0=banks[1][:, :], in1=tmp[:], op=ALU.subtract),
                 reads=[tb[1], tk["tmp"]], writes=[tk["rs"]])
            S.op("act", lambda e: e.activation(out=rs[:], in_=rs[:], func=AF.Sqrt, bias=g.eps_ln[:, 0:1], scale=1.0),
                 reads=[tk["rs"], g.t_const], writes=[tk["rs"]])
            S.op("dve", lambda e: e.reciprocal(out=rs[:], in_=rs[:]), reads=[tk["rs"]], writes=[tk["rs"]])
            for cc in range(8):
                S.op("dve", lambda e, cc=cc: e.tensor_tensor(out=tmp[:], in0=ZCt[:, cc, :], in1=mu[:], op=ALU.subtract),
                     reads=[tk["ZCt"], tk["mu"]], writes=[tk["tmp"]])
                S.op("dve", lambda e: e.tensor_tensor(out=tmp[:], in0=tmp[:], in1=rs[:], op=ALU.mult),
                     reads=[tk["rs"]], writes=[tk["tmp"]])
                S.op("act", lambda e, cc=cc, tsl=tsl: e.activation(out=HN[:, cc, tsl], in_=tmp[:], func=AF.Silu,
                                                                   bias=pvc(g, "clnb_%d" % cc), scale=pvc(g, "clng_%d" % cc)),
                     reads=[tk["tmp"], g.t_pv], writes=[tk["HN"]])
        for kc in range(8):
            for tg in range(4):
                i = (kc * 4 + tg) % 2
                tsl = slice(tg * 512, (tg + 1) * 512)
                S.dma("sp", "mx_ld%d" % i, sz[i][:], g.ORW[kc * 128:(kc + 1) * 128, tsl], reads=[g.t_ORW],
                      writes=[tk["sz%d" % i]])
                S.op("pool", lambda e, i=i, kc=kc, tsl=tsl: e.tensor_copy(out=ORWb[:, kc, tsl], in_=sz[i][:]),
                     reads=[tk["sz%d" % i]], writes=[tk["ORWb"]])
        wcv = g.w_conv_o.rearrange("(kc p) n -> p kc n", p=128)
        wrv = g.w_rwkv_o.rearrange("(kc p) n -> p kc n", p=128)
        for f in range(16):
            fsl = slice(f * 128, (f + 1) * 128)
            for i, wv in enumerate((wcv, wrv)):
                S.dma("sp", "mx_w%d" % i, wf[i][:], wv[:, :, fsl], writes=[tk["wf%d" % i]])
                S.op("pool", lambda e, i=i: e.tensor_copy(out=wb[i][:], in_=wf[i][:]), reads=[tk["wf%d" % i]],
                     writes=[tk["wb%d" % i]])
            for tg in range(4):
                tsl = slice(tg * 512, (tg + 1) * 512)
                ba, bb = (0, 1) if tg % 2 == 0 else (2, 3)
                for kc in range(8):
                    S.op("pe", lambda e, kc=kc, ba=ba, tsl=tsl: e.matmul(banks[ba][:, :], lhsT=wb[0][:, kc, :],
                                                                        rhs=HN[:, kc, tsl], start=(kc == 0), stop=(kc == 7)),
                         reads=[tk["wb0"], tk["HN"]], writes=[tb[ba]])
                for kc in range(8):
                    S.op("pe", lambda e, kc=kc, bb=bb, tsl=tsl: e.matmul(banks[bb][:, :], lhsT=wb[1][:, kc, :],
                                                                        rhs=ORWb[:, kc, tsl], start=(kc == 0), stop=(kc == 7)),
                         reads=[tk["wb1"], tk["ORWb"]], writes=[tb[bb]])
                S.dma("sp", "mx_sz0", sz[0][:], g.SGZ[f * 128:(f + 1) * 128, tsl], reads=[g.t_SGZ], writes=[tk["sz0"]])
                S.dma("sp", "mx_sz1", sz[1][:], g.SGZ[2048 + f * 128:2048 + (f + 1) * 128, tsl], reads=[g.t_SGZ],
                      writes=[tk["sz1"]])
                S.op("dve", lambda e, ba=ba: e.scalar_tensor_tensor(out=t1[:], in0=banks[ba][:, :], scalar=pvc(g, "bco_%d" % f),
                                                                    in1=sz[0][:], op0=ALU.add, op1=ALU.mult),
                     reads=[tb[ba], tk["sz0"], g.t_pv], writes=[tk["t1"]])
                S.op("dve", lambda e, bb=bb: e.tensor_tensor(out=t2[:], in0=banks[bb][:, :], in1=sz[1][:], op=ALU.mult),
                     reads=[tb[bb], tk["sz1"]], writes=[tk["t2"]])
                S.op("pool", lambda e, f=f, tsl=tsl: e.tensor_tensor(out=g.mixT[:, f, tsl], in0=t1[:], in1=t2[:], op=ALU.add),
                     reads=[tk["t1"], tk["t2"]], writes=[g.t_mixT])
    S.barrier()


def phase_out1(g):
    nc, S = g.nc, g.S
    dr = lambda name, shape, dt=F32: nc.dram_tensor(name, list(shape), dt, kind="Internal").ap()
    g.XMID = dr("s_xmid", [T, D])
    g.UT = dr("s_ut", [128, 16, T], BF16)
    g.CT = dr("s_ct", [32, T])
    g.t_XMID, g.t_UT, g.t_CT = Tok(), Tok(), Tok()
    with ExitStack() as es:
        sb = lambda name, shape, dt=F32: es.enter_context(nc.sbuf_tensor("o1_" + name, shape, dt))
        OF = sb("OF", [128, 16, 512])
        wf = [sb("wf0", [128, 16, 128]), sb("wf1", [128, 16, 128])]
        wb = [sb("wb0", [128, 16, 128], BF16), sb("wb1", [128, 16, 128], BF16)]
        xt, un = sb("xt", [128, D]), sb("un", [128, D])
        G1, B1 = sb("G1", [128, D]), sb("B1", [128, D])
        st, mv = sb("st", [128, 4, 6]), sb("mv", [128, 4])
        uTb, uTf = sb("uTb", [128, 16, 128], BF16), sb("uTf", [128, 16, 128])
        WR, BR = sb("WR", [128, 16, 32]), sb("BR", [128, 32])
        lg, ex, m8, sm = sb("lg", [128, 32]), sb("ex", [128, 32]), sb("m8", [128, 8]), sb("sm", [128, 4])
        cTt = sb("cTt", [32, 128])
        banks = [es.enter_context(nc.psum_tensor("o1_bk%d" % i, [128, 512], F32)) for i in range(7)]
        tb = bank_toks(7)
        tk = {n: Tok(n) for n in ("OF", "wf0", "wf1", "wb0", "wb1", "xt", "un", "GB", "st", "uTb", "uTf", "WR", "lg",
                                  "cTt")}
        S.dma("pool", "o1_c", G1[:], g.ln1_g[0:1, :].partition_broadcast(128), writes=[tk["GB"]])
        S.dma("pool", "o1_c", B1[:], g.ln1_b[0:1, :].partition_broadcast(128), writes=[tk["GB"]])
        S.dma("pool", "o1_c", BR[:], g.b_router[0:1, :].partition_broadcast(128), writes=[tk["WR"]])
        S.dma("sp", "o1_c2", WR[:], g.w_router, writes=[tk["WR"]])
        wov = g.w_out.rearrange("(kc p) n -> p kc n", p=128)
        for tg in range(4):
            tsl = slice(tg * 512, (tg + 1) * 512)
            for f in range(16):
                i = f % 2
                S.dma("sp", "o1_w%d" % i, wf[i][:], wov[:, :, f * 128:(f + 1) * 128], writes=[tk["wf%d" % i]])
                S.op("pool", lambda e, i=i: e.tensor_copy(out=wb[i][:], in_=wf[i][:]), reads=[tk["wf%d" % i]],
                     writes=[tk["wb%d" % i]])
                bk = 4 + i
                for kc in range(16):
                    S.op("pe", lambda e, kc=kc, i=i, bk=bk: e.matmul(banks[bk][:, :], lhsT=wb[i][:, kc, :],
                                                                    rhs=g.mixT[:, kc, tsl], start=(kc == 0), stop=(kc == 15)),
                         reads=[tk["wb%d" % i], g.t_mixT], writes=[tb[bk]])
                S.op("dve", lambda e, f=f, bk=bk: e.tensor_scalar(out=OF[:, f, :], in0=banks[bk][:, :],
                                                                  scalar1=pvc(g, "bout_%d" % f), scalar2=g.mod[:, 32 + f, 0:1],
                                                                  op0=ALU.add, op1=ALU.mult),
                     reads=[tb[bk], g.t_pv, g.t_mod], writes=[tk["OF"]])
            for j in range(4):
                tok0 = tg * 512 + j * 128
                S.dma("sp", "o1_x", xt[:], g.x[tok0:tok0 + 128, :], writes=[tk["xt"]])
                for f in range(16):
                    S.op("pe", lambda e, f=f, j=j: e.transpose(
                        banks[f // 4][:, (f % 4) * 128:(f % 4 + 1) * 128], OF[:, f, j * 128:(j + 1) * 128], g.ident[:]),
                         reads=[tk["OF"], g.t_ident], writes=[tb[f // 4]])
                for q in range(4):
                    S.op("dve", lambda e, q=q: e.scalar_tensor_tensor(
                        out=xt[:, q * 512:(q + 1) * 512], in0=xt[:, q * 512:(q + 1) * 512], scalar=ALPHA,
                        in1=banks[q][:, :], op0=ALU.mult, op1=ALU.add), reads=[tb[q]], writes=[tk["xt"]])
                ln_rows(g, xt, tk["xt"], st, mv, tk["st"])
                S.op("dve", lambda e: e.tensor_tensor(out=xt[:], in0=xt[:], in1=G1[:], op=ALU.mult),
                     reads=[tk["GB"]], writes=[tk["xt"]])
                S.op("pool", lambda e: e.tensor_tensor(out=xt[:], in0=xt[:], in1=B1[:], op=ALU.add),
                     reads=[tk["GB"]], writes=[tk["xt"]])
                S.dma("pool", "o1_xm", g.XMID[tok0:tok0 + 128, :], xt[:], reads=[tk["xt"]], writes=[g.t_XMID])
                S.op("pool", lambda e: e.tensor_copy(out=un[:], in_=xt[:]), reads=[tk["xt"]], writes=[tk["un"]])
                ln_rows(g, un, tk["un"], st, mv, tk["st"])
                for f in range(16):
                    S.op("pe", lambda e, f=f: e.transpose(banks[f // 4][:, (f % 4) * 128:(f % 4 + 1) * 128],
                                                          un[:, f * 128:(f + 1) * 128], g.ident[:]),
                         reads=[tk["un"], g.t_ident], writes=[tb[f // 4]])
                for f in range(16):
                    src = banks[f // 4][:, (f % 4) * 128:(f % 4 + 1) * 128]
                    S.op("act", lambda e, f=f, src=src: e.activation(out=uTb[:, f, :], in_=src, func=AF.Identity,
                                                                     bias=g.mod[:, 48 + f, 0:1], scale=g.modp[:, 64 + f, 0:1]),
                         reads=[tb[f // 4], g.t_mod], writes=[tk["uTb"]])
                    S.op("act", lambda e, f=f, src=src: e.activation(out=uTf[:, f, :], in_=src, func=AF.Identity,
                                                                     bias=g.mod[:, 48 + f, 0:1], scale=g.modp[:, 64 + f, 0:1]),
                         reads=[tb[f // 4], g.t_mod], writes=[tk["uTf"]])
                S.dma("act", "o1_ut", g.UT[:, :, tok0:tok0 + 128], uTb[:], reads=[tk["uTb"]], writes=[g.t_UT])
                for kc in range(16):
                    S.op("pe", lambda e, kc=kc: e.matmul(banks[6][:, 0:32], lhsT=uTf[:, kc, :], rhs=WR[:, kc, :],
                                                        start=(kc == 0), stop=(kc == 15)),
                         reads=[tk["uTf"], tk["WR"]], writes=[tb[6]])
                S.op("dve", lambda e: e.tensor_tensor(out=lg[:], in0=banks[6][:, 0:32], in1=BR[:], op=ALU.add),
                     reads=[tb[6], tk["WR"]], writes=[tk["lg"]])
                S.op("dve", lambda e: e.max(out=m8[:], in_=lg[:]), reads=[tk["lg"]], writes=[tk["lg"]])
                S.op("dve", lambda e: e.tensor_scalar(out=sm[:, 0:1], in0=m8[:, 0:1], scalar1=-1.0, scalar2=None,
                                                      op0=ALU.mult), reads=[tk["lg"]], writes=[tk["lg"]])
                S.op("act", lambda e: e.activation(out=ex[:], in_=lg[:], func=AF.Exp, bias=sm[:, 0:1], scale=1.0),
                     reads=[tk["lg"]], writes=[tk["lg"]])
                S.op("dve", lambda e: e.tensor_scalar(out=lg[:], in0=lg[:], scalar1=m8[:, 3:4], scalar2=None,
                                                      op0=ALU.is_ge), reads=[tk["lg"]], writes=[tk["lg"]])
                S.op("dve", lambda e: e.tensor_tensor(out=ex[:], in0=ex[:], in1=lg[:], op=ALU.mult),
                     reads=[tk["lg"]], writes=[tk["lg"]])
                S.op("dve", lambda e: e.tensor_reduce(out=sm[:, 1:2], in_=ex[:], axis=AX.X, op=ALU.add),
                     reads=[tk["lg"]], writes=[tk["lg"]])
                S.op("dve", lambda e: e.reciprocal(out=sm[:, 2:3], in_=sm[:, 1:2]), reads=[tk["lg"]], writes=[tk["lg"]])
                S.op("dve", lambda e: e.tensor_scalar(out=ex[:], in0=ex[:], scalar1=sm[:, 2:3], scalar2=None,
                                                      op0=ALU.mult), reads=[tk["lg"]], writes=[tk["lg"]])
                S.op("pe", lambda e: e.matmul(banks[6][0:32, 128:256], lhsT=ex[:], rhs=g.ident[:], start=True, stop=True),
                     reads=[tk["lg"], g.t_ident], writes=[tb[6]])
                S.op("act", lambda e: e.activation(out=cTt[:], in_=banks[6][0:32, 128:256], func=AF.Identity),
                     reads=[tb[6]], writes=[tk["cTt"]])
                S.dma("act", "o1_ct", g.CT[:, tok0:tok0 + 128], cTt[:], reads=[tk["cTt"]], writes=[g.t_CT])
    g.es2.close()
    S.barrier()


def phase_moe(g, out):
    nc, S = g.nc, g.S
    HB = 1024
    for hb in range(2):
        with ExitStack() as es0:
            acc = es0.enter_context(nc.sbuf_tensor("me_acc%d" % hb, [128, 16, HB], F32))
            t_acc = Tok()
            with ExitStack() as es:
                sb = lambda name, shape, dt=F32: es.enter_context(nc.sbuf_tensor("me%d_" % hb + name, shape, dt))
                uTh, hidT = sb("uTh", [128, 16, HB], BF16), sb("hidT", [128, 16, HB], BF16)
                cT, rhe, cb = sb("cT", [32, HB]), sb("rhe", [32, HB]), sb("cb", [128, HB])
                ones32 = sb("ones32", [32, 128])
                wf = [sb("wf0", [128, 16, 128]), sb("wf1", [128, 16, 128])]
                wb = [sb("wb%d" % i, [128, 16, 128], BF16) for i in range(4)]
                t1, t2, t3 = sb("t1", [128, 512]), sb("t2", [128, 512]), sb("t3", [128, 512])
                BGU = sb("BGU", [128, 1024])
                bd = sb("bd", [32, 128])
                banks = [es.enter_context(nc.psum_tensor("me%d_bk%d" % (hb, i), [128, 512], F32)) for i in range(8)]
                tb = bank_toks(8)
                tk = {n: Tok(n) for n in ("uTh", "hidT", "cT", "rhe", "cb", "c", "wf0", "wf1", "wb0", "wb1", "wb2", "wb3",
                                          "t1", "t2", "t3", "BGU", "bd")}
                hsl = slice(hb * HB, (hb + 1) * HB)
                S.dma("sp", "me_ld", uTh[:], g.UT[:, :, hsl], reads=[g.t_UT], writes=[tk["uTh"]])
                S.dma("sp", "me_ld", cT[:], g.CT[:, hsl], reads=[g.t_CT], writes=[tk["cT"]])
                S.dma("sp", "me_ld", BGU[:], g.b_gu, writes=[tk["BGU"]])
                S.op("pool", lambda e: e.memset(ones32[:], 1.0), writes=[tk["c"]])
                st = {"w": 0, "pb": 0}
                for ex_ in range(32):
                    S.op("dve", lambda e, ex_=ex_: e.tensor_scalar(out=rhe[:], in0=cT[:], scalar1=g.ident[0:32, ex_:ex_ + 1],
                                                                   scalar2=None, op0=ALU.mult),
                         reads=[tk["cT"], g.t_ident], writes=[tk["rhe"]])
                    for tg in range(2):
                        S.op("pe", lambda e, tg=tg: e.matmul(banks[6 + tg][:, :], lhsT=ones32[:],
                                                            rhs=rhe[:, tg * 512:(tg + 1) * 512], start=True, stop=True),
                             reads=[tk["rhe"], tk["c"]], writes=[tb[6 + tg]])
                        S.op("act", lambda e, tg=tg: e.activation(out=cb[:, tg * 512:(tg + 1) * 512], in_=banks[6 + tg][:, :],
                                                                  func=AF.Identity), reads=[tb[6 + tg]], writes=[tk["cb"]])
                    wgv = g.w_gu[ex_].rearrange("(kc p) n -> p kc n", p=128)
                    wdv = g.w_dn[ex_].rearrange("(kc p) n -> p kc n", p=128)
                    for j in range(16):
                        pair = (st["w"] % 2) * 2
                        st["w"] += 1
                        for i, c0 in enumerate((j * 128, 2048 + j * 128)):
                            S.dma("sp", "me_w%d" % i, wf[i][:], wgv[:, :, c0:c0 + 128], writes=[tk["wf%d" % i]])
                            S.op("act", lambda e, i=i, pair=pair: e.activation(out=wb[pair + i][:], in_=wf[i][:], func=AF.Identity),
                                 reads=[tk["wf%d" % i]], writes=[tk["wb%d" % (pair + i)]])
                        for tg in range(2):
                            tsl = slice(tg * 512, (tg + 1) * 512)
                            pb = (st["pb"] % 2) * 2
                            st["pb"] += 1
                            for i in range(2):
                                for kc in range(16):
                                    S.op("pe", lambda e, kc=kc, i=i, pb=pb, pair=pair, tsl=tsl: e.matmul(
                                        banks[pb + i][:, :], lhsT=wb[pair + i][:, kc, :], rhs=uTh[:, kc, tsl],
                                        start=(kc == 0), stop=(kc == 15)),
                                         reads=[tk["wb%d" % (pair + i)], tk["uTh"]], writes=[tb[pb + i]])
                            bgc = BGU[:, ex_ * 32 + j:ex_ * 32 + j + 1]
                            buc = BGU[:, ex_ * 32 + 16 + j:ex_ * 32 + 16 + j + 1]
                            S.op("dve", lambda e, pb=pb, bgc=bgc: e.tensor_scalar(out=t1[:], in0=banks[pb][:, :], scalar1=bgc,
                                                                                  scalar2=7.0, op0=ALU.add, op1=ALU.min),
                                 reads=[tb[pb], tk["BGU"]], writes=[tk["t1"]])
                            S.op("act", lambda e: e.activation(out=t2[:], in_=t1[:], func=AF.Sigmoid, scale=1.702),
                                 reads=[tk["t1"]], writes=[tk["t2"]])
                            S.op("dve", lambda e, pb=pb, buc=buc: e.tensor_scalar(out=t3[:], in0=banks[pb + 1][:, :],
                                                                                  scalar1=buc, scalar2=-7.0, op0=ALU.add,
                                                                                  op1=ALU.max),
                                 reads=[tb[pb + 1], tk["BGU"]], writes=[tk["t3"]])
                            S.op("dve", lambda e: e.tensor_scalar(out=t3[:], in0=t3[:], scalar1=7.0, scalar2=1.0, op0=ALU.min,
                                                                  op1=ALU.add), reads=[tk["t3"]], writes=[tk["t3"]])
                            S.op("dve", lambda e: e.tensor_tensor(out=t1[:], in0=t1[:], in1=t2[:], op=ALU.mult),
                                 reads=[tk["t2"]], writes=[tk["t1"]])
                            S.op("pool", lambda e: e.tensor_tensor(out=t1[:], in0=t1[:], in1=t3[:], op=ALU.mult),
                                 reads=[tk["t3"]], writes=[tk["t1"]])
                            S.op("dve", lambda e, j=j, tsl=tsl: e.tensor_tensor(out=hidT[:, j, tsl], in0=t1[:], in1=cb[:, tsl],
                                                                                op=ALU.mult),
                                 reads=[tk["t1"], tk["cb"]], writes=[tk["hidT"]])
                    for f in range(16):
                        wi = st["w"] % 4
                        st["w"] += 1
                        i = f % 2
                        S.dma("sp", "me_w%d" % i, wf[i][:], wdv[:, :, f * 128:(f + 1) * 128], writes=[tk["wf%d" % i]])
                        S.op("act", lambda e, i=i, wi=wi: e.activation(out=wb[wi][:], in_=wf[i][:], func=AF.Identity),
                             reads=[tk["wf%d" % i]], writes=[tk["wb%d" % wi]])
                        if ex_ == 0:
                            S.dma("sp", "me_bd", bd[:], g.b_dn[:, f * 128:(f + 1) * 128], writes=[tk["bd"]])
                        for tg in range(2):
                            tsl = slice(tg * 512, (tg + 1) * 512)
                            bk = 4 + (f * 2 + tg) % 2
                            for kc in range(16):
                                S.op("pe", lambda e, kc=kc, bk=bk, wi=wi, tsl=tsl: e.matmul(
                                    banks[bk][:, :], lhsT=wb[wi][:, kc, :], rhs=hidT[:, kc, tsl], start=(kc == 0),
                                    stop=(kc == 15)), reads=[tk["wb%d" % wi], tk["hidT"]], writes=[tb[bk]])
                            if ex_ == 0:
                                S.op("pe", lambda e, tg=tg, tsl=tsl: e.matmul(banks[6 + tg][:, :], lhsT=bd[:], rhs=cT[:, tsl],
                                                                              start=True, stop=True),
                                     reads=[tk["bd"], tk["cT"]], writes=[tb[6 + tg]])
                                S.op("act", lambda e, f=f, tg=tg, tsl=tsl: e.activation(out=acc[:, f, tsl], in_=banks[6 + tg][:, :],
                                                                                      func=AF.Identity),
                                     reads=[tb[6 + tg]], writes=[t_acc])
                            S.op("dve", lambda e, f=f, bk=bk, tsl=tsl: e.tensor_tensor(out=acc[:, f, tsl], in0=banks[bk][:, :],
                                                                                      in1=acc[:, f, tsl], op=ALU.add),
                                 reads=[tb[bk]], writes=[t_acc])
            S.barrier()
            with ExitStack() as es:
                sb = lambda name, shape, dt=F32: es.enter_context(nc.sbuf_tensor("fin%d_" % hb + name, shape, dt))
                xt = [sb("xt0", [128, D]), sb("xt1", [128, D])]
                G2, B2 = sb("G2", [128, D]), sb("B2", [128, D])
                st_, mv = sb("st", [128, 4, 6]), sb("mv", [128, 4])
                banks = [es.enter_context(nc.psum_tensor("fin%d_bk%d" % (hb, i), [128, 512], F32)) for i in range(4)]
                tb = bank_toks(4)
                tk = {n: Tok(n) for n in ("xt0", "xt1", "GB", "st")}
                t_out = Tok()
                S.dma("pool", "fin_c", G2[:], g.ln2_g[0:1, :].partition_broadcast(128), writes=[tk["GB"]])
                S.dma("pool", "fin_c", B2[:], g.ln2_b[0:1, :].partition_broadcast(128), writes=[tk["GB"]])
                for f in range(16):
                    S.op("dve", lambda e, f=f: e.tensor_scalar(out=acc[:, f, :], in0=acc[:, f, :], scalar1=g.mod[:, 80 + f, 0:1],
                                                               scalar2=None, op0=ALU.mult), reads=[g.t_mod], writes=[t_acc])
                for j in range(HB // 128):
                    b = j % 2
                    tok0 = hb * HB + j * 128
                    S.dma("sp", "fin_x%d" % b, xt[b][:], g.XMID[tok0:tok0 + 128, :], reads=[g.t_XMID], writes=[tk["xt%d" % b]])
                    for f in range(16):
                        S.op("pe", lambda e, f=f, j=j: e.transpose(
                            banks[f // 4][:, (f % 4) * 128:(f % 4 + 1) * 128], acc[:, f, j * 128:(j + 1) * 128], g.ident[:]),
                             reads=[t_acc, g.t_ident], writes=[tb[f // 4]])
                    for q in range(4):
                        S.op("dve", lambda e, q=q, b=b: e.scalar_tensor_tensor(
                            out=xt[b][:, q * 512:(q + 1) * 512], in0=xt[b][:, q * 512:(q + 1) * 512], scalar=ALPHA,
                            in1=banks[q][:, :], op0=ALU.mult, op1=ALU.add), reads=[tb[q]], writes=[tk["xt%d" % b]])
                    ln_rows(g, xt[b], tk["xt%d" % b], st_, mv, tk["st"])
                    S.op("dve", lambda e, b=b: e.tensor_tensor(out=xt[b][:], in0=xt[b][:], in1=G2[:], op=ALU.mult),
                         reads=[tk["GB"]], writes=[tk["xt%d" % b]])
                    S.op("pool", lambda e, b=b: e.tensor_tensor(out=xt[b][:], in0=xt[b][:], in1=B2[:], op=ALU.add),
                         reads=[tk["GB"]], writes=[tk["xt%d" % b]])
                    S.dma("sp", "fin_o%d" % b, out[tok0:tok0 + 128, :], xt[b][:], reads=[tk["xt%d" % b]],
                          writes=[t_out, tk["xt%d" % b]])
                g.out_toks.append(t_out)
            S.barrier()


def make_in_maps(inp, ncores=8):
    maps = []
    for b in range(ncores):
        m = {}
        m["x"] = np.ascontiguousarray(inp["x"][b])
        m["ctx"] = np.ascontiguousarray(inp["ctx"][b])
        cc = np.stack([inp["c"][b], inp["c_ctx"]], axis=-1)
        m["cc"] = np.ascontiguousarray(cc.reshape(16, 128, 2).transpose(1, 0, 2))
        m["w_ada"] = np.ascontiguousarray(inp["w_ada"][0])
        m["b_ada"] = np.ascontiguousarray(inp["b_ada"][0].reshape(96, 128).T)
        m["w_in"] = np.ascontiguousarray(inp["w_in"][0])
        m["pv"] = make_pv(inp)
        m["g2"] = np.ascontiguousarray(inp["g2"][0])
        m["lnx_g"] = np.ascontiguousarray(inp["lnx_g"][0].reshape(16, 64))
        m["lnx_b"] = np.ascontiguousarray(inp["lnx_b"][0].reshape(16, 64))
        m["w_conv_o"] = np.ascontiguousarray(inp["w_conv_o"][0])
        m["w_rwkv_o"] = np.ascontiguousarray(inp["w_rwkv_o"][0])
        m["w_out"] = np.ascontiguousarray(inp["w_out"][0])
        for nm in ("ln1_g", "ln1_b", "ln2_g", "ln2_b", "b_router"):
            m[nm] = np.ascontiguousarray(inp[nm][0][None, :])
        m["w_router"] = np.ascontiguousarray(inp["w_router"][0].reshape(16, 128, 32).transpose(1, 0, 2))
        m["w_gu"] = np.ascontiguousarray(inp["w_gate_up"][0])
        m["b_gu"] = np.ascontiguousarray(inp["b_gate_up"][0].reshape(32, 32, 128).transpose(2, 0, 1).reshape(128, 1024))
        m["w_dn"] = np.ascontiguousarray(inp["w_down"][0])
        m["b_dn"] = np.ascontiguousarray(inp["b_down"][0])
        for nm, key in (("w2bd", "w2"), ("a2bd", "a2")):
            bd = np.zeros((16, 128, 128), np.float32)
            for h in range(16):
                for d in range(2):
                    bd[h, d * 64:(d + 1) * 64, d * 64:(d + 1) * 64] = inp[key][0, d, :, h * 64:(h + 1) * 64]
            m[nm] = bd
        maps.append(m)
    return maps


def kernel(**inputs):
    inp = {k: np.asarray(v) for k, v in inputs.items()}
    nc = build_program()
    in_maps = make_in_maps(inp)
    res = run_bass_kernel_spmd(nc, in_maps, core_ids=list(range(8)))
    return np.stack([r["out"] for r in res.results], axis=0)
```
